# Optimizing a Trainium2 kernel written in Bass

```python
import math
import jax
import jax.numpy as jnp
from jax import lax
import numpy as np

D_MODEL = 1024
BATCH = 8
SEQ = 2048
DEPTH = 1

N_META = 16
CHUNK = 64
META_PAD = (-N_META) % CHUNK
N_FGROUPS = 4
FGROUP_DIM = 128
F_WIDTH = N_FGROUPS * FGROUP_DIM
N_HEADS = 8
HEAD_DK = 128
HEAD_DV = 128
QK_WIDTH = N_HEADS * HEAD_DK
V_WIDTH = N_HEADS * HEAD_DV
QKV_WIDTH = 2 * QK_WIDTH + V_WIDTH
CONV_K = 5
SPLIT_SIZES = (F_WIDTH, QKV_WIDTH, V_WIDTH, 4 * N_HEADS, D_MODEL, D_MODEL)
IN_WIDTH = sum(SPLIT_SIZES)
N_GROUPS = 8
EXPERTS_PER_GROUP = 8
N_EXPERTS = N_GROUPS * EXPERTS_PER_GROUP
TOP_K = 2
D_EXPERT = 256
EXPERT_BLOCK = 128
NORM_EPS = 1e-6

kernel_name = "fnet_bigdn_hier_moe_hybrid"


def rms_norm(x, gain):
    xf = x.astype(jnp.float32)
    y = xf * lax.rsqrt(jnp.mean(xf * xf, axis=-1, keepdims=True) + NORM_EPS)
    return (y * gain.astype(jnp.float32)).astype(x.dtype)


def l2_normalize(x):
    return x * lax.rsqrt(jnp.sum(x * x, axis=-1, keepdims=True) + NORM_EPS)


def depthwise_conv_centred(x, w):
    k = w.shape[0]
    return lax.conv_general_dilated(
        x, w[:, None, :].astype(x.dtype), window_strides=(1,),
        padding=[((k - 1) // 2, k // 2)],
        dimension_numbers=("NWC", "WIO", "NWC"),
        feature_group_count=x.shape[-1])


def gated_delta_chunked(q, k, v, g, beta):
    b, h, lp, dk = k.shape
    dv = v.shape[-1]
    nc = lp // CHUNK
    q = q.reshape(b, h, nc, CHUNK, dk)
    k = k.reshape(b, h, nc, CHUNK, dk)
    v = v.reshape(b, h, nc, CHUNK, dv)
    beta = beta.reshape(b, h, nc, CHUNK)
    g = jnp.cumsum(g.reshape(b, h, nc, CHUNK), axis=-1)
    lower = jnp.tril(jnp.ones((CHUNK, CHUNK), dtype=bool))
    strict = jnp.tril(jnp.ones((CHUNK, CHUNK), dtype=bool), -1)
    diff = g[..., :, None] - g[..., None, :]
    decay = jnp.where(lower, jnp.exp(jnp.where(lower, diff, 0.0)), 0.0)
    k_beta = k * beta[..., None]
    a_mat = jnp.where(strict, jnp.einsum("bhncd,bhnsd->bhncs", k_beta, k) * decay, 0.0)
    eye = jnp.eye(CHUNK, dtype=a_mat.dtype)
    t_mat = lax.linalg.triangular_solve(
        eye + a_mat, jnp.broadcast_to(eye, a_mat.shape),
        left_side=True, lower=True, unit_diagonal=True)
    u = jnp.einsum("bhncs,bhnsv->bhncv", t_mat, v * beta[..., None])
    w = jnp.einsum("bhncs,bhnsd->bhncd", t_mat, k_beta * jnp.exp(g)[..., None])
    qk = jnp.where(lower, jnp.einsum("bhncd,bhnsd->bhncs", q, k) * decay, 0.0)
    q_dec = q * jnp.exp(g)[..., None]
    k_dec = k * jnp.exp(g[..., -1:] - g)[..., None]
    chunk_decay = jnp.exp(g[..., -1])

    def step(state, inp):
        q_c, k_c, u_c, w_c, qk_c, dec_c = inp
        v_new = u_c - jnp.einsum("bhcd,bhdv->bhcv", w_c, state)
        o_c = (jnp.einsum("bhcd,bhdv->bhcv", q_c, state)
               + jnp.einsum("bhcs,bhsv->bhcv", qk_c, v_new))
        state = state * dec_c[..., None, None] + jnp.einsum("bhcd,bhcv->bhdv", k_c, v_new)
        return state, o_c

    xs = tuple(jnp.moveaxis(t, 2, 0) for t in (q_dec, k_dec, u, w, qk, chunk_decay))
    state0 = jnp.zeros((b, h, dk, dv), jnp.float32)
    _, o = lax.scan(step, state0, xs)
    return jnp.moveaxis(o, 0, 2).reshape(b, h, lp, dv)


def pad_to_chunks(t):
    t = jnp.pad(t, [(0, 0), (META_PAD, 0)] + [(0, 0)] * (t.ndim - 2))
    return jnp.moveaxis(t, 1, 2)


def hybrid_mixer(h, norm_g, w_in, conv_w, a_log_f, dt_bias_f, a_log_b, dt_bias_b,
                 out_norm_g, w_fourier, w_delta, w_out):
    bsz, seq_len, _ = h.shape
    hn = rms_norm(h, norm_g)
    proj = hn @ w_in
    points = np.cumsum(SPLIT_SIZES)[:-1].tolist()
    f_in, qkv, z, ab, gate_f, gate_d = jnp.split(proj, points, axis=-1)

    fg = f_in.reshape(bsz, seq_len, N_FGROUPS, FGROUP_DIM).astype(jnp.float32)
    f_mix = jnp.real(jnp.fft.fft2(fg, axes=(1, 3), norm="ortho")).astype(h.dtype)
    y_f = f_mix.reshape(bsz, seq_len, F_WIDTH) @ w_fourier

    qkv = jax.nn.silu(depthwise_conv_centred(qkv, conv_w))
    q, k, v = jnp.split(qkv, [QK_WIDTH, 2 * QK_WIDTH], axis=-1)
    q = l2_normalize(q.reshape(bsz, seq_len, N_HEADS, HEAD_DK).astype(jnp.float32)) * (HEAD_DK ** -0.5)
    k = l2_normalize(k.reshape(bsz, seq_len, N_HEADS, HEAD_DK).astype(jnp.float32))
    v = v.reshape(bsz, seq_len, N_HEADS, HEAD_DV).astype(jnp.float32)
    a_f, b_f, a_b, b_b = jnp.split(ab.astype(jnp.float32), 4, axis=-1)
    g_f = -jnp.exp(a_log_f.astype(jnp.float32)) * jax.nn.softplus(a_f + dt_bias_f.astype(jnp.float32))
    g_b = -jnp.exp(a_log_b.astype(jnp.float32)) * jax.nn.softplus(a_b + dt_bias_b.astype(jnp.float32))
    beta_f = jax.nn.sigmoid(b_f)
    beta_b = jax.nn.sigmoid(b_b)

    qp, kp, vp = pad_to_chunks(q), pad_to_chunks(k), pad_to_chunks(v)
    o_fwd = gated_delta_chunked(qp, kp, vp, pad_to_chunks(g_f), pad_to_chunks(beta_f))
    flip = lambda t: jnp.flip(t, axis=2)
    o_bwd = flip(gated_delta_chunked(flip(qp), flip(kp), flip(vp),
                                     flip(pad_to_chunks(g_b)), flip(pad_to_chunks(beta_b))))
    o = jnp.moveaxis(o_fwd + o_bwd, 1, 2)[:, META_PAD:]
    zf = z.reshape(bsz, seq_len, N_HEADS, HEAD_DV).astype(jnp.float32)
    o = rms_norm(o, out_norm_g) * jax.nn.silu(zf)
    y_d = o.reshape(bsz, seq_len, V_WIDTH).astype(h.dtype) @ w_delta

    merged = jax.nn.sigmoid(gate_f) * y_f + jax.nn.sigmoid(gate_d) * y_d
    return merged @ w_out


def hierarchical_moe(h, norm_g, w_rg, b_rg, w_re, b_re, w_gate, w_up, w_down):
    bsz, seq_len, d = h.shape
    hn = rms_norm(h, norm_g).reshape(-1, d)
    n_tok = hn.shape[0]
    gl = (hn @ w_rg).astype(jnp.float32) + b_rg.astype(jnp.float32)
    gp = jax.nn.softmax(gl, axis=-1)
    p_group, g_idx = lax.top_k(gp, 1)
    el = (hn @ w_re).astype(jnp.float32) + b_re.astype(jnp.float32)
    el = el.reshape(n_tok, N_GROUPS, EXPERTS_PER_GROUP)
    el_g = jnp.take_along_axis(el, g_idx[:, :, None], axis=1)[:, 0]
    ep = jax.nn.softmax(el_g, axis=-1)
    top_w, top_i = lax.top_k(ep, TOP_K)
    top_w = top_w / jnp.sum(top_w, axis=-1, keepdims=True)
    weights = p_group * top_w
    expert_idx = g_idx * EXPERTS_PER_GROUP + top_i

    n_assign = n_tok * TOP_K
    n_blocks = -(-n_assign // EXPERT_BLOCK) + N_EXPERTS
    n_slots = n_blocks * EXPERT_BLOCK
    flat_e = expert_idx.reshape(-1).astype(jnp.int32)
    flat_tok = jnp.repeat(jnp.arange(n_tok, dtype=jnp.int32), TOP_K)
    flat_w = weights.reshape(-1)
    order = jnp.argsort(flat_e, stable=True)
    e_sorted = flat_e[order]
    counts = jnp.bincount(flat_e, length=N_EXPERTS)
    padded_counts = (counts + EXPERT_BLOCK - 1) // EXPERT_BLOCK * EXPERT_BLOCK
    padded_ends = jnp.cumsum(padded_counts)
    padded_start = padded_ends - padded_counts
    start = jnp.cumsum(counts) - counts
    rank = jnp.arange(n_assign, dtype=jnp.int32) - start[e_sorted]
    dest = padded_start[e_sorted] + rank
    slot_tok = jnp.zeros((n_slots,), jnp.int32).at[dest].set(flat_tok[order])
    slot_w = jnp.zeros((n_slots,), hn.dtype).at[dest].set(flat_w[order].astype(hn.dtype))
    block_start = jnp.arange(n_blocks, dtype=jnp.int32) * EXPERT_BLOCK
    block_expert = jnp.minimum(jnp.searchsorted(padded_ends, block_start, side="right"),
                               N_EXPERTS - 1).astype(jnp.int32)
    xb = hn[slot_tok].reshape(n_blocks, EXPERT_BLOCK, d)

    def expert_block(args):
        x_blk, e = args
        return (jax.nn.silu(x_blk @ w_gate[e]) * (x_blk @ w_up[e])) @ w_down[e]

    yb = lax.map(expert_block, (xb, block_expert)).reshape(n_slots, d)
    out = jnp.zeros((n_tok, d), hn.dtype).at[slot_tok].add(yb * slot_w[:, None])
    return out.reshape(bsz, seq_len, d)


def setup_inputs(seed: int = 0) -> dict:
    key = jax.random.key(seed)
    ks = jax.random.split(key, 24)
    nrm = lambda k, shape, scale: jax.random.normal(k, shape, jnp.float32) * scale
    gain = lambda k, shape: 1.0 + 0.05 * jax.random.normal(k, shape, jnp.float32)

    def a_log(k):
        return jnp.log(jax.random.uniform(k, (DEPTH, N_HEADS), jnp.float32, minval=1.0, maxval=16.0))

    def dt_bias(k):
        dt = jnp.exp(jax.random.uniform(k, (DEPTH, N_HEADS), jnp.float32,
                                        minval=math.log(1e-3), maxval=math.log(1e-1)))
        return dt + jnp.log(-jnp.expm1(-dt))

    return {
        "x": nrm(ks[0], (BATCH, SEQ, D_MODEL), 1.0),
        "meta_tokens": nrm(ks[1], (N_META, D_MODEL), 1.0),
        "norm1_g": gain(ks[2], (DEPTH, D_MODEL)),
        "w_in": nrm(ks[3], (DEPTH, D_MODEL, IN_WIDTH), D_MODEL ** -0.5),
        "conv_w": nrm(ks[4], (DEPTH, CONV_K, QKV_WIDTH), CONV_K ** -0.5),
        "a_log_fwd": a_log(ks[5]),
        "dt_bias_fwd": dt_bias(ks[6]),
        "a_log_bwd": a_log(ks[7]),
        "dt_bias_bwd": dt_bias(ks[8]),
        "out_norm_g": gain(ks[9], (DEPTH, HEAD_DV)),
        "w_fourier": nrm(ks[10], (DEPTH, F_WIDTH, D_MODEL), F_WIDTH ** -0.5),
        "w_delta": nrm(ks[11], (DEPTH, V_WIDTH, D_MODEL), V_WIDTH ** -0.5),
        "w_out": nrm(ks[12], (DEPTH, D_MODEL, D_MODEL), D_MODEL ** -0.5),
        "norm2_g": gain(ks[13], (DEPTH, D_MODEL)),
        "w_router_group": nrm(ks[14], (DEPTH, D_MODEL, N_GROUPS), D_MODEL ** -0.5),
        "b_router_group": nrm(ks[15], (DEPTH, N_GROUPS), 0.01),
        "w_router_expert": nrm(ks[16], (DEPTH, D_MODEL, N_EXPERTS), D_MODEL ** -0.5),
        "b_router_expert": nrm(ks[17], (DEPTH, N_EXPERTS), 0.01),
        "w_gate_e": nrm(ks[18], (DEPTH, N_EXPERTS, D_MODEL, D_EXPERT), D_MODEL ** -0.5),
        "w_up_e": nrm(ks[19], (DEPTH, N_EXPERTS, D_MODEL, D_EXPERT), D_MODEL ** -0.5),
        "w_down_e": nrm(ks[20], (DEPTH, N_EXPERTS, D_EXPERT, D_MODEL), D_EXPERT ** -0.5),
        "final_norm_g": gain(ks[21], (D_MODEL,)),
    }


def reference(x, meta_tokens, norm1_g, w_in, conv_w, a_log_fwd, dt_bias_fwd, a_log_bwd,
              dt_bias_bwd, out_norm_g, w_fourier, w_delta, w_out, norm2_g, w_router_group,
              b_router_group, w_router_expert, b_router_expert, w_gate_e, w_up_e, w_down_e,
              final_norm_g):
    bsz = x.shape[0]
    meta = jnp.broadcast_to(meta_tokens[None].astype(x.dtype), (bsz, N_META, D_MODEL))
    h = jnp.concatenate([meta, x], axis=1)
    for layer in range(DEPTH):
        h = h + hybrid_mixer(h, norm1_g[layer], w_in[layer], conv_w[layer],
                             a_log_fwd[layer], dt_bias_fwd[layer], a_log_bwd[layer],
                             dt_bias_bwd[layer], out_norm_g[layer], w_fourier[layer],
                             w_delta[layer], w_out[layer])
        h = h + hierarchical_moe(h, norm2_g[layer], w_router_group[layer], b_router_group[layer],
                                 w_router_expert[layer], b_router_expert[layer],
                                 w_gate_e[layer], w_up_e[layer], w_down_e[layer])
    return rms_norm(h, final_norm_g)[:, N_META:]
```

```python
import contextlib
import os
import numpy as np
import ml_dtypes
import concourse.bass as bass
import concourse.mybir as mybir
from concourse.bass_utils import run_bass_kernel_spmd

F32 = mybir.dt.float32
BF16 = mybir.dt.bfloat16
I32 = mybir.dt.int32
AF = mybir.ActivationFunctionType
ALU = mybir.AluOpType
AX = mybir.AxisListType

D = 1024
NM = 16
SEQ = 2048
L = NM + SEQ
NH = 8
INW = 6688
EPS = 1e-6
NEXP = 64
CAP = 128
DE = 256
NT = SEQ // 128


class Sched:
    def __init__(self, nc):
        self.nc = nc
        self.eng = {"pe": nc.tensor, "act": nc.scalar, "dve": nc.vector, "pool": nc.gpsimd, "sp": nc.sync}
        self.sem = {e: nc.alloc_semaphore(f"s_{e}") for e in self.eng}
        self.cnt = {e: 0 for e in self.eng}
        self.seen = {e: {} for e in self.eng}
        self.semobj = {}
        self.bufs = {}
        self.nsem = 0
        self.excl = set()
        self.free_sems = []

    def _wait(self, e, ev):
        sem, val = ev
        if self.seen[e].get(sem.name, 0) >= val:
            return
        self.eng[e].wait_ge(sem, val)
        self.seen[e][sem.name] = val

    def deps(self, e, reads, writes):
        evs = []
        own = self.sem[e]
        for b in reads:
            st = self.bufs.get(b)
            if st and st["w"]:
                evs.append(st["w"])
            if st and b in self.excl:
                for r in st["r"]:
                    if r[0] is not own:
                        evs.append(r)
        for b in writes:
            st = self.bufs.get(b)
            if st:
                if st["w"]:
                    evs.append(st["w"])
                for r in st["r"]:
                    if r[0] is own:
                        continue
                    evs.append(r)
        best = {}
        for sem, val in evs:
            if e == "pe" and sem is own:
                continue
            if sem.name not in best or best[sem.name][1] < val:
                best[sem.name] = (sem, val)
        for ev in best.values():
            self._wait(e, ev)

    def record(self, ev, reads, writes):
        for b in reads:
            st = self.bufs.setdefault(b, {"w": None, "r": []})
            st["r"] = [r for r in st["r"] if r[0] is not ev[0]] + [ev]
        for b in writes:
            self.bufs[b] = {"w": ev, "r": []}

    def op(self, e, fn, r=(), w=()):
        self.deps(e, r, w)
        ins = fn()
        self.cnt[e] += 1
        ins.then_inc(self.sem[e], 1)
        ev = (self.sem[e], self.cnt[e])
        self.record(ev, r, w)
        return ev

    def dma(self, e, out, in_, r=(), w=(), key=None, indirect=None, **kw):
        self.deps(e, r, w)
        key = key or (w[0] if w else r[0])
        if key not in self.semobj:
            if self.free_sems:
                self.semobj[key] = self.free_sems.pop()
            else:
                self.semobj[key] = [self.nc.alloc_semaphore(f"d{self.nsem}"), 0]
                self.nsem += 1
        so = self.semobj[key]
        if indirect is None:
            ins = self.eng[e].dma_start(out=out, in_=in_, **kw)
        else:
            ins = indirect()
        so[1] += 16
        ins.then_inc(so[0], 16)
        ev = (so[0], so[1])
        self.record(ev, r, w)
        return ev

    def barrier(self):
        allev = {}
        for st in self.bufs.values():
            for ev in ([st["w"]] if st["w"] else []) + st["r"]:
                if ev[0].name not in allev or allev[ev[0].name][1] < ev[1]:
                    allev[ev[0].name] = ev
        for e in self.eng:
            for ev in allev.values():
                self._wait(e, ev)
        self.free_sems.extend(self.semobj.values())
        self.semobj = {}

    def finish(self):
        allev = {}
        for st in self.bufs.values():
            for ev in ([st["w"]] if st["w"] else []) + st["r"]:
                if ev[0].name not in allev or allev[ev[0].name][1] < ev[1]:
                    allev[ev[0].name] = ev
        for ev in allev.values():
            self._wait("sp", ev)


def col_tiles():
    tl = []
    for g in range(4):
        tl.append(("f", g, g * 128))
    for h in range(NH):
        tl.append(("q", h, 512 + h * 128))
    for h in range(NH):
        tl.append(("k", h, 1536 + h * 128))
    for h in range(NH):
        tl.append(("v", h, 2560 + h * 128))
    for h in range(NH):
        tl.append(("z", h, 3584 + h * 128))
    for j in range(8):
        tl.append(("gf", j, 4640 + j * 128))
    for j in range(8):
        tl.append(("gd", j, 5664 + j * 128))
    return tl


PBLK = [(0, 512), (512, 512), (1024, 512), (1536, 512), (2048, 16)]
RBLK = [(16 + 512 * i, 512) for i in range(4)]


def build(stage=99, debug=False):
    nc = bass.Bass("TRN2", target_bir_lowering=False)
    S = Sched(nc)
    global LAST_SCHED
    LAST_SCHED = S
    op, dma = S.op, S.dma
    V, A, PE, G = nc.vector, nc.scalar, nc.tensor, nc.gpsimd

    def din(name, shape, dt=F32):
        return nc.dram_tensor(name, list(shape), dt, kind="ExternalInput").ap()

    x = din("x", [SEQ, D])
    meta = din("meta_tokens", [NM, D])
    norm1_g = din("norm1_g", [1, D])
    w_in = din("w_in", [1, D, INW])
    conv_w = din("conv_w", [1, 5, 3072])
    a_log_f = din("a_log_fwd", [1, 8]); dt_b_f = din("dt_bias_fwd", [1, 8])
    a_log_b = din("a_log_bwd", [1, 8]); dt_b_b = din("dt_bias_bwd", [1, 8])
    out_norm_g = din("out_norm_g", [1, 128])
    w_fourier = din("w_fourier", [1, 512, D])
    w_delta = din("w_delta", [1, D, D])
    w_out = din("w_out", [1, D, D])
    norm2_g = din("norm2_g", [1, D])
    w_rg = din("w_router_group", [1, D, 8]); b_rg = din("b_router_group", [1, 8])
    w_re = din("w_router_expert", [1, D, 64]); b_re = din("b_router_expert", [1, 64])
    w_gate_e = din("w_gate_e", [1, NEXP, D, DE]); w_up_e = din("w_up_e", [1, NEXP, D, DE])
    w_down_e = din("w_down_e", [1, NEXP, DE, D])
    final_g = din("final_norm_g", [D])
    c_identb = din("c_identb", [128, 128], BF16)
    c_identf = din("c_identf", [128, 128], F32)
    c_cosL = din("c_cosL", [L, L], BF16)
    c_nsinL = din("c_nsinL", [L, L], BF16)
    c_cs128 = din("c_cs128", [128, 256], BF16)
    c_masks = din("c_masks", [7, 128, 128], F32)
    c_ecap = din("c_ecap", [128, 64], F32)

    out = nc.dram_tensor("out", [SEQ, D], F32, kind="ExternalOutput").ap()

    dbg = {}

    def scratch(name, shape, dt):
        kind = "ExternalOutput" if debug else "Internal"
        t = nc.dram_tensor(name, list(shape), dt, kind=kind).ap()
        dbg[name] = t
        return t

    qT_s = scratch("qT_s", [NH, 128, L], BF16)
    kT_s = scratch("kT_s", [NH, 128, L], BF16)
    vT_s = scratch("vT_s", [NH, 128, L], BF16)
    z_s = scratch("z_s", [NH, 128, SEQ], BF16)
    gf_s = scratch("gf_s", [8, 128, SEQ], BF16)
    gd_s = scratch("gd_s", [8, 128, SEQ], BF16)
    ab_s = scratch("ab_s", [L, 32], F32)
    fin_s = scratch("fin_s", [4, 128, L], BF16)

    wq_g = nc.dram_tensor("wq_g", [NEXP, 128, 8 * DE], BF16, kind="Internal").ap()
    wq_u = nc.dram_tensor("wq_u", [NEXP, 128, 8 * DE], BF16, kind="Internal").ap()
    wq_d = nc.dram_tensor("wq_d", [NEXP, 128, 2 * D], BF16, kind="Internal").ap()

    def precast_gen():
        for e in range(NEXP):
            kk_ = ("pc", e // 2)
            dma("pool", wq_g[e], w_gate_e[0, e].rearrange("(p kt) j -> p (kt j)", kt=8), w=[("wq", e // 2)], key=kk_)
            yield
            dma("pool", wq_u[e], w_up_e[0, e].rearrange("(p kt) j -> p (kt j)", kt=8), w=[("wq", e // 2)], key=kk_)
            yield
            dma("pool", wq_d[e].rearrange("p (jt d) -> p jt d", jt=2), w_down_e[0, e].rearrange("(jt p) d -> p jt d", p=128), w=[("wq", e // 2)], key=kk_)
            yield

    pcg = precast_gen() if (stage >= 5 and os.environ.get("PRECAST", "1") == "1") else iter(())

    def pc_step(k=1):
        for _ in range(k):
            next(pcg, None)

    with contextlib.ExitStack() as gst:
        def sb(name, shape, dt, st=gst):
            return st.enter_context(nc.sbuf_tensor(name, list(shape), dt))

        def ps(name, shape, dt, st=gst):
            S.excl.add(name)
            return st.enter_context(nc.psum_tensor(name, list(shape), dt))

        identb = sb("identb", [128, 128], BF16)
        identf = sb("identf", [128, 128], F32)
        epst = sb("epst", [128, 1], F32)
        dma("sp", identb[:], c_identb, w=["identb"])
        dma("sp", identf[:], c_identf, w=["identf"])
        op("dve", lambda: V.memset(epst[:], EPS), w=["epst"])
        Xs = nc.dram_tensor("Xs", [NEXP * CAP, D], BF16, kind="Internal").ap()
        Ys = nc.dram_tensor("Ys", [NEXP * CAP, D], F32, kind="Internal").ap()
        zfill = sb("zfill", [128, 2048], BF16)
        op("dve", lambda: V.memset(zfill[:], 0.0), w=["zfill"])
        Xs_v = Xs.rearrange("(p a) c -> p (a c)", p=128)
        for i in range(32):
            dma("pool", Xs_v[:, i * 2048:(i + 1) * 2048], zfill[:], r=["zfill"], w=["Xs"], key="Xs")

        with contextlib.ExitStack() as st:
            hnT = sb("hnT", [128, 8, L], BF16, st)
            stP1 = contextlib.ExitStack()
            g1b = sb("g1b", [128, D], F32, stP1)
            dma("sp", g1b[:], norm1_g[0].partition_broadcast(128), w=["g1b"])
            xt = [sb(f"xt{i}", [128, D], F32, stP1) for i in range(2)]
            junk = sb("junk", [128, D], BF16, stP1)
            hnb = [sb(f"hnb{i}", [128, D], BF16, stP1) for i in range(2)]
            ss = [sb(f"ss{i}", [128, 1], F32, stP1) for i in range(2)]
            with contextlib.ExitStack() as st1:
                tp = [ps(f"tp{i}", [128, 8, 128], BF16, st1) for i in range(2)]

                for t in (range(NT + 1) if 'KT' not in os.environ else [int(v) for v in os.environ['KT'].split(',')]):
                    i = t % 2
                    if t == 0:
                        n, src, p0 = NM, meta, 0
                    else:
                        n, src, p0 = 128, x[(t - 1) * 128:t * 128, :], NM + (t - 1) * 128
                    dma("sp", xt[i][0:n, :], src, w=[f"xt{i}"])
                    op("act", lambda: A.activation(out=junk[0:n, :], in_=xt[i][0:n, :], func=AF.Square, accum_out=ss[i][0:n, :]),
                       r=[f"xt{i}"], w=["junk", f"ss{i}"])
                    op("act", lambda: A.activation(out=ss[i][0:n, :], in_=ss[i][0:n, :], func=AF.Sqrt, scale=1.0 / D, bias=epst[0:n, :]),
                       r=[f"ss{i}", "epst"], w=[f"ss{i}"])
                    op("dve", lambda: V.reciprocal(out=ss[i][0:n, :], in_=ss[i][0:n, :]), r=[f"ss{i}"], w=[f"ss{i}"])
                    op("dve", lambda: V.scalar_tensor_tensor(out=hnb[i][0:n, :], in0=xt[i][0:n, :], scalar=ss[i][0:n, 0:1], in1=g1b[0:n, :],
                                                             op0=ALU.mult, op1=ALU.mult),
                       r=[f"xt{i}", f"ss{i}", "g1b"], w=[f"hnb{i}"])
                    for kt in range(8):
                        op("pe", lambda: PE.transpose(tp[i][:, kt, 0:n], hnb[i][0:n, kt * 128:(kt + 1) * 128], identb[0:n, 0:n]),
                           r=[f"hnb{i}", "identb"], w=[f"tp{i}"])
                    op("act", lambda: A.copy(out=hnT[:, :, p0:p0 + n], in_=tp[i][:, :, 0:n]), r=[f"tp{i}"], w=[("hnT", t)])
                S.barrier()
            stP1.close()
            hnT_all = [("hnT", t) for t in range(NT + 1)]
            if 'KT' in os.environ:
                hnT_all = [("hnT", int(v)) for v in os.environ['KT'].split(',')]

            if debug:
                hnT_d = scratch("hnT_d", [8, 128, L], BF16)
                for kt in range(8):
                    dma("sp", hnT_d[kt], hnT[:, kt, :], r=hnT_all, key="dbg_hnT")

            cwrow = sb("cwrow", [5, 3072], F32, st)
            dma("sp", cwrow[:], conv_w[0], w=["cwrow"])
            cw = sb("cw", [128, 24, 5], F32, st)
            with contextlib.ExitStack() as st1:
                pcw = ps("pcw", [128, 24, 5], F32, st1)
                for tt in range(24):
                    op("pe", lambda: PE.matmul(pcw[:, tt, :], lhsT=cwrow[:, tt * 128:(tt + 1) * 128], rhs=identf[0:5, 0:5], start=True, stop=True),
                       r=["cwrow", "identf"], w=["pcw"])
                op("dve", lambda: V.tensor_copy(out=cw[:], in_=pcw[:]), r=["pcw"], w=["cw"])
                S.barrier()

            NW = 4
            wt = [sb(f"wt{i}", [128, 8, 512], BF16, st) for i in range(NW)]
            NSTG = 3
            stg = [sb(f"stg{i}", [128, L + 4], BF16, st) for i in range(NSTG)]
            dgt = [sb(f"dgt{i}", [128, 5, 128], BF16, st) for i in range(3)]
            for i in range(NSTG):
                op("pool", lambda: G.memset(stg[i][:], 0.0), w=[f"stg{i}"])
            sact2 = [sb(f"sact{i}", [128, L], F32, st) for i in range(3)]
            sq2 = [sb(f"sq{i}", [128, L], BF16, st) for i in range(3)]
            rn2 = [sb(f"rn{i}", [128, L], F32, st) for i in range(3)]
            NOB = 3
            ob = [sb(f"ob{i}", [128, L], BF16, st) for i in range(NOB)]
            onesb = sb("onesb", [128, 128], BF16, st)
            op("dve", lambda: V.memset(onesb[:], 1.0), w=["onesb"])
            w_in_v = w_in[0].rearrange("(kt p) c -> p kt c", p=128)
            tiles_all = col_tiles()
            conv_t = [i for i, tl in enumerate(tiles_all) if tl[0] in ("q", "k", "v")]
            plain_t = [i for i, tl in enumerate(tiles_all) if tl[0] not in ("q", "k", "v")]
            order = []
            for j in range(max(len(conv_t), len(plain_t))):
                if j < len(conv_t):
                    order.append(conv_t[j])
                if j < len(plain_t):
                    order.append(plain_t[j])
            if stage < 1:
                order = order[:int(stage * 100)]
            grp_buf = {}
            cnt = {"na": 0, "nob": 0, "nconv": 0, "npc": 0}
            tstate = {}
            with contextlib.ExitStack() as st1:
                acc = [ps(f"acc{i}", [128, 512], F32, st1) for i in range(4)]
                ssb = [ps(f"ssb{i}", [128, 512], F32, st1) for i in range(2)]
                pcv = [ps(f"pcv{i}", [128, 512], F32, st1) for i in range(2)]

                def next_ob():
                    oi = cnt["nob"] % NOB
                    cnt["nob"] += 1
                    return oi

                def stageA(ti):
                    typ, idx, c0 = tiles_all[ti]
                    g = ti // 4
                    if g not in grp_buf:
                        grp_buf[g] = len(grp_buf) % NW
                        gc0 = tiles_all[g * 4][2]
                        dma("pool", wt[grp_buf[g]][:], w_in_v[:, :, gc0:gc0 + 512], w=[f"wt{grp_buf[g]}"])
                    wi = grp_buf[g]
                    wo_ = (ti % 4) * 128
                    conv = typ in ("q", "k", "v")
                    blks = PBLK if typ in ("f", "q", "k", "v") else RBLK
                    stt = {}
                    if conv:
                        stt["si"] = cnt["nconv"] % NSTG
                        stt["ci"] = cnt["nconv"] % 3
                        cnt["nconv"] += 1
                    else:
                        stt["oi"] = next_ob()
                    tstate[ti] = stt
                    for (b0, bn) in blks:
                        ai = cnt["na"] % 4
                        cnt["na"] += 1
                        for kt in range(8):
                            op("pe", lambda: PE.matmul(acc[ai][:, 0:bn], lhsT=wt[wi][:, kt, wo_:wo_ + 128], rhs=hnT[:, kt, b0:b0 + bn], start=(kt == 0), stop=(kt == 7)),
                               r=[f"wt{wi}"] + hnT_all, w=[f"acc{ai}"])
                        if conv:
                            si = stt["si"]
                            op("act", lambda: A.copy(out=stg[si][:, 2 + b0:2 + b0 + bn], in_=acc[ai][:, 0:bn]), r=[f"acc{ai}"], w=[f"stg{si}"])
                        else:
                            oi = stt["oi"]
                            if typ == "f":
                                op("act", lambda: A.copy(out=ob[oi][:, b0:b0 + bn], in_=acc[ai][:, 0:bn]), r=[f"acc{ai}"], w=[f"ob{oi}"])
                            else:
                                fn_ = AF.Silu if typ == "z" else AF.Sigmoid
                                op("act", lambda: A.activation(out=ob[oi][:, b0:b0 + bn], in_=acc[ai][:, 0:bn], func=fn_), r=[f"acc{ai}"], w=[f"ob{oi}"])
                        yield
                    if not conv:
                        oi = stt["oi"]
                        if typ == "f":
                            dma("sp", fin_s[idx], ob[oi][:], r=[f"ob{oi}"], key=f"ob{oi}")
                        else:
                            dst = {"z": z_s, "gf": gf_s, "gd": gd_s}[typ]
                            dma("sp", dst[idx], ob[oi][:, NM:L], r=[f"ob{oi}"], key=f"ob{oi}")

                def stageB(ti):
                    typ, idx, c0 = tiles_all[ti]
                    if typ not in ("q", "k", "v"):
                        return
                        yield
                    stt = tstate[ti]
                    si, ci = stt["si"], stt["ci"]
                    sact, sq = sact2[ci], sq2[ci]
                    ks_, kq = f"sact{ci}", f"sq{ci}"
                    ct = {"q": 0, "k": 8, "v": 16}[typ] + idx
                    dg, kdg = dgt[ci], f"dgt{ci}"
                    for kk in range(5):
                        op("dve", lambda: V.tensor_scalar_mul(out=dg[:, kk, :], in0=identb[:], scalar1=cw[:, ct, kk:kk + 1]), r=["identb", "cw"], w=[kdg])
                    if typ == "v":
                        oi = next_ob()
                    for bi, (b0, bn) in enumerate(PBLK):
                        pi = cnt["npc"] % 2
                        cnt["npc"] += 1
                        for kk in range(5):
                            op("pe", lambda: PE.matmul(pcv[pi][:, 0:bn], lhsT=dg[:, kk, :], rhs=stg[si][:, b0 + kk:b0 + kk + bn], start=(kk == 0), stop=(kk == 4)),
                               r=[kdg, f"stg{si}"], w=[f"pcv{pi}"])
                        if typ == "v":
                            op("act", lambda: A.activation(out=ob[oi][:, b0:b0 + bn], in_=pcv[pi][:, 0:bn], func=AF.Silu), r=[f"pcv{pi}"], w=[f"ob{oi}"])
                        else:
                            op("act", lambda: A.activation(out=sact[:, b0:b0 + bn], in_=pcv[pi][:, 0:bn], func=AF.Silu), r=[f"pcv{pi}"], w=[ks_])
                        yield
                    if typ == "v":
                        dma("sp", vT_s[idx], ob[oi][:], r=[f"ob{oi}"], key=f"ob{oi}")
                    else:
                        op("act", lambda: A.activation(out=sq[:], in_=sact[:], func=AF.Square), r=[ks_], w=[kq])

                def stageC(ti):
                    typ, idx, c0 = tiles_all[ti]
                    if typ not in ("q", "k"):
                        return
                    stt = tstate[ti]
                    ci = stt["ci"]
                    sact, sq, rn = sact2[ci], sq2[ci], rn2[ci]
                    ks_, kq, kr = f"sact{ci}", f"sq{ci}", f"rn{ci}"
                    for bi, (b0, bn) in enumerate(PBLK):
                        pi = bi % 2
                        op("pe", lambda: PE.matmul(ssb[pi][:, 0:bn], lhsT=onesb[:], rhs=sq[:, b0:b0 + bn], start=True, stop=True), r=["onesb", kq], w=[f"ssb{pi}"])
                        op("act", lambda: A.activation(out=rn[:, b0:b0 + bn], in_=ssb[pi][:, 0:bn], func=AF.Sqrt, bias=epst[:], scale=1.0), r=[f"ssb{pi}", "epst"], w=[kr])
                    op("dve", lambda: V.reciprocal(out=rn[:], in_=rn[:]), r=[kr], w=[kr])
                    oi = next_ob()
                    sc = (128 ** -0.5) if typ == "q" else 1.0
                    op("dve", lambda: V.scalar_tensor_tensor(out=ob[oi][:], in0=sact[:], scalar=sc, in1=rn[:], op0=ALU.mult, op1=ALU.mult), r=[ks_, kr], w=[f"ob{oi}"])
                    dst = qT_s if typ == "q" else kT_s
                    dma("sp", dst[idx], ob[oi][:], r=[f"ob{oi}"], key=f"ob{oi}")

                for s_ in range(len(order) + 4):
                    pc_step(2)
                    gA = stageA(order[s_]) if s_ < len(order) else iter(())
                    gB = stageB(order[s_ - 1]) if 0 <= s_ - 1 < len(order) else iter(())
                    doneA = doneB = False
                    while not (doneA and doneB):
                        if not doneA:
                            doneA = next(gA, "END") == "END"
                        if not doneB:
                            doneB = next(gB, "END") == "END"
                    if 0 <= s_ - 4 < len(order):
                        stageC(order[s_ - 4])
                na = cnt["na"]
                wab = sb("wab", [128, 8, 32], BF16, st)
                dma("pool", wab[:], w_in_v[:, :, 4608:4640], w=["wab"])
                abt = sb("abt", [128, 32], F32, st)
                for t in range(NT + 1 if stage >= 1 else 0):
                    n, p0 = (NM, 0) if t == 0 else (128, NM + (t - 1) * 128)
                    ai = na % 4
                    na += 1
                    for kt in range(8):
                        op("pe", lambda: PE.matmul(acc[ai][0:n, 0:32], lhsT=hnT[:, kt, p0:p0 + n], rhs=wab[:, kt, :], start=(kt == 0), stop=(kt == 7)),
                           r=["wab"] + hnT_all, w=[f"acc{ai}"])
                    op("act", lambda: A.copy(out=abt[0:n, :], in_=acc[ai][0:n, 0:32]), r=[f"acc{ai}"], w=["abt"])
                    dma("sp", ab_s[p0:p0 + n, :], abt[0:n, :], r=["abt"], key="abt")
                S.barrier()
            S.barrier()

        if stage < 2:
            S.finish()
            return nc, dbg
        mrg_s = scratch("mrg_s", [8, 128, SEQ], BF16)
        with contextlib.ExitStack() as st:
            mrgT = sb("mrgT", [128, 8, SEQ], BF16, st)
            finT = sb("finT", [128, 4, L], BF16, st)
            for g in range(4):
                dma("sp", finT[:, g, :], fin_s[g], w=["finT"], key="finT")
            cs128 = sb("cs128", [128, 256], BF16, st)
            dma("sp", cs128[:], c_cs128, w=["cs128"])
            Y = sb("Y", [128, NT + 1, 4, 256], BF16, st)
            fmixT = sb("fmixT", [128, 4, SEQ], BF16, st)
            wf = sb("wf", [128, 4, D], BF16, st)
            dma("pool", wf[:], w_fourier[0].rearrange("(g p) d -> p g d", p=128), w=["wf"])
            CLb = [sb(f"CLb{i}", [128, NT + 1, 512], BF16, st) for i in range(2)]
            SLb = [sb(f"SLb{i}", [128, NT + 1, 512], BF16, st) for i in range(2)]
            sgf = [sb(f"sgf{i}", [128, SEQ], BF16, st) for i in range(2)]
            with contextlib.ExitStack() as st1:
                py = [ps(f"py{i}", [128, 2, 256], F32, st1) for i in range(2)]
                pa = [ps(f"pa{i}", [128, 512], F32, st1) for i in range(4)]
                npy = 0
                for t in range(NT + 1):
                    n, p0 = (NM, 0) if t == 0 else (128, NM + (t - 1) * 128)
                    for g2 in range(2):
                        pi = npy % 2
                        npy += 1
                        for gi in range(2):
                            g = g2 * 2 + gi
                            op("pe", lambda: PE.matmul(py[pi][0:n, gi, :], lhsT=finT[:, g, p0:p0 + n], rhs=cs128[:], start=True, stop=True),
                               r=["finT", "cs128"], w=[f"py{pi}"])
                        op("act", lambda: A.copy(out=Y[0:n, t, g2 * 2:g2 * 2 + 2, :], in_=py[pi][0:n, :, :]), r=[f"py{pi}"], w=["Y"])
                npa = 0
                for bi, (b0, bn) in enumerate(RBLK):
                    ci = bi % 2
                    for (dst, srcm, nm) in ((CLb[ci], c_cosL, f"CLb{ci}"), (SLb[ci], c_nsinL, f"SLb{ci}")):
                        dma("sp", dst[0:NM, 0, :], srcm[0:NM, b0:b0 + bn], w=[nm], key=nm)
                        for hh in range(2):
                            dma("sp", dst[:, 1 + hh * 8:9 + hh * 8, :],
                                srcm[NM + hh * 1024:NM + (hh + 1) * 1024, b0:b0 + bn].rearrange("(t p) c -> p t c", p=128), w=[nm], key=nm)
                    for g in range(4):
                        ai = npa % 4
                        npa += 1
                        for t in range(NT + 1):
                            n = NM if t == 0 else 128
                            op("pe", lambda: PE.matmul(pa[ai][:, :], lhsT=Y[0:n, t, g, 0:128], rhs=CLb[ci][0:n, t, :], start=(t == 0), stop=False),
                               r=["Y", f"CLb{ci}"], w=[f"pa{ai}"])
                            op("pe", lambda: PE.matmul(pa[ai][:, :], lhsT=Y[0:n, t, g, 128:256], rhs=SLb[ci][0:n, t, :], start=False, stop=(t == NT)),
                               r=["Y", f"SLb{ci}"], w=[f"pa{ai}"])
                        op("act", lambda: A.copy(out=fmixT[:, g, b0 - NM:b0 - NM + bn], in_=pa[ai][:, :]), r=[f"pa{ai}"], w=["fmixT"])
                if debug:
                    fmix_d = scratch("fmix_d", [4, 128, SEQ], BF16)
                    for g in range(4):
                        dma("sp", fmix_d[g], fmixT[:, g, :], r=["fmixT"], key="dbg_fmix")
                for dt_ in range(8):
                    gi_ = dt_ % 2
                    dma("sp", sgf[gi_][:], gf_s[dt_], w=[f"sgf{gi_}"])
                    for bi in range(4):
                        ai = npa % 4
                        npa += 1
                        for g in range(4):
                            op("pe", lambda: PE.matmul(pa[ai][:, :], lhsT=wf[:, g, dt_ * 128:(dt_ + 1) * 128], rhs=fmixT[:, g, bi * 512:(bi + 1) * 512],
                                                       start=(g == 0), stop=(g == 3)),
                               r=["wf", "fmixT"], w=[f"pa{ai}"])
                        op("dve", lambda: V.tensor_tensor(out=mrgT[:, dt_, bi * 512:(bi + 1) * 512], in0=pa[ai][:, :], in1=sgf[gi_][:, bi * 512:(bi + 1) * 512], op=ALU.mult),
                           r=[f"pa{ai}", f"sgf{gi_}"], w=[("mrgT", dt_)])
                S.barrier()
            for dt_ in range(8):
                dma("sp", mrg_s[dt_], mrgT[:, dt_, :], r=[("mrgT", dt_)], w=["mrg_s"], key="mrg_s")
            S.barrier()
        if stage < 3:
            S.finish()
            return nc, dbg
        ogT = sb("ogT", [128, 8, SEQ], BF16)
        GE, ge = (G, "pool") if os.environ.get("P4POOL", "1") == "1" else (V, "dve")
        of_s = scratch("of_s", [NT, 128, NH, 128], F32)
        with contextlib.ExitStack() as st:
            masks = sb("masks", [128, 6, 128], F32, st)
            dma("sp", masks[:], c_masks[0:6].rearrange("m p f -> p m f"), w=["masks"])
            onesf = sb("onesf", [128, 128], F32, st)
            op("dve", lambda: V.memset(onesf[:], 1.0), w=["onesf"])
            onec = sb("onec", [128, 1], F32, st)
            op("dve", lambda: V.memset(onec[:], 1.0), w=["onec"])
            gout = sb("gout", [128, 1], F32, st)
            dma("sp", gout[:], out_norm_g.rearrange("o d -> d o"), w=["gout"])
            gball = sb("gball", [128, NT + 1, 2, 2, 8], F32, st)
            dtb = sb("dtb", [128, 2, 8], F32, st)
            nega = sb("nega", [128, 2, 8], F32, st)
            dma("sp", dtb[:, 0, :], dt_b_f[0].partition_broadcast(128), w=["dtb"], key="dtb")
            dma("sp", dtb[:, 1, :], dt_b_b[0].partition_broadcast(128), w=["dtb"], key="dtb")
            dma("sp", nega[:, 0, :], a_log_f[0].partition_broadcast(128), w=["nega"], key="nega")
            dma("sp", nega[:, 1, :], a_log_b[0].partition_broadcast(128), w=["nega"], key="nega")
            op("act", lambda: A.activation(out=nega[:], in_=nega[:], func=AF.Exp), r=["nega"], w=["nega"])
            op("act", lambda: A.mul(out=nega[:], in_=nega[:], mul=-1.0), r=["nega"], w=["nega"])
            abl = sb("abl", [128, NT + 1, 2, 2, 8], F32, st)
            xa = sb("xa", [128, NT + 1, 2, 8], F32, st)
            op("dve", lambda: V.memset(abl[:], 0.0), w=["abl"])
            dma("sp", abl[0:NM, 0], ab_s[0:NM, :].rearrange("p (d t h) -> p d t h", d=2, t=2), w=["abl"], key="abl")
            for hh in range(2):
                dma("sp", abl[:, 1 + hh * 8:9 + hh * 8], ab_s[NM + hh * 1024:NM + (hh + 1) * 1024, :].rearrange("(t p) (d u h) -> p t d u h", p=128, d=2, u=2),
                    w=["abl"], key="abl")
            NT1 = NT + 1
            op("dve", lambda: V.tensor_tensor(out=xa[:], in0=abl[:, :, :, 0, :], in1=dtb[:].unsqueeze(1).to_broadcast([128, NT1, 2, 8]), op=ALU.add), r=["abl", "dtb"], w=["xa"])
            op("act", lambda: A.activation(out=xa[:], in_=xa[:], func=AF.Exp), r=["xa"], w=["xa"])
            op("act", lambda: A.activation(out=xa[:], in_=xa[:], func=AF.Ln, bias=onec[:, :], scale=1.0), r=["xa", "onec"], w=["xa"])
            op("dve", lambda: V.tensor_tensor(out=gball[:, :, :, 0, :], in0=xa[:], in1=nega[:].unsqueeze(1).to_broadcast([128, NT1, 2, 8]), op=ALU.mult), r=["xa", "nega"], w=["gball"])
            op("act", lambda: A.activation(out=gball[:, :, :, 1, :], in_=abl[:, :, :, 1, :], func=AF.Sigmoid), r=["abl"], w=["gball"])
            if debug:
                gb_d = scratch("gb_d", [L, 32], F32)
                for t in range(NT + 1):
                    n, p0 = (NM, 0) if t == 0 else (128, NM + (t - 1) * 128)
                    dma("sp", gb_d[p0:p0 + n, :].rearrange("p (d t h) -> p d t h", d=2, t=2), gball[0:n, t], r=["gball"], key="dbg_gb")

            ob_s = scratch("ob_s", [NT, 128, NH, 128], F32)
            with contextlib.ExitStack() as st2:
                F4 = lambda nm, dt=F32: sb(nm, [128, 4, 128], dt, st2)
                SB = {}
                NSTR = 4
                for sid in range(NSTR):
                    for i in range(2):
                        for nm in ("qTt", "kTt", "vTt"):
                            SB[f"{nm}{i}_{sid}"] = F4(f"{nm}{i}_{sid}", BF16)
                    for nm in ("ktm", "vtm", "Sb_"):
                        SB[f"{nm}_{sid}"] = F4(f"{nm}_{sid}", BF16)
                    for nm in ("Gm", "oTt", "Sf"):
                        SB[f"{nm}_{sid}"] = F4(f"{nm}_{sid}", F32)
                    for nm in ("gcc", "egc", "bgc", "gend", "kds"):
                        SB[f"{nm}_{sid}"] = sb(f"{nm}_{sid}", [128, 4], F32, st2)
                    for nm in ("diff", "DL", "DU", "u_sb", "egb"):
                        SB[f"{nm}_{sid}"] = F4(f"{nm}_{sid}")
                    for nm in ("Ab", "ATb", "QKT", "TTb", "vbt", "kbg", "kdec", "nwT", "qdT", "vnew", "Pb0", "Pb1", "PTb0", "PTb1"):
                        SB[f"{nm}_{sid}"] = F4(f"{nm}_{sid}", BF16)

                def p4_stream(sid, pX, pY):
                    d_, hg = sid // 2, sid % 2
                    H0 = hg * 4
                    K_ = lambda nm: f"{nm}_{sid}"
                    B_ = lambda nm: SB[f"{nm}_{sid}"]
                    Gm, gcc, egc, bgc, gend, kds = B_("Gm"), B_("gcc"), B_("egc"), B_("bgc"), B_("gend"), B_("kds")
                    diff, DL, DU, u_sb, egb = [B_(x) for x in ("diff", "DL", "DU", "u_sb", "egb")]
                    Ab, ATb, QKT, TTb, vbt, kbg, kdec, nwT, qdT, vnew = [B_(x) for x in ("Ab", "ATb", "QKT", "TTb", "vbt", "kbg", "kdec", "nwT", "qdT", "vnew")]
                    ktm, vtm, Sb_, oTt, Sf = B_("ktm"), B_("vtm"), B_("Sb_"), B_("oTt"), B_("Sf")
                    kX, kY = K_("pX"), K_("pY")
                    S.excl.update([kX, kY])
                    pYb = pY[:].bitcast(BF16)
                    op("dve", lambda: V.memset(Sf[:], 0.0), r=[], w=[K_("Sf")])
                    op("dve", lambda: V.memset(Sb_[:], 0.0), r=[], w=[K_("Sb_")])
                    order = list(range(0, NT + 1)) if d_ == 0 else list(range(NT, 0, -1))
                    mC, mA, mQ = masks[:, 0 + d_, :], masks[:, 2 + d_, :], masks[:, 4 + d_, :]
                    o_dst = of_s if d_ == 0 else ob_s
                    for it, t in enumerate(order):
                        n, p0 = (NM, 0) if t == 0 else (128, NM + (t - 1) * 128)
                        li = it % 2
                        qTl, kTl, vTl = B_(f"qTt{li}"), B_(f"kTt{li}"), B_(f"vTt{li}")
                        qn, kn, vn_ = K_(f"qTt{li}"), K_(f"kTt{li}"), K_(f"vTt{li}")
                        for (dst, src, nm) in ((qTl, qT_s, qn), (kTl, kT_s, kn), (vTl, vT_s, vn_)):
                            dma("sp", dst[:, :, 0:n], src[H0:H0 + 4, :, p0:p0 + n].rearrange("h d p -> d h p"), w=[nm])
                        yield
                        for (srcT, dstm, sn, dn) in ((kTl, ktm, kn, K_("ktm")), (vTl, vtm, vn_, K_("vtm"))):
                            for hi in range(4):
                                op("pe", lambda: PE.transpose(pYb[0:n, hi, 0:128], srcT[:, hi, 0:n], identb[:, :]), r=[sn, "identb"], w=[kY])
                            op("act", lambda: A.copy(out=dstm[0:n], in_=pYb[0:n, :, 0:128]), r=[kY], w=[dn])
                            yield
                        gcol = gball[0:n, t, d_, 0, H0:H0 + 4]
                        bcol = gball[0:n, t, d_, 1, H0:H0 + 4]
                        op("dve", lambda: V.tensor_tensor(out=Gm[0:n, :, 0:n], in0=mC[0:n, 0:n].unsqueeze(1).to_broadcast([n, 4, n]),
                                                          in1=gcol.unsqueeze(2).to_broadcast([n, 4, n]), op=ALU.mult), r=["masks", "gball"], w=[K_("Gm")])
                        op("pe", lambda: PE.matmul(pX[0:n, 0, 0:4], lhsT=mC[0:n, 0:n], rhs=gcol, start=True, stop=True), r=["masks", "gball"], w=[kX])
                        op("dve", lambda: V.tensor_copy(out=gcc[0:n], in_=pX[0:n, 0, 0:4]), r=[kX], w=[K_("gcc")])
                        op("act", lambda: A.activation(out=egc[0:n], in_=gcc[0:n], func=AF.Exp), r=[K_("gcc")], w=[K_("egc")])
                        op("dve", lambda: V.tensor_tensor(out=bgc[0:n], in0=egc[0:n], in1=bcol, op=ALU.mult), r=[K_("egc"), "gball"], w=[K_("bgc")])
                        yield
                        if t == 0:
                            chunks, ends = [(0, NM)], [NM - 1]
                        elif d_ == 0:
                            chunks, ends = [(0, 64), (64, 64)], [63, 127]
                        else:
                            chunks, ends = [(64, 64), (0, 64)], [64, 0]
                        if n == 128:
                            op("pe", lambda: PE.matmul(pX[:, :, 0:n], lhsT=onesf[0:n, :], rhs=Gm[0:n, :, 0:n], start=True, stop=True), r=["onesf", K_("Gm")], w=[kX])
                        else:
                            for hi in range(4):
                                op("pe", lambda: PE.matmul(pX[:, hi, 0:n], lhsT=onesf[0:n, :], rhs=Gm[0:n, hi, 0:n], start=True, stop=True), r=["onesf", K_("Gm")], w=[kX])
                        for hi in range(4):
                            op("pe", lambda: PE.matmul(pY[0:n, hi, 0:n], lhsT=kTl[:, hi, 0:n], rhs=kTl[:, hi, 0:n], start=True, stop=True), r=[kn], w=[kY])
                        op("dve", lambda: V.tensor_tensor(out=diff[0:n, :, 0:n], in0=gcc[0:n, :].unsqueeze(2).to_broadcast([n, 4, n]),
                                                          in1=pX[0:n, :, 0:n], op=ALU.subtract), r=[K_("gcc"), kX], w=[K_("diff")])
                        op("act", lambda: A.activation(out=egb[:, :, 0:n], in_=pX[:, :, 0:n], func=AF.Exp), r=[kX], w=[K_("egb")])
                        for ci, (r0, cn) in enumerate(chunks):
                            op("dve", lambda: V.tensor_copy(out=gend[r0:r0 + cn, :], in_=pX[r0:r0 + cn, :, ends[ci]]), r=[kX], w=[K_("gend")])
                        yield
                        op("dve", lambda: V.scalar_tensor_tensor(out=DL[0:n, :, 0:n], in0=diff[0:n, :, 0:n], scalar=0.0,
                                                                 in1=mA[0:n, 0:n].unsqueeze(1).to_broadcast([n, 4, n]), op0=ALU.min, op1=ALU.add),
                           r=[K_("diff"), "masks"], w=[K_("DL")])
                        op("dve", lambda: V.scalar_tensor_tensor(out=DU[0:n, :, 0:n], in0=diff[0:n, :, 0:n], scalar=0.0,
                                                                 in1=mQ[0:n, 0:n].unsqueeze(1).to_broadcast([n, 4, n]), op0=ALU.max, op1=ALU.add),
                           r=[K_("diff"), "masks"], w=[K_("DU")])
                        op("act", lambda: A.activation(out=DL[0:n, :, 0:n], in_=DL[0:n, :, 0:n], func=AF.Exp), r=[K_("DL")], w=[K_("DL")])
                        op("act", lambda: A.activation(out=DU[0:n, :, 0:n], in_=DU[0:n, :, 0:n], func=AF.Exp, scale=-1.0), r=[K_("DU")], w=[K_("DU")])
                        for hi in range(4):
                            op("pe", lambda: PE.matmul(pX[0:n, hi, 0:n], lhsT=kTl[:, hi, 0:n], rhs=qTl[:, hi, 0:n], start=True, stop=True), r=[kn, qn], w=[kX])
                        op("dve", lambda: V.tensor_tensor(out=kds[0:n, :], in0=gend[0:n, :], in1=gcc[0:n, :], op=ALU.subtract), r=[K_("gend"), K_("gcc")], w=[K_("kds")])
                        op("act", lambda: A.activation(out=kds[0:n, :], in_=kds[0:n, :], func=AF.Exp), r=[K_("kds")], w=[K_("kds")])
                        yield
                        op("dve", lambda: V.tensor_tensor(out=diff[0:n, :, 0:n], in0=pY[0:n, :, 0:n], in1=DL[0:n, :, 0:n], op=ALU.mult), r=[kY, K_("DL")], w=[K_("diff")])
                        op(ge, lambda: GE.tensor_tensor(out=Ab[0:n, :, 0:n], in0=diff[0:n, :, 0:n], in1=bcol.unsqueeze(2).to_broadcast([n, 4, n]), op=ALU.mult),
                           r=[K_("diff"), "gball"], w=[K_("Ab")])
                        op("dve", lambda: V.tensor_tensor(out=QKT[0:n, :, 0:n], in0=pX[0:n, :, 0:n], in1=DU[0:n, :, 0:n], op=ALU.mult), r=[kX, K_("DU")], w=[K_("QKT")])
                        yield
                        for hi in range(4):
                            op("pe", lambda: PE.transpose(pYb[0:n, hi, 0:n], Ab[0:n, hi, 0:n], identb[0:n, 0:n]), r=[K_("Ab"), "identb"], w=[kY])
                        op("act", lambda: A.copy(out=ATb[0:n, :, 0:n], in_=pYb[0:n, :, 0:n]), r=[kY], w=[K_("ATb")])
                        yield
                        op("dve", lambda: V.tensor_tensor(out=TTb[0:n, :, 0:n], in0=identf[0:n, 0:n].unsqueeze(1).to_broadcast([n, 4, n]), in1=ATb[0:n, :, 0:n], op=ALU.subtract),
                           r=["identf", K_("ATb")], w=[K_("TTb")])
                        yield
                        Pc, PTc, Pn_, PTn_ = Ab, ATb, K_("Ab"), K_("ATb")
                        for lvl in range(1, 6):
                            Pd, PTd = B_(f"Pb{lvl % 2}"), B_(f"PTb{lvl % 2}")
                            Pdn, PTdn = K_(f"Pb{lvl % 2}"), K_(f"PTb{lvl % 2}")
                            for hi in range(4):
                                op("pe", lambda: PE.matmul(pX[0:n, hi, 0:n], lhsT=PTc[0:n, hi, 0:n], rhs=Pc[0:n, hi, 0:n], start=True, stop=True), r=[Pn_, PTn_], w=[kX])
                            if lvl < 5:
                                for hi in range(4):
                                    op("pe", lambda: PE.matmul(pY[0:n, hi, 0:n], lhsT=Pc[0:n, hi, 0:n], rhs=PTc[0:n, hi, 0:n], start=True, stop=True), r=[Pn_, PTn_], w=[kY])
                            op("act", lambda: A.copy(out=Pd[0:n, :, 0:n], in_=pX[0:n, :, 0:n]), r=[kX], w=[Pdn])
                            if lvl < 5:
                                op("act", lambda: A.copy(out=PTd[0:n, :, 0:n], in_=pY[0:n, :, 0:n]), r=[kY], w=[PTdn])
                            yield
                            for hi in range(4):
                                op("pe", lambda: PE.matmul(pX[0:n, hi, 0:n], lhsT=Pd[0:n, hi, 0:n], rhs=TTb[0:n, hi, 0:n], start=True, stop=True), r=[Pdn, K_("TTb")], w=[kX])
                            op("dve", lambda: V.tensor_tensor(out=TTb[0:n, :, 0:n], in0=TTb[0:n, :, 0:n], in1=pX[0:n, :, 0:n], op=ALU.add), r=[K_("TTb"), kX], w=[K_("TTb")])
                            yield
                            Pc, PTc, Pn_, PTn_ = Pd, PTd, Pdn, PTdn
                        op(ge, lambda: GE.tensor_tensor(out=vbt[0:n], in0=vtm[0:n], in1=bcol.unsqueeze(2).to_broadcast([n, 4, 128]), op=ALU.mult), r=[K_("vtm"), "gball"], w=[K_("vbt")])
                        op(ge, lambda: GE.tensor_tensor(out=kbg[0:n], in0=ktm[0:n], in1=bgc[0:n, :].unsqueeze(2).to_broadcast([n, 4, 128]), op=ALU.mult), r=[K_("ktm"), K_("bgc")], w=[K_("kbg")])
                        op(ge, lambda: GE.tensor_tensor(out=kdec[0:n], in0=ktm[0:n], in1=kds[0:n, :].unsqueeze(2).to_broadcast([n, 4, 128]), op=ALU.mult), r=[K_("ktm"), K_("kds")], w=[K_("kdec")])
                        op(ge, lambda: GE.tensor_tensor(out=qdT[:, :, 0:n], in0=qTl[:, :, 0:n], in1=egb[:, :, 0:n], op=ALU.mult), r=[qn, K_("egb")], w=[K_("qdT")])
                        yield
                        for hi in range(4):
                            op("pe", lambda: PE.matmul(pX[0:n, hi, :], lhsT=TTb[0:n, hi, 0:n], rhs=vbt[0:n, hi, :], start=True, stop=True), r=[K_("TTb"), K_("vbt")], w=[kX])
                        for hi in range(4):
                            op("pe", lambda: PE.matmul(pY[:, hi, 0:n], lhsT=kbg[0:n, hi, :], rhs=TTb[0:n, hi, 0:n], start=True, stop=True), r=[K_("TTb"), K_("kbg")], w=[kY])
                        op("act", lambda: A.copy(out=u_sb[0:n], in_=pX[0:n]), r=[kX], w=[K_("u_sb")])
                        op("act", lambda: A.mul(out=nwT[:, :, 0:n], in_=pY[:, :, 0:n], mul=-1.0), r=[kY], w=[K_("nwT")])
                        yield
                        for ci, (r0, cn) in enumerate(chunks):
                            rs_ = slice(r0, r0 + cn)
                            for hi in range(4):
                                op("pe", lambda: PE.matmul(pX[rs_, hi, :], lhsT=nwT[:, hi, rs_], rhs=Sb_[:, hi, :], start=True, stop=True), r=[K_("nwT"), K_("Sb_")], w=[kX])
                            op("dve", lambda: V.tensor_tensor(out=vnew[rs_], in0=u_sb[rs_], in1=pX[rs_], op=ALU.add), r=[K_("u_sb"), kX], w=[K_("vnew")])
                            yield
                            if t > 0:
                                for hi in range(4):
                                    op("pe", lambda: PE.matmul(pY[:, hi, 0:cn], lhsT=Sb_[:, hi, :], rhs=qdT[:, hi, rs_], start=True, stop=False), r=[K_("Sb_"), K_("qdT")], w=[kY])
                                    op("pe", lambda: PE.matmul(pY[:, hi, 0:cn], lhsT=vnew[rs_, hi, :], rhs=QKT[rs_, hi, rs_], start=False, stop=True), r=[K_("vnew"), K_("QKT")], w=[kY])
                                op("act", lambda: A.copy(out=oTt[:, :, rs_], in_=pY[:, :, 0:cn]), r=[kY], w=[K_("oTt")])
                            for hi in range(4):
                                op("pe", lambda: PE.matmul(pX[:, hi, :], lhsT=kdec[rs_, hi, :], rhs=vnew[rs_, hi, :], start=True, stop=True), r=[K_("kdec"), K_("vnew")], w=[kX])
                            op("dve", lambda: V.tensor_tensor(out=Sf[:], in0=Sf[:], in1=egb[:, :, ends[ci]].unsqueeze(2).to_broadcast([128, 4, 128]), op=ALU.mult),
                               r=[K_("Sf"), K_("egb")], w=[K_("Sf")])
                            op("dve", lambda: V.tensor_tensor(out=Sf[:], in0=Sf[:], in1=pX[:], op=ALU.add), r=[K_("Sf"), kX], w=[K_("Sf")])
                            op("act", lambda: A.copy(out=Sb_[:], in_=Sf[:]), r=[K_("Sf")], w=[K_("Sb_")])
                            yield
                        if t == 0:
                            continue
                        c0 = (t - 1) * 128
                        dma("sp", o_dst[t - 1][:, H0:H0 + 4, :], oTt[:], r=[K_("oTt")], w=[("osc", d_, hg, t)], key=K_("oTt"))
                        yield

                with contextlib.ExitStack() as st1:
                    pXs = [ps(f"p4X{i}", [128, 4, 128], F32, st1) for i in range(NSTR)]
                    pYs = [ps(f"p4Y{i}", [128, 4, 128], F32, st1) for i in range(NSTR)]
                    gens = [p4_stream(i, pXs[i], pYs[i]) for i in range(NSTR)]
                    alive = [True] * NSTR
                    nstep = 0
                    while any(alive):
                        for i in range(NSTR):
                            if alive[i]:
                                try:
                                    next(gens[i])
                                except StopIteration:
                                    alive[i] = False
                                nstep += 1
                                if nstep % 18 == 0:
                                    pc_step(1)
                    S.barrier()
            F8 = lambda nm, dt=F32: sb(nm, [128, 8, 128], dt, st)
            ofl = [F8(f"ofl{i}") for i in range(2)]
            obl = [F8(f"obl{i}") for i in range(2)]
            osq2 = [F8(f"osq{i}") for i in range(2)]
            ors2 = [F8(f"ors{i}") for i in range(2)]
            zall = sb("zall", [128, 8, SEQ], BF16, st)
            for h in range(8):
                dma("sp", zall[:, h, :], z_s[h], w=["zall"], key="zall")
            with contextlib.ExitStack() as st1:
                pss = [ps(f"pss{i}", [128, 4, 128], F32, st1) for i in range(4)]
                for t in (range(1, NT + 1) if os.environ.get('P4COMB', '1') == '1' else []):
                    c0 = (t - 1) * 128
                    i = t % 2
                    osq, ors, kosq, kors = osq2[i], ors2[i], f"osq{i}", f"ors{i}"
                    dma("sp", ofl[i][:], of_s[t - 1], r=[("osc", 0, 0, t), ("osc", 0, 1, t)], w=[f"ofl{i}"])
                    dma("sp", obl[i][:], ob_s[t - 1], r=[("osc", 1, 0, t), ("osc", 1, 1, t)], w=[f"obl{i}"])
                    op("dve", lambda: V.tensor_tensor(out=ofl[i][:], in0=ofl[i][:], in1=obl[i][:], op=ALU.add), r=[f"ofl{i}", f"obl{i}"], w=[f"ofl{i}"])
                    op("act", lambda: A.activation(out=osq[:], in_=ofl[i][:], func=AF.Square), r=[f"ofl{i}"], w=[kosq])
                    for hg in range(2):
                        pi = (2 * t + hg) % 4
                        op("pe", lambda: PE.matmul(pss[pi][:], lhsT=onesf[:], rhs=osq[:, hg * 4:hg * 4 + 4, :], start=True, stop=True), r=["onesf", kosq], w=[f"pss{pi}"])
                        op("act", lambda: A.activation(out=ors[:, hg * 4:hg * 4 + 4, :], in_=pss[pi][:], func=AF.Sqrt, scale=1.0 / 128, bias=epst[:]), r=[f"pss{pi}", "epst"], w=[kors])
                    op("dve", lambda: V.reciprocal(out=ors[:], in_=ors[:]), r=[kors], w=[kors])
                    op("dve", lambda: V.scalar_tensor_tensor(out=ofl[i][:], in0=ofl[i][:], scalar=gout[:, 0:1], in1=ors[:], op0=ALU.mult, op1=ALU.mult),
                       r=[f"ofl{i}", "gout", kors], w=[f"ofl{i}"])
                    op("pool", lambda: G.tensor_tensor(out=ogT[:, :, c0:c0 + 128], in0=ofl[i][:], in1=zall[:, :, c0:c0 + 128], op=ALU.mult), r=[f"ofl{i}", "zall"], w=[("ogT", t)])
                S.barrier()
            if debug:
                og_d = scratch("og_d", [NH, 128, SEQ], BF16)
                for h in range(8):
                    dma("sp", og_d[h], ogT[:, h, :], r=[("ogT", t) for t in range(1, NT + 1)], key="dbg_og")
            S.barrier()
        if stage < 4:
            S.finish()
            return nc, dbg
        h2_s = scratch("h2_s", [SEQ, D], F32)
        hn2_s = scratch("hn2_s", [SEQ, D], BF16)
        IOA = bass.IndirectOffsetOnAxis
        call = sb("call", [128, NT, 2], F32)
        dall = sb("dall", [128, NT, 2], I32)
        NB = 3
        wgu = [sb(f"wgu{i}", [128, 8, 512], BF16) for i in range(NB)]
        wde = [sb(f"wde{i}", [128, 2, D], BF16) for i in range(NB)]

        PRECAST = os.environ.get("PRECAST", "1") == "1"

        def load_expert(e):
            wi = e % NB
            if PRECAST:
                rk_ = [("wq", e // 2)]
                dma("pool", wgu[wi][:, :, 0:256], wq_g[e].rearrange("p (kt j) -> p kt j", kt=8), r=rk_, w=[f"wgu{wi}"], key=f"wgu{wi}")
                dma("pool", wgu[wi][:, :, 256:512], wq_u[e].rearrange("p (kt j) -> p kt j", kt=8), r=rk_, w=[f"wgu{wi}"], key=f"wgu{wi}")
                dma("pool", wde[wi][:], wq_d[e].rearrange("p (jt d) -> p jt d", jt=2), r=rk_, w=[f"wde{wi}"])
                return
            dma("pool", wgu[wi][:, :, 0:256], w_gate_e[0, e].rearrange("(p kt) j -> p kt j", kt=8), w=[f"wgu{wi}"], key=f"wgu{wi}")
            dma("pool", wgu[wi][:, :, 256:512], w_up_e[0, e].rearrange("(p kt) j -> p kt j", kt=8), w=[f"wgu{wi}"], key=f"wgu{wi}")
            dma("pool", wde[wi][:], w_down_e[0, e].rearrange("(jt p) d -> p jt d", p=128), w=[f"wde{wi}"])

        pc_step(1000)
        if stage >= 5:
            for e in range(NB):
                load_expert(e)
        with contextlib.ExitStack() as st:
            mrgT = sb("mrgT2", [128, 8, SEQ], BF16, st)
            for dt_ in range(8):
                dma("sp", mrgT[:, dt_, :], mrg_s[dt_], r=["mrg_s"], w=[("mrgT", dt_)], key=f"mrgT2_{dt_}")
            wd = sb("wd", [128, 8, D], BF16, st)
            wo = sb("wo", [128, 8, D], BF16, st)
            dma("pool", wd[:], w_delta[0].rearrange("(h p) d -> p h d", p=128), w=["wd"])
            dma("pool", wo[:], w_out[0].rearrange("(h p) d -> p h d", p=128), w=["wo"])
            sgd = [sb(f"sgd{i}", [128, SEQ], BF16, st) for i in range(2)]
            tmpm = sb("tmpm", [128, 512], F32, st)
            g2b = sb("g2b", [128, D], F32, st)
            dma("sp", g2b[:], norm2_g[0].partition_broadcast(128), w=["g2b"])
            wr = sb("wr", [128, 8, 72], F32, st)
            with nc.allow_non_contiguous_dma(reason="small router weights"):
                dma("sp", wr[:, :, 0:8], w_rg[0].rearrange("(kt p) g -> p kt g", p=128), w=["wr"], key="wr")
                dma("sp", wr[:, :, 8:72], w_re[0].rearrange("(kt p) g -> p kt g", p=128), w=["wr"], key="wr")
            rbias = sb("rbias", [128, 72], F32, st)
            dma("sp", rbias[:, 0:8], b_rg[0].partition_broadcast(128), w=["rbias"], key="rbias")
            dma("sp", rbias[:, 8:72], b_re[0].partition_broadcast(128), w=["rbias"], key="rbias")
            ustf = sb("ustf", [128, 128], F32, st)
            dma("sp", ustf[:], c_masks[6], w=["ustf"])
            ustb = sb("ustb", [128, 128], BF16, st)
            op("act", lambda: A.copy(out=ustb[:], in_=ustf[:]), r=["ustf"], w=["ustb"])
            onesb2 = sb("onesb2", [128, 128], BF16, st)
            op("dve", lambda: V.memset(onesb2[:], 1.0), w=["onesb2"])
            ecap = sb("ecap", [128, 64], F32, st)
            dma("sp", ecap[:], c_ecap, w=["ecap"])
            Mall = sb("Mall", [128, NT, 64], BF16, st)
            lgall = sb("lgall", [128, NT, 72], F32, st)
            sm = {"ss": sb("sm_ss", [128, 1], F32, st)}
            stA = contextlib.ExitStack()
            xr = [sb(f"xr{i}", [128, D], F32, stA) for i in range(2)]
            h2t = [sb(f"h2t{i}", [128, D], F32, stA) for i in range(2)]
            hn2f2 = [sb(f"hn2f{i}", [128, D], F32, stA) for i in range(2)]
            hn2b = [sb(f"hn2b{i}", [128, D], BF16, stA) for i in range(2)]
            junk2 = sb("junk2", [128, D], BF16, stA)
            hn2T = sb("hn2T", [128, 8, 128], F32, stA)
            with contextlib.ExitStack() as st1:
                pa = [ps(f"pb{i}", [128, 512], F32, st1) for i in range(3)]
                ptf = [ps(f"ptf{i}", [128, 4, 128], F32, st1) for i in range(2)]
                plg = ps("plg", [128, 72], F32, st1)
                prk2 = [ps(f"prk{i}", [128, 8, 64], F32, st1) for i in range(2)]
                npa = 0
                for dt_ in range(8):
                    gi_ = dt_ % 2
                    dma("sp", sgd[gi_][:], gd_s[dt_], w=[f"sgd{gi_}"])
                    for bi in range(4):
                        ai = npa % 3
                        npa += 1
                        for h in range(8):
                            op("pe", lambda: PE.matmul(pa[ai][:, :], lhsT=wd[:, h, dt_ * 128:(dt_ + 1) * 128], rhs=ogT[:, h, bi * 512:(bi + 1) * 512],
                                                       start=(h == 0), stop=(h == 7)),
                               r=["wd"] + [("ogT", t) for t in range(1, NT + 1)], w=[f"pb{ai}"])
                        op("dve", lambda: V.tensor_tensor(out=tmpm[:], in0=pa[ai][:, :], in1=sgd[gi_][:, bi * 512:(bi + 1) * 512], op=ALU.mult),
                           r=[f"pb{ai}", f"sgd{gi_}"], w=["tmpm"])
                        op("dve", lambda: V.tensor_tensor(out=mrgT[:, dt_, bi * 512:(bi + 1) * 512], in0=tmpm[:], in1=mrgT[:, dt_, bi * 512:(bi + 1) * 512], op=ALU.add),
                           r=["tmpm", ("mrgT", dt_)], w=[("mrgT", dt_)])
                mrg_all = [("mrgT", d2) for d2 in range(8)]
                def LA(t):
                    i = t % 2
                    hn2f = hn2f2[i]
                    nonlocal_npa = None
                    dma("sp", xr[i][:], x[t * 128:(t + 1) * 128, :], w=[f"xr{i}"])
                    for half in range(2):
                        ai = cntA[0] % 3
                        cntA[0] += 1
                        for dt_ in range(8):
                            op("pe", lambda: PE.matmul(pa[ai][:, :], lhsT=mrgT[:, dt_, t * 128:(t + 1) * 128], rhs=wo[:, dt_, half * 512:(half + 1) * 512],
                                                       start=(dt_ == 0), stop=(dt_ == 7)), r=["wo"] + mrg_all, w=[f"pb{ai}"])
                        op("dve", lambda: V.tensor_tensor(out=h2t[i][:, half * 512:(half + 1) * 512], in0=pa[ai][:, :], in1=xr[i][:, half * 512:(half + 1) * 512], op=ALU.add),
                           r=[f"pb{ai}", f"xr{i}"], w=[f"h2t{i}"])
                    dma("sp", h2_s[t * 128:(t + 1) * 128, :], h2t[i][:], r=[f"h2t{i}"], w=["h2_s"], key="h2_s")
                    op("act", lambda: A.activation(out=junk2[:], in_=h2t[i][:], func=AF.Square, accum_out=sm["ss"][:]), r=[f"h2t{i}"], w=["junk2", "sm_ss"])
                    op("act", lambda: A.activation(out=sm["ss"][:], in_=sm["ss"][:], func=AF.Sqrt, scale=1.0 / D, bias=epst[:]), r=["sm_ss", "epst"], w=["sm_ss"])
                    op("dve", lambda: V.reciprocal(out=sm["ss"][:], in_=sm["ss"][:]), r=["sm_ss"], w=["sm_ss"])
                    op("dve", lambda: V.scalar_tensor_tensor(out=hn2f[:], in0=h2t[i][:], scalar=sm["ss"][:, 0:1], in1=g2b[:], op0=ALU.mult, op1=ALU.mult),
                       r=[f"h2t{i}", "sm_ss", "g2b"], w=[f"hn2f{i}"])
                    op("act", lambda: A.copy(out=hn2b[i][:], in_=hn2f[:]), r=[f"hn2f{i}"], w=[f"hn2b{i}"])
                    dma("sp", hn2_s[t * 128:(t + 1) * 128, :], hn2b[i][:], r=[f"hn2b{i}"], w=["hn2_s"], key=f"hn2b{i}")

                def LB(t):
                    i = t % 2
                    hn2f = hn2f2[i]
                    for kt in range(8):
                        op("pe", lambda: PE.transpose(ptf[kt // 4][:, kt % 4, :], hn2f[:, kt * 128:(kt + 1) * 128], identf[:]), r=[f"hn2f{i}", "identf"], w=[f"ptf{kt // 4}"])
                    for q_ in range(2):
                        op("act", lambda: A.copy(out=hn2T[:, q_ * 4:q_ * 4 + 4, :], in_=ptf[q_][:]), r=[f"ptf{q_}"], w=["hn2T"])
                    for kt in range(8):
                        op("pe", lambda: PE.matmul(plg[:, :], lhsT=hn2T[:, kt, :], rhs=wr[:, kt, :], start=(kt == 0), stop=(kt == 7)), r=["hn2T", "wr"], w=["plg"])
                    op("dve", lambda: V.tensor_tensor(out=lgall[:, t, :], in0=plg[:, :], in1=rbias[:], op=ALU.add), r=["plg", "rbias"], w=["lgall"])

                cntA = [npa]
                for t in range(NT + 1):
                    if t < NT:
                        LA(t)
                    if t >= 1:
                        LB(t - 1)
                S.barrier()
                stA.close()
                hn2b = [sb(f"hn2c{i}", [128, D], BF16, st) for i in range(2)]
                TT_ = lambda o, a, b, o_: V.tensor_tensor(out=o, in0=a, in1=b, op=o_)
                R = {nm: sb("rt_" + nm, [128, NT, w_], F32, st) for nm, w_ in
                     (("gmax", 1), ("ge", 8), ("gsum", 1), ("pg", 1), ("ohg", 8), ("tmp", 64), ("elg", 8), ("m1", 1), ("oh1", 8), ("el2", 8),
                      ("m2", 1), ("oh2", 8), ("d12", 1), ("w1", 1), ("M1", 64), ("M2", 64), ("rk", 64), ("t3", 64), ("d1f", 1), ("d2f", 1))}
                k = lambda nm: "rt_" + nm
                lgg = lgall[:, :, 0:8]
                op("dve", lambda: V.tensor_reduce(out=R["gmax"][:, :, 0], in_=lgg, axis=AX.X, op=ALU.max), r=["lgall"], w=[k("gmax")])
                op("dve", lambda: TT_(R["ge"][:], lgg, R["gmax"][:].to_broadcast([128, NT, 8]), ALU.subtract), r=["lgall", k("gmax")], w=[k("ge")])
                op("act", lambda: A.activation(out=R["ge"][:], in_=R["ge"][:], func=AF.Exp), r=[k("ge")], w=[k("ge")])
                op("dve", lambda: V.tensor_reduce(out=R["gsum"][:, :, 0], in_=R["ge"][:], axis=AX.X, op=ALU.add), r=[k("ge")], w=[k("gsum")])
                op("dve", lambda: V.reciprocal(out=R["pg"][:], in_=R["gsum"][:]), r=[k("gsum")], w=[k("pg")])
                op("dve", lambda: TT_(R["ohg"][:], lgg, R["gmax"][:].to_broadcast([128, NT, 8]), ALU.is_equal), r=["lgall", k("gmax")], w=[k("ohg")])
                el4 = lgall[:, :, 8:72].rearrange("p t (g e) -> p t g e", g=8)
                tmp4 = R["tmp"][:].rearrange("p t (g e) -> p t g e", g=8)
                op("dve", lambda: TT_(tmp4, el4, R["ohg"][:].unsqueeze(3).to_broadcast([128, NT, 8, 8]), ALU.mult), r=["lgall", k("ohg")], w=[k("tmp")])
                op("dve", lambda: V.tensor_reduce(out=R["elg"][:], in_=tmp4.rearrange("p t g e -> p t e g"), axis=AX.X, op=ALU.add), r=[k("tmp")], w=[k("elg")])
                op("dve", lambda: V.tensor_reduce(out=R["m1"][:, :, 0], in_=R["elg"][:], axis=AX.X, op=ALU.max), r=[k("elg")], w=[k("m1")])
                op("dve", lambda: TT_(R["oh1"][:], R["elg"][:], R["m1"][:].to_broadcast([128, NT, 8]), ALU.is_equal), r=[k("elg"), k("m1")], w=[k("oh1")])
                op("dve", lambda: V.scalar_tensor_tensor(out=R["el2"][:], in0=R["oh1"][:], scalar=-1.0e30, in1=R["elg"][:], op0=ALU.mult, op1=ALU.add),
                   r=[k("oh1"), k("elg")], w=[k("el2")])
                op("dve", lambda: V.tensor_reduce(out=R["m2"][:, :, 0], in_=R["el2"][:], axis=AX.X, op=ALU.max), r=[k("el2")], w=[k("m2")])
                op("dve", lambda: TT_(R["oh2"][:], R["el2"][:], R["m2"][:].to_broadcast([128, NT, 8]), ALU.is_equal), r=[k("el2"), k("m2")], w=[k("oh2")])
                op("dve", lambda: TT_(R["d12"][:], R["m1"][:], R["m2"][:], ALU.subtract), r=[k("m1"), k("m2")], w=[k("d12")])
                op("act", lambda: A.activation(out=R["w1"][:], in_=R["d12"][:], func=AF.Sigmoid), r=[k("d12")], w=[k("w1")])
                op("dve", lambda: TT_(call[:, :, 0:1], R["pg"][:], R["w1"][:], ALU.mult), r=[k("pg"), k("w1")], w=["call"])
                op("dve", lambda: TT_(call[:, :, 1:2], R["pg"][:], call[:, :, 0:1], ALU.subtract), r=[k("pg"), "call"], w=["call"])
                for (Mn, ohn) in (("M1", "oh1"), ("M2", "oh2")):
                    op("dve", lambda: TT_(R[Mn][:].rearrange("p t (g e) -> p t g e", g=8), R["ohg"][:].unsqueeze(3).to_broadcast([128, NT, 8, 8]),
                                          R[ohn][:].unsqueeze(2).to_broadcast([128, NT, 8, 8]), ALU.mult), r=[k("ohg"), k(ohn)], w=[k(Mn)])
                op("dve", lambda: TT_(Mall[:], R["M1"][:], R["M2"][:], ALU.add), r=[k("M1"), k("M2")], w=["Mall"])
                for t in range(NT):
                    pr = prk2[t // 8]
                    prn = f"prk{t // 8}"
                    op("pe", lambda: PE.matmul(pr[:, t % 8, :], lhsT=ustb[:], rhs=Mall[:, t, :], start=True, stop=(t == 0)), r=["ustb", "Mall"], w=[prn])
                    for j in range(t):
                        op("pe", lambda: PE.matmul(pr[:, t % 8, :], lhsT=onesb2[:], rhs=Mall[:, j, :], start=False, stop=(j == t - 1)), r=["onesb2", "Mall"], w=[prn])
                for q_ in range(2):
                    op("dve", lambda: TT_(R["rk"][:, q_ * 8:q_ * 8 + 8, :], prk2[q_][:], ecap[:].unsqueeze(1).to_broadcast([128, 8, 64]), ALU.add), r=[f"prk{q_}", "ecap"], w=[k("rk")])
                for (Mn, dn, ci_) in (("M1", "d1f", 0), ("M2", "d2f", 1)):
                    op("dve", lambda: TT_(R["t3"][:], R["rk"][:], R[Mn][:], ALU.mult), r=[k("rk"), k(Mn)], w=[k("t3")])
                    op("dve", lambda: V.tensor_reduce(out=R[dn][:, :, 0], in_=R["t3"][:], axis=AX.X, op=ALU.add), r=[k("t3")], w=[k(dn)])
                    op("dve", lambda: V.tensor_copy(out=dall[:, :, ci_:ci_ + 1], in_=R[dn][:]), r=[k(dn)], w=["dall"])
                for t in range(NT):
                    i = t % 2
                    dma("sp", hn2b[i][:], hn2_s[t * 128:(t + 1) * 128, :], r=["hn2_s"], w=[f"hn2c{i}"])
                    for ci_ in range(2):
                        dma("pool", None, None, r=[f"hn2c{i}", "dall"], w=["Xs"], key="Xs",
                            indirect=lambda: G.indirect_dma_start(out=Xs[:, :], out_offset=IOA(ap=dall[:, t, ci_:ci_ + 1], axis=0), in_=hn2b[i][:], in_offset=None))
                S.barrier()
            S.barrier()
        if stage < 5:
            S.finish()
            return nc, dbg
        with contextlib.ExitStack() as st:
            Xe = [sb(f"Xe{i}", [128, D], BF16, st) for i in range(2)]
            XeT2 = [sb(f"XeT{i}", [128, 8, 128], BF16, st) for i in range(2)]
            sg2 = [sb(f"sg{i}", [128, 256], F32, st) for i in range(2)]
            actb2 = [sb(f"actb{i}", [128, 256], BF16, st) for i in range(2)]
            actT2 = [sb(f"actT{i}", [128, 2, 128], BF16, st) for i in range(2)]
            Ye = [sb(f"Ye{i}", [128, D], F32, st) for i in range(2)]
            gfb = sb("gfb", [128, D], F32, st)
            dma("sp", gfb[:], final_g.partition_broadcast(128), w=["gfb"])
            ya2 = [sb(f"ya{i}", [128, D], F32, st) for i in range(4)]
            yb2 = [sb(f"yb{i}", [128, D], F32, st) for i in range(4)]
            hh = [sb(f"hh{i}", [128, D], F32, st) for i in range(2)]
            oo = [sb(f"oo{i}", [128, D], F32, st) for i in range(2)]
            junk3 = sb("junk3", [128, D], BF16, st)
            ssf = sb("ssf", [128, 1], F32, st)
            with contextlib.ExitStack() as st1:
                ptx = ps("ptx", [128, 8, 128], BF16, st1)
                pta = ps("pta", [128, 8, 128], BF16, st1)
                ph = [ps(f"ph{i}", [128, 512], F32, st1) for i in range(2)]
                pyy = [ps(f"pyy{i}", [128, 512], F32, st1) for i in range(2)]
                def expA(e):
                    wi, xi, pi = e % NB, e % 2, e % 2
                    if e == 0:
                        dma("sp", Xe[0][:], Xs[0:CAP, :], r=["Xs"], w=["Xe0"])
                    if e + 1 < NEXP:
                        dma("sp", Xe[1 - xi][:], Xs[(e + 1) * CAP:(e + 2) * CAP, :], r=["Xs"], w=[f"Xe{1 - xi}"])
                    for kt in range(8):
                        op("pe", lambda: PE.transpose(ptx[:, kt, :], Xe[xi][:].rearrange("s (p k) -> s k p", k=8)[:, kt, :], identb[:]), r=[f"Xe{xi}", "identb"], w=["ptx"])
                    op("act", lambda: A.copy(out=XeT2[xi][:], in_=ptx[:]), r=["ptx"], w=[f"XeT{xi}"])
                    for kt in range(8):
                        op("pe", lambda: PE.matmul(ph[pi][:, :], lhsT=XeT2[xi][:, kt, :], rhs=wgu[wi][:, kt, :], start=(kt == 0), stop=(kt == 7)), r=[f"XeT{xi}", f"wgu{wi}"], w=[f"ph{pi}"])

                def expB(e):
                    wi, xi, pi = e % NB, e % 2, e % 2
                    op("act", lambda: A.activation(out=sg2[xi][:], in_=ph[pi][:, 0:256], func=AF.Silu), r=[f"ph{pi}"], w=[f"sg{xi}"])
                    op("dve", lambda: V.tensor_tensor(out=actb2[xi][:], in0=sg2[xi][:], in1=ph[pi][:, 256:512], op=ALU.mult), r=[f"sg{xi}", f"ph{pi}"], w=[f"actb{xi}"])
                    for jt in range(2):
                        op("pe", lambda: PE.transpose(pta[:, jt, :], actb2[xi][:, jt * 128:(jt + 1) * 128], identb[:]), r=[f"actb{xi}", "identb"], w=["pta"])
                    op("act", lambda: A.copy(out=actT2[xi][:], in_=pta[:, 0:2, :]), r=["pta"], w=[f"actT{xi}"])
                    for half in range(2):
                        for jt in range(2):
                            op("pe", lambda: PE.matmul(pyy[half][:, :], lhsT=actT2[xi][:, jt, :], rhs=wde[wi][:, jt, half * 512:(half + 1) * 512], start=(jt == 0), stop=(jt == 1)),
                               r=[f"actT{xi}", f"wde{wi}"], w=[f"pyy{half}"])
                    op("act", lambda: A.copy(out=Ye[xi][:, 0:512], in_=pyy[0][:, :]), r=["pyy0"], w=[f"Ye{xi}"])
                    op("dve", lambda: V.tensor_copy(out=Ye[xi][:, 512:1024], in_=pyy[1][:, :]), r=["pyy1"], w=[f"Ye{xi}"])
                    dma("act", Ys[e * CAP:(e + 1) * CAP, :], Ye[xi][:], r=[f"Ye{xi}"], w=["Ys"], key="Ys")
                    if e + NB < NEXP:
                        load_expert(e + NB)

                for e in range(NEXP + 1):
                    if e < NEXP:
                        expA(e)
                    if e >= 1:
                        expB(e - 1)
                for t in range(NT):
                    i = t % 2
                    ya, yb, kya, kyb = ya2[t % 4], yb2[t % 4], f"ya{t % 4}", f"yb{t % 4}"
                    dma("sp", hh[i][:], h2_s[t * 128:(t + 1) * 128, :], r=["h2_s"], w=[f"hh{i}"])
                    dma("pool", None, None, r=["Ys", "dall"], w=[kya], key=kya,
                        indirect=lambda: G.indirect_dma_start(out=ya[:], out_offset=None, in_=Ys[:, :], in_offset=IOA(ap=dall[:, t, 0:1], axis=0)))
                    dma("pool", None, None, r=["Ys", "dall"], w=[kyb], key=kyb,
                        indirect=lambda: G.indirect_dma_start(out=yb[:], out_offset=None, in_=Ys[:, :], in_offset=IOA(ap=dall[:, t, 1:2], axis=0)))
                    op("dve", lambda: V.scalar_tensor_tensor(out=hh[i][:], in0=ya[:], scalar=call[:, t, 0:1], in1=hh[i][:], op0=ALU.mult, op1=ALU.add),
                       r=[kya, "call", f"hh{i}"], w=[f"hh{i}"])
                    op("dve", lambda: V.scalar_tensor_tensor(out=hh[i][:], in0=yb[:], scalar=call[:, t, 1:2], in1=hh[i][:], op0=ALU.mult, op1=ALU.add),
                       r=[kyb, "call", f"hh{i}"], w=[f"hh{i}"])
                    op("act", lambda: A.activation(out=junk3[:], in_=hh[i][:], func=AF.Square, accum_out=ssf[:]), r=[f"hh{i}"], w=["junk3", "ssf"])
                    op("act", lambda: A.activation(out=ssf[:], in_=ssf[:], func=AF.Sqrt, scale=1.0 / D, bias=epst[:]), r=["ssf", "epst"], w=["ssf"])
                    op("dve", lambda: V.reciprocal(out=ssf[:], in_=ssf[:]), r=["ssf"], w=["ssf"])
                    op("dve", lambda: V.scalar_tensor_tensor(out=oo[i][:], in0=hh[i][:], scalar=ssf[:, 0:1], in1=gfb[:], op0=ALU.mult, op1=ALU.mult),
                       r=[f"hh{i}", "ssf", "gfb"], w=[f"oo{i}"])
                    dma("sp", out[t * 128:(t + 1) * 128, :], oo[i][:], r=[f"oo{i}"], key=f"oo{i}")
                S.barrier()
            S.barrier()
        S.finish()
    return nc, dbg


def host_consts():
    c = {}
    c["c_identb"] = np.eye(128, dtype=np.float32).astype(ml_dtypes.bfloat16)
    c["c_identf"] = np.eye(128, dtype=np.float32)
    p = np.arange(L, dtype=np.float64)
    ang = 2.0 * np.pi * ((p[:, None] * p[None, :]) % L) / L
    c["c_cosL"] = np.cos(ang).astype(np.float32).astype(ml_dtypes.bfloat16)
    c["c_nsinL"] = (-np.sin(ang)).astype(np.float32).astype(ml_dtypes.bfloat16)
    q = np.arange(128, dtype=np.float64)
    a2 = 2.0 * np.pi * ((q[:, None] * q[None, :]) % 128) / 128
    sc = 1.0 / np.sqrt(L * 128.0)
    c["c_cs128"] = np.concatenate([np.cos(a2) * sc, np.sin(a2) * sc], axis=1).astype(np.float32).astype(ml_dtypes.bfloat16)
    c["c_masks"] = make_masks()
    c["c_ecap"] = np.tile((np.arange(64, dtype=np.float32) * CAP)[None, :], (128, 1))
    return c


def make_masks():
    i = np.arange(128)
    same = (i[:, None] // 64) == (i[None, :] // 64)
    m = np.zeros((7, 128, 128), np.float32)
    m[0] = (same & (i[:, None] <= i[None, :])).astype(np.float32)
    m[1] = (same & (i[:, None] >= i[None, :])).astype(np.float32)
    BIG = 30000.0
    m[2] = np.where(same & (i[None, :] < i[:, None]), 0.0, -BIG)
    m[3] = np.where(same & (i[None, :] > i[:, None]), 0.0, -BIG)
    m[4] = np.where(same & (i[:, None] <= i[None, :]), 0.0, BIG)
    m[5] = np.where(same & (i[:, None] >= i[None, :]), 0.0, BIG)
    m[6] = (i[:, None] < i[None, :]).astype(np.float32)
    return m


_CACHE = {}
PARAM_NAMES = ["meta_tokens", "norm1_g", "w_in", "conv_w", "a_log_fwd", "dt_bias_fwd", "a_log_bwd", "dt_bias_bwd",
               "out_norm_g", "w_fourier", "w_delta", "w_out", "norm2_g", "w_router_group", "b_router_group",
               "w_router_expert", "b_router_expert", "w_gate_e", "w_up_e", "w_down_e", "final_norm_g"]


def core_inputs(inputs, b):
    m = {"x": np.ascontiguousarray(np.asarray(inputs["x"])[b], dtype=np.float32)}
    for k in PARAM_NAMES:
        m[k] = np.ascontiguousarray(np.asarray(inputs[k]), dtype=np.float32)
    return m


def kernel(**inputs):
    if "nc" not in _CACHE:
        _CACHE["nc"] = build()[0]
        _CACHE["consts"] = host_consts()
    nc = _CACHE["nc"]
    maps = []
    for b in range(8):
        m = core_inputs(inputs, b)
        m.update(_CACHE["consts"])
        maps.append(m)
    res = run_bass_kernel_spmd(nc, maps, core_ids=list(range(8)))
    return np.stack([np.asarray(r["out"], dtype=np.float32) for r in res.results], axis=0)
```

```python
import contextlib
import os
import numpy as np
import ml_dtypes
import concourse.bass as bass
import concourse.mybir as mybir
from concourse.bass_utils import run_bass_kernel_spmd

F32 = mybir.dt.float32
BF16 = mybir.dt.bfloat16
I32 = mybir.dt.int32
AF = mybir.ActivationFunctionType
ALU = mybir.AluOpType
AX = mybir.AxisListType

D = 1024
NM = 16
SEQ = 2048
L = NM + SEQ
NH = 8
INW = 6688
EPS = 1e-6
NEXP = 64
CAP = 128
DE = 256
NT = SEQ // 128


class Sched:
    def __init__(self, nc):
        self.nc = nc
        self.eng = {"pe": nc.tensor, "act": nc.scalar, "dve": nc.vector, "pool": nc.gpsimd, "sp": nc.sync}
        self.sem = {e: nc.alloc_semaphore(f"s_{e}") for e in self.eng}
        self.cnt = {e: 0 for e in self.eng}
        self.seen = {e: {} for e in self.eng}
        self.semobj = {}
        self.bufs = {}
        self.nsem = 0
        self.excl = set()
        self.free_sems = []

    def _wait(self, e, ev):
        sem, val = ev
        if self.seen[e].get(sem.name, 0) >= val:
            return
        self.eng[e].wait_ge(sem, val)
        self.seen[e][sem.name] = val

    def deps(self, e, reads, writes):
        evs = []
        own = self.sem[e]
        for b in reads:
            st = self.bufs.get(b)
            if st and st["w"]:
                evs.append(st["w"])
            if st and b in self.excl:
                for r in st["r"]:
                    if r[0] is not own:
                        evs.append(r)
        for b in writes:
            st = self.bufs.get(b)
            if st:
                if st["w"]:
                    evs.append(st["w"])
                for r in st["r"]:
                    if r[0] is own:
                        continue
                    evs.append(r)
        best = {}
        for sem, val in evs:
            if e == "pe" and sem is own:
                continue
            if sem.name not in best or best[sem.name][1] < val:
                best[sem.name] = (sem, val)
        for ev in best.values():
            self._wait(e, ev)

    def record(self, ev, reads, writes):
        for b in reads:
            st = self.bufs.setdefault(b, {"w": None, "r": []})
            st["r"] = [r for r in st["r"] if r[0] is not ev[0]] + [ev]
        for b in writes:
            self.bufs[b] = {"w": ev, "r": []}

    def op(self, e, fn, r=(), w=()):
        self.deps(e, r, w)
        ins = fn()
        self.cnt[e] += 1
        ins.then_inc(self.sem[e], 1)
        ev = (self.sem[e], self.cnt[e])
        self.record(ev, r, w)
        return ev

    def dma(self, e, out, in_, r=(), w=(), key=None, indirect=None, **kw):
        self.deps(e, r, w)
        key = key or (w[0] if w else r[0])
        if key not in self.semobj:
            if self.free_sems:
                self.semobj[key] = self.free_sems.pop()
            else:
                self.semobj[key] = [self.nc.alloc_semaphore(f"d{self.nsem}"), 0]
                self.nsem += 1
        so = self.semobj[key]
        if indirect is None:
            ins = self.eng[e].dma_start(out=out, in_=in_, **kw)
        else:
            ins = indirect()
        so[1] += 16
        ins.then_inc(so[0], 16)
        ev = (so[0], so[1])
        self.record(ev, r, w)
        return ev

    def barrier(self):
        allev = {}
        for st in self.bufs.values():
            for ev in ([st["w"]] if st["w"] else []) + st["r"]:
                if ev[0].name not in allev or allev[ev[0].name][1] < ev[1]:
                    allev[ev[0].name] = ev
        for e in self.eng:
            for ev in allev.values():
                self._wait(e, ev)
        self.free_sems.extend(self.semobj.values())
        self.semobj = {}

    def finish(self):
        allev = {}
        for st in self.bufs.values():
            for ev in ([st["w"]] if st["w"] else []) + st["r"]:
                if ev[0].name not in allev or allev[ev[0].name][1] < ev[1]:
                    allev[ev[0].name] = ev
        for ev in allev.values():
            self._wait("sp", ev)


def col_tiles():
    tl = []
    for g in range(4):
        tl.append(("f", g, g * 128))
    for h in range(NH):
        tl.append(("q", h, 512 + h * 128))
    for h in range(NH):
        tl.append(("k", h, 1536 + h * 128))
    for h in range(NH):
        tl.append(("v", h, 2560 + h * 128))
    for h in range(NH):
        tl.append(("z", h, 3584 + h * 128))
    for j in range(8):
        tl.append(("gf", j, 4640 + j * 128))
    for j in range(8):
        tl.append(("gd", j, 5664 + j * 128))
    return tl


PBLK = [(0, 512), (512, 512), (1024, 512), (1536, 512), (2048, 16)]
RBLK = [(16 + 512 * i, 512) for i in range(4)]


def build(stage=99, debug=False):
    nc = bass.Bass("TRN2", target_bir_lowering=False)
    S = Sched(nc)
    global LAST_SCHED
    LAST_SCHED = S
    op, dma = S.op, S.dma
    V, A, PE, G = nc.vector, nc.scalar, nc.tensor, nc.gpsimd

    def din(name, shape, dt=F32):
        return nc.dram_tensor(name, list(shape), dt, kind="ExternalInput").ap()

    x = din("x", [SEQ, D])
    meta = din("meta_tokens", [NM, D])
    norm1_g = din("norm1_g", [1, D])
    w_in = din("w_in", [1, D, INW])
    conv_w = din("conv_w", [1, 5, 3072])
    a_log_f = din("a_log_fwd", [1, 8]); dt_b_f = din("dt_bias_fwd", [1, 8])
    a_log_b = din("a_log_bwd", [1, 8]); dt_b_b = din("dt_bias_bwd", [1, 8])
    out_norm_g = din("out_norm_g", [1, 128])
    w_fourier = din("w_fourier", [1, 512, D])
    w_delta = din("w_delta", [1, D, D])
    w_out = din("w_out", [1, D, D])
    norm2_g = din("norm2_g", [1, D])
    w_rg = din("w_router_group", [1, D, 8]); b_rg = din("b_router_group", [1, 8])
    w_re = din("w_router_expert", [1, D, 64]); b_re = din("b_router_expert", [1, 64])
    w_gate_e = din("w_gate_e", [1, NEXP, D, DE]); w_up_e = din("w_up_e", [1, NEXP, D, DE])
    w_down_e = din("w_down_e", [1, NEXP, DE, D])
    final_g = din("final_norm_g", [D])
    c_identb = din("c_identb", [128, 128], BF16)
    c_identf = din("c_identf", [128, 128], F32)
    c_cosL = din("c_cosL", [L, L], BF16)
    c_nsinL = din("c_nsinL", [L, L], BF16)
    c_cs128 = din("c_cs128", [128, 256], BF16)
    c_masks = din("c_masks", [7, 128, 128], F32)
    c_ecap = din("c_ecap", [128, 64], F32)

    out = nc.dram_tensor("out", [SEQ, D], F32, kind="ExternalOutput").ap()

    dbg = {}

    def scratch(name, shape, dt):
        kind = "ExternalOutput" if debug else "Internal"
        t = nc.dram_tensor(name, list(shape), dt, kind=kind).ap()
        dbg[name] = t
        return t

    qT_s = scratch("qT_s", [NH, 128, L], BF16)
    kT_s = scratch("kT_s", [NH, 128, L], BF16)
    vT_s = scratch("vT_s", [NH, 128, L], BF16)
    z_s = scratch("z_s", [NH, 128, SEQ], BF16)
    gf_s = scratch("gf_s", [8, 128, SEQ], BF16)
    gd_s = scratch("gd_s", [8, 128, SEQ], BF16)
    ab_s = scratch("ab_s", [L, 32], F32)
    fin_s = scratch("fin_s", [4, 128, L], BF16)

    wq_g = nc.dram_tensor("wq_g", [NEXP, 128, 8 * DE], BF16, kind="Internal").ap()
    wq_u = nc.dram_tensor("wq_u", [NEXP, 128, 8 * DE], BF16, kind="Internal").ap()
    wq_d = nc.dram_tensor("wq_d", [NEXP, 128, 2 * D], BF16, kind="Internal").ap()

    def precast_gen():
        for e in range(NEXP):
            kk_ = ("pc", e // 2)
            dma("pool", wq_g[e], w_gate_e[0, e].rearrange("(p kt) j -> p (kt j)", kt=8), w=[("wq", e // 2)], key=kk_)
            yield
            dma("pool", wq_u[e], w_up_e[0, e].rearrange("(p kt) j -> p (kt j)", kt=8), w=[("wq", e // 2)], key=kk_)
            yield
            dma("pool", wq_d[e].rearrange("p (jt d) -> p jt d", jt=2), w_down_e[0, e].rearrange("(jt p) d -> p jt d", p=128), w=[("wq", e // 2)], key=kk_)
            yield

    pcg = precast_gen() if (stage >= 5 and os.environ.get("PRECAST", "1") == "1") else iter(())

    def pc_step(k=1):
        for _ in range(k):
            next(pcg, None)

    with contextlib.ExitStack() as gst:
        def sb(name, shape, dt, st=gst):
            return st.enter_context(nc.sbuf_tensor(name, list(shape), dt))

        def ps(name, shape, dt, st=gst):
            S.excl.add(name)
            return st.enter_context(nc.psum_tensor(name, list(shape), dt))

        identb = sb("identb", [128, 128], BF16)
        identf = sb("identf", [128, 128], F32)
        epst = sb("epst", [128, 1], F32)
        dma("sp", identb[:], c_identb, w=["identb"])
        dma("sp", identf[:], c_identf, w=["identf"])
        op("dve", lambda: V.memset(epst[:], EPS), w=["epst"])
        Xs = nc.dram_tensor("Xs", [NEXP * CAP, D], BF16, kind="Internal").ap()
        Ys = nc.dram_tensor("Ys", [NEXP * CAP, D], F32, kind="Internal").ap()
        zfill = sb("zfill", [128, 2048], BF16)
        op("dve", lambda: V.memset(zfill[:], 0.0), w=["zfill"])
        Xs_v = Xs.rearrange("(p a) c -> p (a c)", p=128)
        for i in range(32):
            dma("pool", Xs_v[:, i * 2048:(i + 1) * 2048], zfill[:], r=["zfill"], w=["Xs"], key="Xs")

        with contextlib.ExitStack() as st:
            hnT = sb("hnT", [128, 8, L], BF16, st)
            stP1 = contextlib.ExitStack()
            g1b = sb("g1b", [128, D], F32, stP1)
            dma("sp", g1b[:], norm1_g[0].partition_broadcast(128), w=["g1b"])
            xt = [sb(f"xt{i}", [128, D], F32, stP1) for i in range(2)]
            junk = sb("junk", [128, D], BF16, stP1)
            hnb = [sb(f"hnb{i}", [128, D], BF16, stP1) for i in range(2)]
            ss = [sb(f"ss{i}", [128, 1], F32, stP1) for i in range(2)]
            with contextlib.ExitStack() as st1:
                tp = [ps(f"tp{i}", [128, 8, 128], BF16, st1) for i in range(2)]

                for t in (range(NT + 1) if 'KT' not in os.environ else [int(v) for v in os.environ['KT'].split(',')]):
                    i = t % 2
                    if t == 0:
                        n, src, p0 = NM, meta, 0
                    else:
                        n, src, p0 = 128, x[(t - 1) * 128:t * 128, :], NM + (t - 1) * 128
                    dma("sp", xt[i][0:n, :], src, w=[f"xt{i}"])
                    op("act", lambda: A.activation(out=junk[0:n, :], in_=xt[i][0:n, :], func=AF.Square, accum_out=ss[i][0:n, :]),
                       r=[f"xt{i}"], w=["junk", f"ss{i}"])
                    op("act", lambda: A.activation(out=ss[i][0:n, :], in_=ss[i][0:n, :], func=AF.Sqrt, scale=1.0 / D, bias=epst[0:n, :]),
                       r=[f"ss{i}", "epst"], w=[f"ss{i}"])
                    op("dve", lambda: V.reciprocal(out=ss[i][0:n, :], in_=ss[i][0:n, :]), r=[f"ss{i}"], w=[f"ss{i}"])
                    op("dve", lambda: V.scalar_tensor_tensor(out=hnb[i][0:n, :], in0=xt[i][0:n, :], scalar=ss[i][0:n, 0:1], in1=g1b[0:n, :],
                                                             op0=ALU.mult, op1=ALU.mult),
                       r=[f"xt{i}", f"ss{i}", "g1b"], w=[f"hnb{i}"])
                    for kt in range(8):
                        op("pe", lambda: PE.transpose(tp[i][:, kt, 0:n], hnb[i][0:n, kt * 128:(kt + 1) * 128], identb[0:n, 0:n]),
                           r=[f"hnb{i}", "identb"], w=[f"tp{i}"])
                    op("act", lambda: A.copy(out=hnT[:, :, p0:p0 + n], in_=tp[i][:, :, 0:n]), r=[f"tp{i}"], w=[("hnT", t)])
                S.barrier()
            stP1.close()
            hnT_all = [("hnT", t) for t in range(NT + 1)]
            if 'KT' in os.environ:
                hnT_all = [("hnT", int(v)) for v in os.environ['KT'].split(',')]

            if debug:
                hnT_d = scratch("hnT_d", [8, 128, L], BF16)
                for kt in range(8):
                    dma("sp", hnT_d[kt], hnT[:, kt, :], r=hnT_all, key="dbg_hnT")

            cwrow = sb("cwrow", [5, 3072], F32, st)
            dma("sp", cwrow[:], conv_w[0], w=["cwrow"])
            cw = sb("cw", [128, 24, 5], F32, st)
            with contextlib.ExitStack() as st1:
                pcw = ps("pcw", [128, 24, 5], F32, st1)
                for tt in range(24):
                    op("pe", lambda: PE.matmul(pcw[:, tt, :], lhsT=cwrow[:, tt * 128:(tt + 1) * 128], rhs=identf[0:5, 0:5], start=True, stop=True),
                       r=["cwrow", "identf"], w=["pcw"])
                op("dve", lambda: V.tensor_copy(out=cw[:], in_=pcw[:]), r=["pcw"], w=["cw"])
                S.barrier()

            NW = 4
            wt = [sb(f"wt{i}", [128, 8, 512], BF16, st) for i in range(NW)]
            NSTG = 3
            stg = [sb(f"stg{i}", [128, L + 4], BF16, st) for i in range(NSTG)]
            dgt = [sb(f"dgt{i}", [128, 5, 128], BF16, st) for i in range(3)]
            for i in range(NSTG):
                op("pool", lambda: G.memset(stg[i][:], 0.0), w=[f"stg{i}"])
            sact2 = [sb(f"sact{i}", [128, L], F32, st) for i in range(3)]
            sq2 = [sb(f"sq{i}", [128, L], BF16, st) for i in range(3)]
            rn2 = [sb(f"rn{i}", [128, L], F32, st) for i in range(3)]
            NOB = 3
            ob = [sb(f"ob{i}", [128, L], BF16, st) for i in range(NOB)]
            onesb = sb("onesb", [128, 128], BF16, st)
            op("dve", lambda: V.memset(onesb[:], 1.0), w=["onesb"])
            w_in_v = w_in[0].rearrange("(kt p) c -> p kt c", p=128)
            tiles_all = col_tiles()
            conv_t = [i for i, tl in enumerate(tiles_all) if tl[0] in ("q", "k", "v")]
            plain_t = [i for i, tl in enumerate(tiles_all) if tl[0] not in ("q", "k", "v")]
            order = []
            for j in range(max(len(conv_t), len(plain_t))):
                if j < len(conv_t):
                    order.append(conv_t[j])
                if j < len(plain_t):
                    order.append(plain_t[j])
            if stage < 1:
                order = order[:int(stage * 100)]
            grp_buf = {}
            cnt = {"na": 0, "nob": 0, "nconv": 0, "npc": 0}
            tstate = {}
            with contextlib.ExitStack() as st1:
                acc = [ps(f"acc{i}", [128, 512], F32, st1) for i in range(4)]
                ssb = [ps(f"ssb{i}", [128, 512], F32, st1) for i in range(2)]
                pcv = [ps(f"pcv{i}", [128, 512], F32, st1) for i in range(2)]

                def next_ob():
                    oi = cnt["nob"] % NOB
                    cnt["nob"] += 1
                    return oi

                def stageA(ti):
                    typ, idx, c0 = tiles_all[ti]
                    g = ti // 4
                    if g not in grp_buf:
                        grp_buf[g] = len(grp_buf) % NW
                        gc0 = tiles_all[g * 4][2]
                        dma("pool", wt[grp_buf[g]][:], w_in_v[:, :, gc0:gc0 + 512], w=[f"wt{grp_buf[g]}"])
                    wi = grp_buf[g]
                    wo_ = (ti % 4) * 128
                    conv = typ in ("q", "k", "v")
                    blks = PBLK if typ in ("f", "q", "k", "v") else RBLK
                    stt = {}
                    if conv:
                        stt["si"] = cnt["nconv"] % NSTG
                        stt["ci"] = cnt["nconv"] % 3
                        cnt["nconv"] += 1
                    else:
                        stt["oi"] = next_ob()
                    tstate[ti] = stt
                    for (b0, bn) in blks:
                        ai = cnt["na"] % 4
                        cnt["na"] += 1
                        for kt in range(8):
                            op("pe", lambda: PE.matmul(acc[ai][:, 0:bn], lhsT=wt[wi][:, kt, wo_:wo_ + 128], rhs=hnT[:, kt, b0:b0 + bn], start=(kt == 0), stop=(kt == 7)),
                               r=[f"wt{wi}"] + hnT_all, w=[f"acc{ai}"])
                        if conv:
                            si = stt["si"]
                            op("act", lambda: A.copy(out=stg[si][:, 2 + b0:2 + b0 + bn], in_=acc[ai][:, 0:bn]), r=[f"acc{ai}"], w=[f"stg{si}"])
                        else:
                            oi = stt["oi"]
                            if typ == "f":
                                op("act", lambda: A.copy(out=ob[oi][:, b0:b0 + bn], in_=acc[ai][:, 0:bn]), r=[f"acc{ai}"], w=[f"ob{oi}"])
                            else:
                                fn_ = AF.Silu if typ == "z" else AF.Sigmoid
                                op("act", lambda: A.activation(out=ob[oi][:, b0:b0 + bn], in_=acc[ai][:, 0:bn], func=fn_), r=[f"acc{ai}"], w=[f"ob{oi}"])
                        yield
                    if not conv:
                        oi = stt["oi"]
                        if typ == "f":
                            dma("sp", fin_s[idx], ob[oi][:], r=[f"ob{oi}"], key=f"ob{oi}")
                        else:
                            dst = {"z": z_s, "gf": gf_s, "gd": gd_s}[typ]
                            dma("sp", dst[idx], ob[oi][:, NM:L], r=[f"ob{oi}"], key=f"ob{oi}")

                def stageB(ti):
                    typ, idx, c0 = tiles_all[ti]
                    if typ not in ("q", "k", "v"):
                        return
                    stt = tstate[ti]
                    si, ci = stt["si"], stt["ci"]
                    sact, sq = sact2[ci], sq2[ci]
                    ks_, kq = f"sact{ci}", f"sq{ci}"
                    ct = {"q": 0, "k": 8, "v": 16}[typ] + idx
                    dg, kdg = dgt[ci], f"dgt{ci}"
                    for kk in range(5):
                        op("dve", lambda: V.tensor_scalar_mul(out=dg[:, kk, :], in0=identb[:], scalar1=cw[:, ct, kk:kk + 1]), r=["identb", "cw"], w=[kdg])
                    if typ == "v":
                        oi = next_ob()
                    for bi, (b0, bn) in enumerate(PBLK):
                        pi = cnt["npc"] % 2
                        cnt["npc"] += 1
                        for kk in range(5):
                            op("pe", lambda: PE.matmul(pcv[pi][:, 0:bn], lhsT=dg[:, kk, :], rhs=stg[si][:, b0 + kk:b0 + kk + bn], start=(kk == 0), stop=(kk == 4)),
                               r=[kdg, f"stg{si}"], w=[f"pcv{pi}"])
                        if typ == "v":
                            op("act", lambda: A.activation(out=ob[oi][:, b0:b0 + bn], in_=pcv[pi][:, 0:bn], func=AF.Silu), r=[f"pcv{pi}"], w=[f"ob{oi}"])
                        else:
                            op("act", lambda: A.activation(out=sact[:, b0:b0 + bn], in_=pcv[pi][:, 0:bn], func=AF.Silu), r=[f"pcv{pi}"], w=[ks_])
                    if typ == "v":
                        dma("sp", vT_s[idx], ob[oi][:], r=[f"ob{oi}"], key=f"ob{oi}")
                    else:
                        op("act", lambda: A.activation(out=sq[:], in_=sact[:], func=AF.Square), r=[ks_], w=[kq])

                def stageC(ti):
                    typ, idx, c0 = tiles_all[ti]
                    if typ not in ("q", "k"):
                        return
                        yield
                    stt = tstate[ti]
                    ci = stt["ci"]
                    sact, sq, rn = sact2[ci], sq2[ci], rn2[ci]
                    ks_, kq, kr = f"sact{ci}", f"sq{ci}", f"rn{ci}"
                    for bi, (b0, bn) in enumerate(PBLK):
                        pi = bi % 2
                        op("pe", lambda: PE.matmul(ssb[pi][:, 0:bn], lhsT=onesb[:], rhs=sq[:, b0:b0 + bn], start=True, stop=True), r=["onesb", kq], w=[f"ssb{pi}"])
                        op("act", lambda: A.activation(out=rn[:, b0:b0 + bn], in_=ssb[pi][:, 0:bn], func=AF.Sqrt, bias=epst[:], scale=1.0), r=[f"ssb{pi}", "epst"], w=[kr])
                        yield
                    op("dve", lambda: V.reciprocal(out=rn[:], in_=rn[:]), r=[kr], w=[kr])
                    oi = next_ob()
                    sc = (128 ** -0.5) if typ == "q" else 1.0
                    op("dve", lambda: V.scalar_tensor_tensor(out=ob[oi][:], in0=sact[:], scalar=sc, in1=rn[:], op0=ALU.mult, op1=ALU.mult), r=[ks_, kr], w=[f"ob{oi}"])
                    dst = qT_s if typ == "q" else kT_s
                    dma("sp", dst[idx], ob[oi][:], r=[f"ob{oi}"], key=f"ob{oi}")

                for s_ in range(len(order) + 4):
                    pc_step(2)
                    gA = stageA(order[s_]) if s_ < len(order) else iter(())
                    gC = stageC(order[s_ - 4]) if 0 <= s_ - 4 < len(order) else iter(())
                    doneA = doneC = False
                    while not (doneA and doneC):
                        if not doneA:
                            doneA = next(gA, "END") == "END"
                        if not doneC:
                            doneC = next(gC, "END") == "END"
                    if 0 <= s_ - 1 < len(order):
                        stageB(order[s_ - 1])
                na = cnt["na"]
                wab = sb("wab", [128, 8, 32], BF16, st)
                dma("pool", wab[:], w_in_v[:, :, 4608:4640], w=["wab"])
                abt = sb("abt", [128, 32], F32, st)
                for t in range(NT + 1 if stage >= 1 else 0):
                    n, p0 = (NM, 0) if t == 0 else (128, NM + (t - 1) * 128)
                    ai = na % 4
                    na += 1
                    for kt in range(8):
                        op("pe", lambda: PE.matmul(acc[ai][0:n, 0:32], lhsT=hnT[:, kt, p0:p0 + n], rhs=wab[:, kt, :], start=(kt == 0), stop=(kt == 7)),
                           r=["wab"] + hnT_all, w=[f"acc{ai}"])
                    op("act", lambda: A.copy(out=abt[0:n, :], in_=acc[ai][0:n, 0:32]), r=[f"acc{ai}"], w=["abt"])
                    dma("sp", ab_s[p0:p0 + n, :], abt[0:n, :], r=["abt"], key="abt")
                S.barrier()
            S.barrier()

        if stage < 2:
            S.finish()
            return nc, dbg
        mrg_s = scratch("mrg_s", [8, 128, SEQ], BF16)
        with contextlib.ExitStack() as st:
            mrgT = sb("mrgT", [128, 8, SEQ], BF16, st)
            finT = sb("finT", [128, 4, L], BF16, st)
            for g in range(4):
                dma("sp", finT[:, g, :], fin_s[g], w=["finT"], key="finT")
            cs128 = sb("cs128", [128, 256], BF16, st)
            dma("sp", cs128[:], c_cs128, w=["cs128"])
            Y = sb("Y", [128, NT + 1, 4, 256], BF16, st)
            fmixT = sb("fmixT", [128, 4, SEQ], BF16, st)
            wf = sb("wf", [128, 4, D], BF16, st)
            dma("pool", wf[:], w_fourier[0].rearrange("(g p) d -> p g d", p=128), w=["wf"])
            CLb = [sb(f"CLb{i}", [128, NT + 1, 512], BF16, st) for i in range(2)]
            SLb = [sb(f"SLb{i}", [128, NT + 1, 512], BF16, st) for i in range(2)]
            sgf = [sb(f"sgf{i}", [128, SEQ], BF16, st) for i in range(2)]
            with contextlib.ExitStack() as st1:
                py = [ps(f"py{i}", [128, 2, 256], F32, st1) for i in range(2)]
                pa = [ps(f"pa{i}", [128, 512], F32, st1) for i in range(4)]
                npy = 0
                for t in range(NT + 1):
                    n, p0 = (NM, 0) if t == 0 else (128, NM + (t - 1) * 128)
                    for g2 in range(2):
                        pi = npy % 2
                        npy += 1
                        for gi in range(2):
                            g = g2 * 2 + gi
                            op("pe", lambda: PE.matmul(py[pi][0:n, gi, :], lhsT=finT[:, g, p0:p0 + n], rhs=cs128[:], start=True, stop=True),
                               r=["finT", "cs128"], w=[f"py{pi}"])
                        op("act", lambda: A.copy(out=Y[0:n, t, g2 * 2:g2 * 2 + 2, :], in_=py[pi][0:n, :, :]), r=[f"py{pi}"], w=["Y"])
                npa = 0
                for bi, (b0, bn) in enumerate(RBLK):
                    ci = bi % 2
                    for (dst, srcm, nm) in ((CLb[ci], c_cosL, f"CLb{ci}"), (SLb[ci], c_nsinL, f"SLb{ci}")):
                        dma("sp", dst[0:NM, 0, :], srcm[0:NM, b0:b0 + bn], w=[nm], key=nm)
                        for hh in range(2):
                            dma("sp", dst[:, 1 + hh * 8:9 + hh * 8, :],
                                srcm[NM + hh * 1024:NM + (hh + 1) * 1024, b0:b0 + bn].rearrange("(t p) c -> p t c", p=128), w=[nm], key=nm)
                    for g in range(4):
                        ai = npa % 4
                        npa += 1
                        for t in range(NT + 1):
                            n = NM if t == 0 else 128
                            op("pe", lambda: PE.matmul(pa[ai][:, :], lhsT=Y[0:n, t, g, 0:128], rhs=CLb[ci][0:n, t, :], start=(t == 0), stop=False),
                               r=["Y", f"CLb{ci}"], w=[f"pa{ai}"])
                            op("pe", lambda: PE.matmul(pa[ai][:, :], lhsT=Y[0:n, t, g, 128:256], rhs=SLb[ci][0:n, t, :], start=False, stop=(t == NT)),
                               r=["Y", f"SLb{ci}"], w=[f"pa{ai}"])
                        op("act", lambda: A.copy(out=fmixT[:, g, b0 - NM:b0 - NM + bn], in_=pa[ai][:, :]), r=[f"pa{ai}"], w=["fmixT"])
                if debug:
                    fmix_d = scratch("fmix_d", [4, 128, SEQ], BF16)
                    for g in range(4):
                        dma("sp", fmix_d[g], fmixT[:, g, :], r=["fmixT"], key="dbg_fmix")
                for dt_ in range(8):
                    gi_ = dt_ % 2
                    dma("sp", sgf[gi_][:], gf_s[dt_], w=[f"sgf{gi_}"])
                    for bi in range(4):
                        ai = npa % 4
                        npa += 1
                        for g in range(4):
                            op("pe", lambda: PE.matmul(pa[ai][:, :], lhsT=wf[:, g, dt_ * 128:(dt_ + 1) * 128], rhs=fmixT[:, g, bi * 512:(bi + 1) * 512],
                                                       start=(g == 0), stop=(g == 3)),
                               r=["wf", "fmixT"], w=[f"pa{ai}"])
                        op("dve", lambda: V.tensor_tensor(out=mrgT[:, dt_, bi * 512:(bi + 1) * 512], in0=pa[ai][:, :], in1=sgf[gi_][:, bi * 512:(bi + 1) * 512], op=ALU.mult),
                           r=[f"pa{ai}", f"sgf{gi_}"], w=[("mrgT", dt_)])
                S.barrier()
            for dt_ in range(8):
                dma("sp", mrg_s[dt_], mrgT[:, dt_, :], r=[("mrgT", dt_)], w=["mrg_s"], key="mrg_s")
            S.barrier()
        if stage < 3:
            S.finish()
            return nc, dbg
        ogT = sb("ogT", [128, 8, SEQ], BF16)
        GE, ge = (G, "pool") if os.environ.get("P4POOL", "1") == "1" else (V, "dve")
        of_s = scratch("of_s", [NT, 128, NH, 128], F32)
        with contextlib.ExitStack() as st:
            masks = sb("masks", [128, 6, 128], F32, st)
            dma("sp", masks[:], c_masks[0:6].rearrange("m p f -> p m f"), w=["masks"])
            onesf = sb("onesf", [128, 128], F32, st)
            op("dve", lambda: V.memset(onesf[:], 1.0), w=["onesf"])
            onec = sb("onec", [128, 1], F32, st)
            op("dve", lambda: V.memset(onec[:], 1.0), w=["onec"])
            gout = sb("gout", [128, 1], F32, st)
            dma("sp", gout[:], out_norm_g.rearrange("o d -> d o"), w=["gout"])
            gball = sb("gball", [128, NT + 1, 2, 2, 8], F32, st)
            dtb = sb("dtb", [128, 2, 8], F32, st)
            nega = sb("nega", [128, 2, 8], F32, st)
            dma("sp", dtb[:, 0, :], dt_b_f[0].partition_broadcast(128), w=["dtb"], key="dtb")
            dma("sp", dtb[:, 1, :], dt_b_b[0].partition_broadcast(128), w=["dtb"], key="dtb")
            dma("sp", nega[:, 0, :], a_log_f[0].partition_broadcast(128), w=["nega"], key="nega")
            dma("sp", nega[:, 1, :], a_log_b[0].partition_broadcast(128), w=["nega"], key="nega")
            op("act", lambda: A.activation(out=nega[:], in_=nega[:], func=AF.Exp), r=["nega"], w=["nega"])
            op("act", lambda: A.mul(out=nega[:], in_=nega[:], mul=-1.0), r=["nega"], w=["nega"])
            abl = sb("abl", [128, NT + 1, 2, 2, 8], F32, st)
            xa = sb("xa", [128, NT + 1, 2, 8], F32, st)
            op("dve", lambda: V.memset(abl[:], 0.0), w=["abl"])
            dma("sp", abl[0:NM, 0], ab_s[0:NM, :].rearrange("p (d t h) -> p d t h", d=2, t=2), w=["abl"], key="abl")
            for hh in range(2):
                dma("sp", abl[:, 1 + hh * 8:9 + hh * 8], ab_s[NM + hh * 1024:NM + (hh + 1) * 1024, :].rearrange("(t p) (d u h) -> p t d u h", p=128, d=2, u=2),
                    w=["abl"], key="abl")
            NT1 = NT + 1
            op("dve", lambda: V.tensor_tensor(out=xa[:], in0=abl[:, :, :, 0, :], in1=dtb[:].unsqueeze(1).to_broadcast([128, NT1, 2, 8]), op=ALU.add), r=["abl", "dtb"], w=["xa"])
            op("act", lambda: A.activation(out=xa[:], in_=xa[:], func=AF.Exp), r=["xa"], w=["xa"])
            op("act", lambda: A.activation(out=xa[:], in_=xa[:], func=AF.Ln, bias=onec[:, :], scale=1.0), r=["xa", "onec"], w=["xa"])
            op("dve", lambda: V.tensor_tensor(out=gball[:, :, :, 0, :], in0=xa[:], in1=nega[:].unsqueeze(1).to_broadcast([128, NT1, 2, 8]), op=ALU.mult), r=["xa", "nega"], w=["gball"])
            op("act", lambda: A.activation(out=gball[:, :, :, 1, :], in_=abl[:, :, :, 1, :], func=AF.Sigmoid), r=["abl"], w=["gball"])
            if debug:
                gb_d = scratch("gb_d", [L, 32], F32)
                for t in range(NT + 1):
                    n, p0 = (NM, 0) if t == 0 else (128, NM + (t - 1) * 128)
                    dma("sp", gb_d[p0:p0 + n, :].rearrange("p (d t h) -> p d t h", d=2, t=2), gball[0:n, t], r=["gball"], key="dbg_gb")

            ob_s = scratch("ob_s", [NT, 128, NH, 128], F32)
            with contextlib.ExitStack() as st2:
                F4 = lambda nm, dt=F32: sb(nm, [128, 4, 128], dt, st2)
                SB = {}
                NSTR = 4
                for sid in range(NSTR):
                    for i in range(2):
                        for nm in ("qTt", "kTt", "vTt"):
                            SB[f"{nm}{i}_{sid}"] = F4(f"{nm}{i}_{sid}", BF16)
                    for nm in ("ktm", "vtm", "Sb_"):
                        SB[f"{nm}_{sid}"] = F4(f"{nm}_{sid}", BF16)
                    for nm in ("Gm", "oTt", "Sf"):
                        SB[f"{nm}_{sid}"] = F4(f"{nm}_{sid}", F32)
                    for nm in ("gcc", "egc", "bgc", "gend", "kds"):
                        SB[f"{nm}_{sid}"] = sb(f"{nm}_{sid}", [128, 4], F32, st2)
                    for nm in ("diff", "DL", "DU", "u_sb", "egb"):
                        SB[f"{nm}_{sid}"] = F4(f"{nm}_{sid}")
                    for nm in ("Ab", "ATb", "QKT", "TTb", "vbt", "kbg", "kdec", "nwT", "qdT", "vnew", "Pb0", "Pb1", "PTb0", "PTb1"):
                        SB[f"{nm}_{sid}"] = F4(f"{nm}_{sid}", BF16)

                def p4_stream(sid, pX, pY):
                    d_, hg = sid // 2, sid % 2
                    H0 = hg * 4
                    K_ = lambda nm: f"{nm}_{sid}"
                    B_ = lambda nm: SB[f"{nm}_{sid}"]
                    Gm, gcc, egc, bgc, gend, kds = B_("Gm"), B_("gcc"), B_("egc"), B_("bgc"), B_("gend"), B_("kds")
                    diff, DL, DU, u_sb, egb = [B_(x) for x in ("diff", "DL", "DU", "u_sb", "egb")]
                    Ab, ATb, QKT, TTb, vbt, kbg, kdec, nwT, qdT, vnew = [B_(x) for x in ("Ab", "ATb", "QKT", "TTb", "vbt", "kbg", "kdec", "nwT", "qdT", "vnew")]
                    ktm, vtm, Sb_, oTt, Sf = B_("ktm"), B_("vtm"), B_("Sb_"), B_("oTt"), B_("Sf")
                    kX, kY = K_("pX"), K_("pY")
                    S.excl.update([kX, kY])
                    pYb = pY[:].bitcast(BF16)
                    op("dve", lambda: V.memset(Sf[:], 0.0), r=[], w=[K_("Sf")])
                    op("dve", lambda: V.memset(Sb_[:], 0.0), r=[], w=[K_("Sb_")])
                    order = list(range(0, NT + 1)) if d_ == 0 else list(range(NT, 0, -1))
                    mC, mA, mQ = masks[:, 0 + d_, :], masks[:, 2 + d_, :], masks[:, 4 + d_, :]
                    o_dst = of_s if d_ == 0 else ob_s
                    for it, t in enumerate(order):
                        n, p0 = (NM, 0) if t == 0 else (128, NM + (t - 1) * 128)
                        li = it % 2
                        qTl, kTl, vTl = B_(f"qTt{li}"), B_(f"kTt{li}"), B_(f"vTt{li}")
                        qn, kn, vn_ = K_(f"qTt{li}"), K_(f"kTt{li}"), K_(f"vTt{li}")
                        for (dst, src, nm) in ((qTl, qT_s, qn), (kTl, kT_s, kn), (vTl, vT_s, vn_)):
                            dma("sp", dst[:, :, 0:n], src[H0:H0 + 4, :, p0:p0 + n].rearrange("h d p -> d h p"), w=[nm])
                        yield
                        for (srcT, dstm, sn, dn) in ((kTl, ktm, kn, K_("ktm")), (vTl, vtm, vn_, K_("vtm"))):
                            for hi in range(4):
                                op("pe", lambda: PE.transpose(pYb[0:n, hi, 0:128], srcT[:, hi, 0:n], identb[:, :]), r=[sn, "identb"], w=[kY])
                            op("act", lambda: A.copy(out=dstm[0:n], in_=pYb[0:n, :, 0:128]), r=[kY], w=[dn])
                            yield
                        gcol = gball[0:n, t, d_, 0, H0:H0 + 4]
                        bcol = gball[0:n, t, d_, 1, H0:H0 + 4]
                        op("dve", lambda: V.tensor_tensor(out=Gm[0:n, :, 0:n], in0=mC[0:n, 0:n].unsqueeze(1).to_broadcast([n, 4, n]),
                                                          in1=gcol.unsqueeze(2).to_broadcast([n, 4, n]), op=ALU.mult), r=["masks", "gball"], w=[K_("Gm")])
                        op("pe", lambda: PE.matmul(pX[0:n, 0, 0:4], lhsT=mC[0:n, 0:n], rhs=gcol, start=True, stop=True), r=["masks", "gball"], w=[kX])
                        op("dve", lambda: V.tensor_copy(out=gcc[0:n], in_=pX[0:n, 0, 0:4]), r=[kX], w=[K_("gcc")])
                        op("act", lambda: A.activation(out=egc[0:n], in_=gcc[0:n], func=AF.Exp), r=[K_("gcc")], w=[K_("egc")])
                        op("dve", lambda: V.tensor_tensor(out=bgc[0:n], in0=egc[0:n], in1=bcol, op=ALU.mult), r=[K_("egc"), "gball"], w=[K_("bgc")])
                        yield
                        if t == 0:
                            chunks, ends = [(0, NM)], [NM - 1]
                        elif d_ == 0:
                            chunks, ends = [(0, 64), (64, 64)], [63, 127]
                        else:
                            chunks, ends = [(64, 64), (0, 64)], [64, 0]
                        if n == 128:
                            op("pe", lambda: PE.matmul(pX[:, :, 0:n], lhsT=onesf[0:n, :], rhs=Gm[0:n, :, 0:n], start=True, stop=True), r=["onesf", K_("Gm")], w=[kX])
                        else:
                            for hi in range(4):
                                op("pe", lambda: PE.matmul(pX[:, hi, 0:n], lhsT=onesf[0:n, :], rhs=Gm[0:n, hi, 0:n], start=True, stop=True), r=["onesf", K_("Gm")], w=[kX])
                        for hi in range(4):
                            op("pe", lambda: PE.matmul(pY[0:n, hi, 0:n], lhsT=kTl[:, hi, 0:n], rhs=kTl[:, hi, 0:n], start=True, stop=True), r=[kn], w=[kY])
                        op("dve", lambda: V.tensor_tensor(out=diff[0:n, :, 0:n], in0=gcc[0:n, :].unsqueeze(2).to_broadcast([n, 4, n]),
                                                          in1=pX[0:n, :, 0:n], op=ALU.subtract), r=[K_("gcc"), kX], w=[K_("diff")])
                        op("act", lambda: A.activation(out=egb[:, :, 0:n], in_=pX[:, :, 0:n], func=AF.Exp), r=[kX], w=[K_("egb")])
                        for ci, (r0, cn) in enumerate(chunks):
                            op("dve", lambda: V.tensor_copy(out=gend[r0:r0 + cn, :], in_=pX[r0:r0 + cn, :, ends[ci]]), r=[kX], w=[K_("gend")])
                        yield
                        op("dve", lambda: V.scalar_tensor_tensor(out=DL[0:n, :, 0:n], in0=diff[0:n, :, 0:n], scalar=0.0,
                                                                 in1=mA[0:n, 0:n].unsqueeze(1).to_broadcast([n, 4, n]), op0=ALU.min, op1=ALU.add),
                           r=[K_("diff"), "masks"], w=[K_("DL")])
                        op("dve", lambda: V.scalar_tensor_tensor(out=DU[0:n, :, 0:n], in0=diff[0:n, :, 0:n], scalar=0.0,
                                                                 in1=mQ[0:n, 0:n].unsqueeze(1).to_broadcast([n, 4, n]), op0=ALU.max, op1=ALU.add),
                           r=[K_("diff"), "masks"], w=[K_("DU")])
                        op("act", lambda: A.activation(out=DL[0:n, :, 0:n], in_=DL[0:n, :, 0:n], func=AF.Exp), r=[K_("DL")], w=[K_("DL")])
                        op("act", lambda: A.activation(out=DU[0:n, :, 0:n], in_=DU[0:n, :, 0:n], func=AF.Exp, scale=-1.0), r=[K_("DU")], w=[K_("DU")])
                        for hi in range(4):
                            op("pe", lambda: PE.matmul(pX[0:n, hi, 0:n], lhsT=kTl[:, hi, 0:n], rhs=qTl[:, hi, 0:n], start=True, stop=True), r=[kn, qn], w=[kX])
                        op("dve", lambda: V.tensor_tensor(out=kds[0:n, :], in0=gend[0:n, :], in1=gcc[0:n, :], op=ALU.subtract), r=[K_("gend"), K_("gcc")], w=[K_("kds")])
                        op("act", lambda: A.activation(out=kds[0:n, :], in_=kds[0:n, :], func=AF.Exp), r=[K_("kds")], w=[K_("kds")])
                        yield
                        op("dve", lambda: V.tensor_tensor(out=diff[0:n, :, 0:n], in0=pY[0:n, :, 0:n], in1=DL[0:n, :, 0:n], op=ALU.mult), r=[kY, K_("DL")], w=[K_("diff")])
                        op(ge, lambda: GE.tensor_tensor(out=Ab[0:n, :, 0:n], in0=diff[0:n, :, 0:n], in1=bcol.unsqueeze(2).to_broadcast([n, 4, n]), op=ALU.mult),
                           r=[K_("diff"), "gball"], w=[K_("Ab")])
                        op("dve", lambda: V.tensor_tensor(out=QKT[0:n, :, 0:n], in0=pX[0:n, :, 0:n], in1=DU[0:n, :, 0:n], op=ALU.mult), r=[kX, K_("DU")], w=[K_("QKT")])
                        yield
                        for hi in range(4):
                            op("pe", lambda: PE.transpose(pYb[0:n, hi, 0:n], Ab[0:n, hi, 0:n], identb[0:n, 0:n]), r=[K_("Ab"), "identb"], w=[kY])
                        op("act", lambda: A.copy(out=ATb[0:n, :, 0:n], in_=pYb[0:n, :, 0:n]), r=[kY], w=[K_("ATb")])
                        yield
                        op("dve", lambda: V.tensor_tensor(out=TTb[0:n, :, 0:n], in0=identf[0:n, 0:n].unsqueeze(1).to_broadcast([n, 4, n]), in1=ATb[0:n, :, 0:n], op=ALU.subtract),
                           r=["identf", K_("ATb")], w=[K_("TTb")])
                        yield
                        Pc, PTc, Pn_, PTn_ = Ab, ATb, K_("Ab"), K_("ATb")
                        for lvl in range(1, 6):
                            Pd, PTd = B_(f"Pb{lvl % 2}"), B_(f"PTb{lvl % 2}")
                            Pdn, PTdn = K_(f"Pb{lvl % 2}"), K_(f"PTb{lvl % 2}")
                            for hi in range(4):
                                op("pe", lambda: PE.matmul(pX[0:n, hi, 0:n], lhsT=PTc[0:n, hi, 0:n], rhs=Pc[0:n, hi, 0:n], start=True, stop=True), r=[Pn_, PTn_], w=[kX])
                            if lvl < 5:
                                for hi in range(4):
                                    op("pe", lambda: PE.matmul(pY[0:n, hi, 0:n], lhsT=Pc[0:n, hi, 0:n], rhs=PTc[0:n, hi, 0:n], start=True, stop=True), r=[Pn_, PTn_], w=[kY])
                            op("act", lambda: A.copy(out=Pd[0:n, :, 0:n], in_=pX[0:n, :, 0:n]), r=[kX], w=[Pdn])
                            if lvl < 5:
                                op("act", lambda: A.copy(out=PTd[0:n, :, 0:n], in_=pY[0:n, :, 0:n]), r=[kY], w=[PTdn])
                            yield
                            for hi in range(4):
                                op("pe", lambda: PE.matmul(pX[0:n, hi, 0:n], lhsT=Pd[0:n, hi, 0:n], rhs=TTb[0:n, hi, 0:n], start=True, stop=True), r=[Pdn, K_("TTb")], w=[kX])
                            op("dve", lambda: V.tensor_tensor(out=TTb[0:n, :, 0:n], in0=TTb[0:n, :, 0:n], in1=pX[0:n, :, 0:n], op=ALU.add), r=[K_("TTb"), kX], w=[K_("TTb")])
                            yield
                            Pc, PTc, Pn_, PTn_ = Pd, PTd, Pdn, PTdn
                        op(ge, lambda: GE.tensor_tensor(out=vbt[0:n], in0=vtm[0:n], in1=bcol.unsqueeze(2).to_broadcast([n, 4, 128]), op=ALU.mult), r=[K_("vtm"), "gball"], w=[K_("vbt")])
                        op(ge, lambda: GE.tensor_tensor(out=kbg[0:n], in0=ktm[0:n], in1=bgc[0:n, :].unsqueeze(2).to_broadcast([n, 4, 128]), op=ALU.mult), r=[K_("ktm"), K_("bgc")], w=[K_("kbg")])
                        op(ge, lambda: GE.tensor_tensor(out=kdec[0:n], in0=ktm[0:n], in1=kds[0:n, :].unsqueeze(2).to_broadcast([n, 4, 128]), op=ALU.mult), r=[K_("ktm"), K_("kds")], w=[K_("kdec")])
                        op(ge, lambda: GE.tensor_tensor(out=qdT[:, :, 0:n], in0=qTl[:, :, 0:n], in1=egb[:, :, 0:n], op=ALU.mult), r=[qn, K_("egb")], w=[K_("qdT")])
                        yield
                        for hi in range(4):
                            op("pe", lambda: PE.matmul(pX[0:n, hi, :], lhsT=TTb[0:n, hi, 0:n], rhs=vbt[0:n, hi, :], start=True, stop=True), r=[K_("TTb"), K_("vbt")], w=[kX])
                        for hi in range(4):
                            op("pe", lambda: PE.matmul(pY[:, hi, 0:n], lhsT=kbg[0:n, hi, :], rhs=TTb[0:n, hi, 0:n], start=True, stop=True), r=[K_("TTb"), K_("kbg")], w=[kY])
                        op("act", lambda: A.copy(out=u_sb[0:n], in_=pX[0:n]), r=[kX], w=[K_("u_sb")])
                        op("act", lambda: A.mul(out=nwT[:, :, 0:n], in_=pY[:, :, 0:n], mul=-1.0), r=[kY], w=[K_("nwT")])
                        yield
                        for ci, (r0, cn) in enumerate(chunks):
                            rs_ = slice(r0, r0 + cn)
                            for hi in range(4):
                                op("pe", lambda: PE.matmul(pX[rs_, hi, :], lhsT=nwT[:, hi, rs_], rhs=Sb_[:, hi, :], start=True, stop=True), r=[K_("nwT"), K_("Sb_")], w=[kX])
                            op("dve", lambda: V.tensor_tensor(out=vnew[rs_], in0=u_sb[rs_], in1=pX[rs_], op=ALU.add), r=[K_("u_sb"), kX], w=[K_("vnew")])
                            yield
                            if t > 0:
                                for hi in range(4):
                                    op("pe", lambda: PE.matmul(pY[:, hi, 0:cn], lhsT=Sb_[:, hi, :], rhs=qdT[:, hi, rs_], start=True, stop=False), r=[K_("Sb_"), K_("qdT")], w=[kY])
                                    op("pe", lambda: PE.matmul(pY[:, hi, 0:cn], lhsT=vnew[rs_, hi, :], rhs=QKT[rs_, hi, rs_], start=False, stop=True), r=[K_("vnew"), K_("QKT")], w=[kY])
                                op("act", lambda: A.copy(out=oTt[:, :, rs_], in_=pY[:, :, 0:cn]), r=[kY], w=[K_("oTt")])
                            for hi in range(4):
                                op("pe", lambda: PE.matmul(pX[:, hi, :], lhsT=kdec[rs_, hi, :], rhs=vnew[rs_, hi, :], start=True, stop=True), r=[K_("kdec"), K_("vnew")], w=[kX])
                            op("dve", lambda: V.tensor_tensor(out=Sf[:], in0=Sf[:], in1=egb[:, :, ends[ci]].unsqueeze(2).to_broadcast([128, 4, 128]), op=ALU.mult),
                               r=[K_("Sf"), K_("egb")], w=[K_("Sf")])
                            op("dve", lambda: V.tensor_tensor(out=Sf[:], in0=Sf[:], in1=pX[:], op=ALU.add), r=[K_("Sf"), kX], w=[K_("Sf")])
                            op("act", lambda: A.copy(out=Sb_[:], in_=Sf[:]), r=[K_("Sf")], w=[K_("Sb_")])
                            yield
                        if t == 0:
                            continue
                        c0 = (t - 1) * 128
                        dma("sp", o_dst[t - 1][:, H0:H0 + 4, :], oTt[:], r=[K_("oTt")], w=[("osc", d_, hg, t)], key=K_("oTt"))
                        yield

                with contextlib.ExitStack() as st1:
                    pXs = [ps(f"p4X{i}", [128, 4, 128], F32, st1) for i in range(NSTR)]
                    pYs = [ps(f"p4Y{i}", [128, 4, 128], F32, st1) for i in range(NSTR)]
                    gens = [p4_stream(i, pXs[i], pYs[i]) for i in range(NSTR)]
                    alive = [True] * NSTR
                    nstep = 0
                    while any(alive):
                        for i in range(NSTR):
                            if alive[i]:
                                try:
                                    next(gens[i])
                                except StopIteration:
                                    alive[i] = False
                                nstep += 1
                                if nstep % 18 == 0:
                                    pc_step(1)
                    S.barrier()
            F8 = lambda nm, dt=F32: sb(nm, [128, 8, 128], dt, st)
            ofl = [F8(f"ofl{i}") for i in range(2)]
            obl = [F8(f"obl{i}") for i in range(2)]
            osq2 = [F8(f"osq{i}") for i in range(2)]
            ors2 = [F8(f"ors{i}") for i in range(2)]
            zall = sb("zall", [128, 8, SEQ], BF16, st)
            for h in range(8):
                dma("sp", zall[:, h, :], z_s[h], w=["zall"], key="zall")
            with contextlib.ExitStack() as st1:
                pss = [ps(f"pss{i}", [128, 4, 128], F32, st1) for i in range(4)]
                for t in (range(1, NT + 1) if os.environ.get('P4COMB', '1') == '1' else []):
                    c0 = (t - 1) * 128
                    i = t % 2
                    osq, ors, kosq, kors = osq2[i], ors2[i], f"osq{i}", f"ors{i}"
                    dma("sp", ofl[i][:], of_s[t - 1], r=[("osc", 0, 0, t), ("osc", 0, 1, t)], w=[f"ofl{i}"])
                    dma("sp", obl[i][:], ob_s[t - 1], r=[("osc", 1, 0, t), ("osc", 1, 1, t)], w=[f"obl{i}"])
                    op("dve", lambda: V.tensor_tensor(out=ofl[i][:], in0=ofl[i][:], in1=obl[i][:], op=ALU.add), r=[f"ofl{i}", f"obl{i}"], w=[f"ofl{i}"])
                    op("act", lambda: A.activation(out=osq[:], in_=ofl[i][:], func=AF.Square), r=[f"ofl{i}"], w=[kosq])
                    for hg in range(2):
                        pi = (2 * t + hg) % 4
                        op("pe", lambda: PE.matmul(pss[pi][:], lhsT=onesf[:], rhs=osq[:, hg * 4:hg * 4 + 4, :], start=True, stop=True), r=["onesf", kosq], w=[f"pss{pi}"])
                        op("act", lambda: A.activation(out=ors[:, hg * 4:hg * 4 + 4, :], in_=pss[pi][:], func=AF.Sqrt, scale=1.0 / 128, bias=epst[:]), r=[f"pss{pi}", "epst"], w=[kors])
                    op("dve", lambda: V.reciprocal(out=ors[:], in_=ors[:]), r=[kors], w=[kors])
                    op("dve", lambda: V.scalar_tensor_tensor(out=ofl[i][:], in0=ofl[i][:], scalar=gout[:, 0:1], in1=ors[:], op0=ALU.mult, op1=ALU.mult),
                       r=[f"ofl{i}", "gout", kors], w=[f"ofl{i}"])
                    op("pool", lambda: G.tensor_tensor(out=ogT[:, :, c0:c0 + 128], in0=ofl[i][:], in1=zall[:, :, c0:c0 + 128], op=ALU.mult), r=[f"ofl{i}", "zall"], w=[("ogT", t)])
                S.barrier()
            if debug:
                og_d = scratch("og_d", [NH, 128, SEQ], BF16)
                for h in range(8):
                    dma("sp", og_d[h], ogT[:, h, :], r=[("ogT", t) for t in range(1, NT + 1)], key="dbg_og")
            S.barrier()
        if stage < 4:
            S.finish()
            return nc, dbg
        h2_s = scratch("h2_s", [SEQ, D], F32)
        hn2_s = scratch("hn2_s", [SEQ, D], BF16)
        IOA = bass.IndirectOffsetOnAxis
        call = sb("call", [128, NT, 2], F32)
        dall = sb("dall", [128, NT, 2], I32)
        NB = 3
        wgu = [sb(f"wgu{i}", [128, 8, 512], BF16) for i in range(NB)]
        wde = [sb(f"wde{i}", [128, 2, D], BF16) for i in range(NB)]

        PRECAST = os.environ.get("PRECAST", "1") == "1"

        def load_expert(e):
            wi = e % NB
            if PRECAST:
                rk_ = [("wq", e // 2)]
                dma("pool", wgu[wi][:, :, 0:256], wq_g[e].rearrange("p (kt j) -> p kt j", kt=8), r=rk_, w=[f"wgu{wi}"], key=f"wgu{wi}")
                dma("pool", wgu[wi][:, :, 256:512], wq_u[e].rearrange("p (kt j) -> p kt j", kt=8), r=rk_, w=[f"wgu{wi}"], key=f"wgu{wi}")
                dma("pool", wde[wi][:], wq_d[e].rearrange("p (jt d) -> p jt d", jt=2), r=rk_, w=[f"wde{wi}"])
                return
            dma("pool", wgu[wi][:, :, 0:256], w_gate_e[0, e].rearrange("(p kt) j -> p kt j", kt=8), w=[f"wgu{wi}"], key=f"wgu{wi}")
            dma("pool", wgu[wi][:, :, 256:512], w_up_e[0, e].rearrange("(p kt) j -> p kt j", kt=8), w=[f"wgu{wi}"], key=f"wgu{wi}")
            dma("pool", wde[wi][:], w_down_e[0, e].rearrange("(jt p) d -> p jt d", p=128), w=[f"wde{wi}"])

        pc_step(1000)
        if stage >= 5:
            for e in range(NB):
                load_expert(e)
        with contextlib.ExitStack() as st:
            mrgT = sb("mrgT2", [128, 8, SEQ], BF16, st)
            for dt_ in range(8):
                dma("sp", mrgT[:, dt_, :], mrg_s[dt_], r=["mrg_s"], w=[("mrgT", dt_)], key=f"mrgT2_{dt_}")
            wd = sb("wd", [128, 8, D], BF16, st)
            wo = sb("wo", [128, 8, D], BF16, st)
            dma("pool", wd[:], w_delta[0].rearrange("(h p) d -> p h d", p=128), w=["wd"])
            dma("pool", wo[:], w_out[0].rearrange("(h p) d -> p h d", p=128), w=["wo"])
            sgd = [sb(f"sgd{i}", [128, SEQ], BF16, st) for i in range(2)]
            tmpm = sb("tmpm", [128, 512], F32, st)
            g2b = sb("g2b", [128, D], F32, st)
            dma("sp", g2b[:], norm2_g[0].partition_broadcast(128), w=["g2b"])
            wr = sb("wr", [128, 8, 72], F32, st)
            with nc.allow_non_contiguous_dma(reason="small router weights"):
                dma("sp", wr[:, :, 0:8], w_rg[0].rearrange("(kt p) g -> p kt g", p=128), w=["wr"], key="wr")
                dma("sp", wr[:, :, 8:72], w_re[0].rearrange("(kt p) g -> p kt g", p=128), w=["wr"], key="wr")
            rbias = sb("rbias", [128, 72], F32, st)
            dma("sp", rbias[:, 0:8], b_rg[0].partition_broadcast(128), w=["rbias"], key="rbias")
            dma("sp", rbias[:, 8:72], b_re[0].partition_broadcast(128), w=["rbias"], key="rbias")
            ustf = sb("ustf", [128, 128], F32, st)
            dma("sp", ustf[:], c_masks[6], w=["ustf"])
            ustb = sb("ustb", [128, 128], BF16, st)
            op("act", lambda: A.copy(out=ustb[:], in_=ustf[:]), r=["ustf"], w=["ustb"])
            onesb2 = sb("onesb2", [128, 128], BF16, st)
            op("dve", lambda: V.memset(onesb2[:], 1.0), w=["onesb2"])
            ecap = sb("ecap", [128, 64], F32, st)
            dma("sp", ecap[:], c_ecap, w=["ecap"])
            Mall = sb("Mall", [128, NT, 64], BF16, st)
            lgall = sb("lgall", [128, NT, 72], F32, st)
            sm = {"ss": sb("sm_ss", [128, 1], F32, st)}
            stA = contextlib.ExitStack()
            xr = [sb(f"xr{i}", [128, D], F32, stA) for i in range(2)]
            h2t = [sb(f"h2t{i}", [128, D], F32, stA) for i in range(2)]
            hn2f2 = [sb(f"hn2f{i}", [128, D], F32, stA) for i in range(2)]
            hn2b = [sb(f"hn2b{i}", [128, D], BF16, stA) for i in range(2)]
            junk2 = sb("junk2", [128, D], BF16, stA)
            hn2T = sb("hn2T", [128, 8, 128], F32, stA)
            with contextlib.ExitStack() as st1:
                pa = [ps(f"pb{i}", [128, 512], F32, st1) for i in range(3)]
                ptf = [ps(f"ptf{i}", [128, 4, 128], F32, st1) for i in range(2)]
                plg = ps("plg", [128, 72], F32, st1)
                prk2 = [ps(f"prk{i}", [128, 8, 64], F32, st1) for i in range(2)]
                npa = 0
                for dt_ in range(8):
                    gi_ = dt_ % 2
                    dma("sp", sgd[gi_][:], gd_s[dt_], w=[f"sgd{gi_}"])
                    for bi in range(4):
                        ai = npa % 3
                        npa += 1
                        for h in range(8):
                            op("pe", lambda: PE.matmul(pa[ai][:, :], lhsT=wd[:, h, dt_ * 128:(dt_ + 1) * 128], rhs=ogT[:, h, bi * 512:(bi + 1) * 512],
                                                       start=(h == 0), stop=(h == 7)),
                               r=["wd"] + [("ogT", t) for t in range(1, NT + 1)], w=[f"pb{ai}"])
                        op("dve", lambda: V.tensor_tensor(out=tmpm[:], in0=pa[ai][:, :], in1=sgd[gi_][:, bi * 512:(bi + 1) * 512], op=ALU.mult),
                           r=[f"pb{ai}", f"sgd{gi_}"], w=["tmpm"])
                        op("dve", lambda: V.tensor_tensor(out=mrgT[:, dt_, bi * 512:(bi + 1) * 512], in0=tmpm[:], in1=mrgT[:, dt_, bi * 512:(bi + 1) * 512], op=ALU.add),
                           r=["tmpm", ("mrgT", dt_)], w=[("mrgT", dt_)])
                mrg_all = [("mrgT", d2) for d2 in range(8)]
                def LA(t):
                    i = t % 2
                    hn2f = hn2f2[i]
                    nonlocal_npa = None
                    dma("sp", xr[i][:], x[t * 128:(t + 1) * 128, :], w=[f"xr{i}"])
                    for half in range(2):
                        ai = cntA[0] % 3
                        cntA[0] += 1
                        for dt_ in range(8):
                            op("pe", lambda: PE.matmul(pa[ai][:, :], lhsT=mrgT[:, dt_, t * 128:(t + 1) * 128], rhs=wo[:, dt_, half * 512:(half + 1) * 512],
                                                       start=(dt_ == 0), stop=(dt_ == 7)), r=["wo"] + mrg_all, w=[f"pb{ai}"])
                        op("dve", lambda: V.tensor_tensor(out=h2t[i][:, half * 512:(half + 1) * 512], in0=pa[ai][:, :], in1=xr[i][:, half * 512:(half + 1) * 512], op=ALU.add),
                           r=[f"pb{ai}", f"xr{i}"], w=[f"h2t{i}"])
                    dma("sp", h2_s[t * 128:(t + 1) * 128, :], h2t[i][:], r=[f"h2t{i}"], w=["h2_s"], key="h2_s")
                    op("act", lambda: A.activation(out=junk2[:], in_=h2t[i][:], func=AF.Square, accum_out=sm["ss"][:]), r=[f"h2t{i}"], w=["junk2", "sm_ss"])
                    op("act", lambda: A.activation(out=sm["ss"][:], in_=sm["ss"][:], func=AF.Sqrt, scale=1.0 / D, bias=epst[:]), r=["sm_ss", "epst"], w=["sm_ss"])
                    op("dve", lambda: V.reciprocal(out=sm["ss"][:], in_=sm["ss"][:]), r=["sm_ss"], w=["sm_ss"])
                    op("dve", lambda: V.scalar_tensor_tensor(out=hn2f[:], in0=h2t[i][:], scalar=sm["ss"][:, 0:1], in1=g2b[:], op0=ALU.mult, op1=ALU.mult),
                       r=[f"h2t{i}", "sm_ss", "g2b"], w=[f"hn2f{i}"])
                    op("act", lambda: A.copy(out=hn2b[i][:], in_=hn2f[:]), r=[f"hn2f{i}"], w=[f"hn2b{i}"])
                    dma("sp", hn2_s[t * 128:(t + 1) * 128, :], hn2b[i][:], r=[f"hn2b{i}"], w=["hn2_s"], key=f"hn2b{i}")

                def LB(t):
                    i = t % 2
                    hn2f = hn2f2[i]
                    for kt in range(8):
                        op("pe", lambda: PE.transpose(ptf[kt // 4][:, kt % 4, :], hn2f[:, kt * 128:(kt + 1) * 128], identf[:]), r=[f"hn2f{i}", "identf"], w=[f"ptf{kt // 4}"])
                    for q_ in range(2):
                        op("act", lambda: A.copy(out=hn2T[:, q_ * 4:q_ * 4 + 4, :], in_=ptf[q_][:]), r=[f"ptf{q_}"], w=["hn2T"])
                    for kt in range(8):
                        op("pe", lambda: PE.matmul(plg[:, :], lhsT=hn2T[:, kt, :], rhs=wr[:, kt, :], start=(kt == 0), stop=(kt == 7)), r=["hn2T", "wr"], w=["plg"])
                    op("dve", lambda: V.tensor_tensor(out=lgall[:, t, :], in0=plg[:, :], in1=rbias[:], op=ALU.add), r=["plg", "rbias"], w=["lgall"])

                cntA = [npa]
                for t in range(NT + 1):
                    if t < NT:
                        LA(t)
                    if t >= 1:
                        LB(t - 1)
                S.barrier()
                stA.close()
                hn2b = [sb(f"hn2c{i}", [128, D], BF16, st) for i in range(2)]
                TT_ = lambda o, a, b, o_: V.tensor_tensor(out=o, in0=a, in1=b, op=o_)
                R = {nm: sb("rt_" + nm, [128, NT, w_], F32, st) for nm, w_ in
                     (("gmax", 1), ("ge", 8), ("gsum", 1), ("pg", 1), ("ohg", 8), ("tmp", 64), ("elg", 8), ("m1", 1), ("oh1", 8), ("el2", 8),
                      ("m2", 1), ("oh2", 8), ("d12", 1), ("w1", 1), ("M1", 64), ("M2", 64), ("rk", 64), ("t3", 64), ("d1f", 1), ("d2f", 1))}
                k = lambda nm: "rt_" + nm
                lgg = lgall[:, :, 0:8]
                op("dve", lambda: V.tensor_reduce(out=R["gmax"][:, :, 0], in_=lgg, axis=AX.X, op=ALU.max), r=["lgall"], w=[k("gmax")])
                op("dve", lambda: TT_(R["ge"][:], lgg, R["gmax"][:].to_broadcast([128, NT, 8]), ALU.subtract), r=["lgall", k("gmax")], w=[k("ge")])
                op("act", lambda: A.activation(out=R["ge"][:], in_=R["ge"][:], func=AF.Exp), r=[k("ge")], w=[k("ge")])
                op("dve", lambda: V.tensor_reduce(out=R["gsum"][:, :, 0], in_=R["ge"][:], axis=AX.X, op=ALU.add), r=[k("ge")], w=[k("gsum")])
                op("dve", lambda: V.reciprocal(out=R["pg"][:], in_=R["gsum"][:]), r=[k("gsum")], w=[k("pg")])
                op("dve", lambda: TT_(R["ohg"][:], lgg, R["gmax"][:].to_broadcast([128, NT, 8]), ALU.is_equal), r=["lgall", k("gmax")], w=[k("ohg")])
                el4 = lgall[:, :, 8:72].rearrange("p t (g e) -> p t g e", g=8)
                tmp4 = R["tmp"][:].rearrange("p t (g e) -> p t g e", g=8)
                op("dve", lambda: TT_(tmp4, el4, R["ohg"][:].unsqueeze(3).to_broadcast([128, NT, 8, 8]), ALU.mult), r=["lgall", k("ohg")], w=[k("tmp")])
                op("dve", lambda: V.tensor_reduce(out=R["elg"][:], in_=tmp4.rearrange("p t g e -> p t e g"), axis=AX.X, op=ALU.add), r=[k("tmp")], w=[k("elg")])
                op("dve", lambda: V.tensor_reduce(out=R["m1"][:, :, 0], in_=R["elg"][:], axis=AX.X, op=ALU.max), r=[k("elg")], w=[k("m1")])
                op("dve", lambda: TT_(R["oh1"][:], R["elg"][:], R["m1"][:].to_broadcast([128, NT, 8]), ALU.is_equal), r=[k("elg"), k("m1")], w=[k("oh1")])
                op("dve", lambda: V.scalar_tensor_tensor(out=R["el2"][:], in0=R["oh1"][:], scalar=-1.0e30, in1=R["elg"][:], op0=ALU.mult, op1=ALU.add),
                   r=[k("oh1"), k("elg")], w=[k("el2")])
                op("dve", lambda: V.tensor_reduce(out=R["m2"][:, :, 0], in_=R["el2"][:], axis=AX.X, op=ALU.max), r=[k("el2")], w=[k("m2")])
                op("dve", lambda: TT_(R["oh2"][:], R["el2"][:], R["m2"][:].to_broadcast([128, NT, 8]), ALU.is_equal), r=[k("el2"), k("m2")], w=[k("oh2")])
                op("dve", lambda: TT_(R["d12"][:], R["m1"][:], R["m2"][:], ALU.subtract), r=[k("m1"), k("m2")], w=[k("d12")])
                op("act", lambda: A.activation(out=R["w1"][:], in_=R["d12"][:], func=AF.Sigmoid), r=[k("d12")], w=[k("w1")])
                op("dve", lambda: TT_(call[:, :, 0:1], R["pg"][:], R["w1"][:], ALU.mult), r=[k("pg"), k("w1")], w=["call"])
                op("dve", lambda: TT_(call[:, :, 1:2], R["pg"][:], call[:, :, 0:1], ALU.subtract), r=[k("pg"), "call"], w=["call"])
                for (Mn, ohn) in (("M1", "oh1"), ("M2", "oh2")):
                    op("dve", lambda: TT_(R[Mn][:].rearrange("p t (g e) -> p t g e", g=8), R["ohg"][:].unsqueeze(3).to_broadcast([128, NT, 8, 8]),
                                          R[ohn][:].unsqueeze(2).to_broadcast([128, NT, 8, 8]), ALU.mult), r=[k("ohg"), k(ohn)], w=[k(Mn)])
                op("dve", lambda: TT_(Mall[:], R["M1"][:], R["M2"][:], ALU.add), r=[k("M1"), k("M2")], w=["Mall"])
                for t in range(NT):
                    pr = prk2[t // 8]
                    prn = f"prk{t // 8}"
                    op("pe", lambda: PE.matmul(pr[:, t % 8, :], lhsT=ustb[:], rhs=Mall[:, t, :], start=True, stop=(t == 0)), r=["ustb", "Mall"], w=[prn])
                    for j in range(t):
                        op("pe", lambda: PE.matmul(pr[:, t % 8, :], lhsT=onesb2[:], rhs=Mall[:, j, :], start=False, stop=(j == t - 1)), r=["onesb2", "Mall"], w=[prn])
                for q_ in range(2):
                    op("dve", lambda: TT_(R["rk"][:, q_ * 8:q_ * 8 + 8, :], prk2[q_][:], ecap[:].unsqueeze(1).to_broadcast([128, 8, 64]), ALU.add), r=[f"prk{q_}", "ecap"], w=[k("rk")])
                for (Mn, dn, ci_) in (("M1", "d1f", 0), ("M2", "d2f", 1)):
                    op("dve", lambda: TT_(R["t3"][:], R["rk"][:], R[Mn][:], ALU.mult), r=[k("rk"), k(Mn)], w=[k("t3")])
                    op("dve", lambda: V.tensor_reduce(out=R[dn][:, :, 0], in_=R["t3"][:], axis=AX.X, op=ALU.add), r=[k("t3")], w=[k(dn)])
                    op("dve", lambda: V.tensor_copy(out=dall[:, :, ci_:ci_ + 1], in_=R[dn][:]), r=[k(dn)], w=["dall"])
                for t in range(NT):
                    i = t % 2
                    dma("sp", hn2b[i][:], hn2_s[t * 128:(t + 1) * 128, :], r=["hn2_s"], w=[f"hn2c{i}"])
                    for ci_ in range(2):
                        dma("pool", None, None, r=[f"hn2c{i}", "dall"], w=["Xs"], key="Xs",
                            indirect=lambda: G.indirect_dma_start(out=Xs[:, :], out_offset=IOA(ap=dall[:, t, ci_:ci_ + 1], axis=0), in_=hn2b[i][:], in_offset=None))
                S.barrier()
            S.barrier()
        if stage < 5:
            S.finish()
            return nc, dbg
        with contextlib.ExitStack() as st:
            Xe = [sb(f"Xe{i}", [128, D], BF16, st) for i in range(2)]
            XeT2 = [sb(f"XeT{i}", [128, 8, 128], BF16, st) for i in range(2)]
            sg2 = [sb(f"sg{i}", [128, 256], F32, st) for i in range(2)]
            actb2 = [sb(f"actb{i}", [128, 256], BF16, st) for i in range(2)]
            actT2 = [sb(f"actT{i}", [128, 2, 128], BF16, st) for i in range(2)]
            Ye = [sb(f"Ye{i}", [128, D], F32, st) for i in range(2)]
            gfb = sb("gfb", [128, D], F32, st)
            dma("sp", gfb[:], final_g.partition_broadcast(128), w=["gfb"])
            ya2 = [sb(f"ya{i}", [128, D], F32, st) for i in range(4)]
            yb2 = [sb(f"yb{i}", [128, D], F32, st) for i in range(4)]
            hh = [sb(f"hh{i}", [128, D], F32, st) for i in range(2)]
            oo = [sb(f"oo{i}", [128, D], F32, st) for i in range(2)]
            junk3 = sb("junk3", [128, D], BF16, st)
            ssf = sb("ssf", [128, 1], F32, st)
            with contextlib.ExitStack() as st1:
                ptx = ps("ptx", [128, 8, 128], BF16, st1)
                pta = ps("pta", [128, 8, 128], BF16, st1)
                ph = [ps(f"ph{i}", [128, 512], F32, st1) for i in range(2)]
                pyy = [ps(f"pyy{i}", [128, 512], F32, st1) for i in range(2)]
                def expA(e):
                    wi, xi, pi = e % NB, e % 2, e % 2
                    if e == 0:
                        dma("sp", Xe[0][:], Xs[0:CAP, :], r=["Xs"], w=["Xe0"])
                    if e + 1 < NEXP:
                        dma("sp", Xe[1 - xi][:], Xs[(e + 1) * CAP:(e + 2) * CAP, :], r=["Xs"], w=[f"Xe{1 - xi}"])
                    for kt in range(8):
                        op("pe", lambda: PE.transpose(ptx[:, kt, :], Xe[xi][:].rearrange("s (p k) -> s k p", k=8)[:, kt, :], identb[:]), r=[f"Xe{xi}", "identb"], w=["ptx"])
                    op("act", lambda: A.copy(out=XeT2[xi][:], in_=ptx[:]), r=["ptx"], w=[f"XeT{xi}"])
                    for kt in range(8):
                        op("pe", lambda: PE.matmul(ph[pi][:, :], lhsT=XeT2[xi][:, kt, :], rhs=wgu[wi][:, kt, :], start=(kt == 0), stop=(kt == 7)), r=[f"XeT{xi}", f"wgu{wi}"], w=[f"ph{pi}"])

                def expB(e):
                    wi, xi, pi = e % NB, e % 2, e % 2
                    op("act", lambda: A.activation(out=sg2[xi][:], in_=ph[pi][:, 0:256], func=AF.Silu), r=[f"ph{pi}"], w=[f"sg{xi}"])
                    op("dve", lambda: V.tensor_tensor(out=actb2[xi][:], in0=sg2[xi][:], in1=ph[pi][:, 256:512], op=ALU.mult), r=[f"sg{xi}", f"ph{pi}"], w=[f"actb{xi}"])
                    for jt in range(2):
                        op("pe", lambda: PE.transpose(pta[:, jt, :], actb2[xi][:, jt * 128:(jt + 1) * 128], identb[:]), r=[f"actb{xi}", "identb"], w=["pta"])
                    op("act", lambda: A.copy(out=actT2[xi][:], in_=pta[:, 0:2, :]), r=["pta"], w=[f"actT{xi}"])
                    for half in range(2):
                        for jt in range(2):
                            op("pe", lambda: PE.matmul(pyy[half][:, :], lhsT=actT2[xi][:, jt, :], rhs=wde[wi][:, jt, half * 512:(half + 1) * 512], start=(jt == 0), stop=(jt == 1)),
                               r=[f"actT{xi}", f"wde{wi}"], w=[f"pyy{half}"])
                    op("act", lambda: A.copy(out=Ye[xi][:, 0:512], in_=pyy[0][:, :]), r=["pyy0"], w=[f"Ye{xi}"])
                    op("dve", lambda: V.tensor_copy(out=Ye[xi][:, 512:1024], in_=pyy[1][:, :]), r=["pyy1"], w=[f"Ye{xi}"])
                    dma("act", Ys[e * CAP:(e + 1) * CAP, :], Ye[xi][:], r=[f"Ye{xi}"], w=["Ys"], key="Ys")
                    if e + NB < NEXP:
                        load_expert(e + NB)

                for e in range(NEXP + 1):
                    if e < NEXP:
                        expA(e)
                    if e >= 1:
                        expB(e - 1)
                for t in range(NT):
                    i = t % 2
                    ya, yb, kya, kyb = ya2[t % 4], yb2[t % 4], f"ya{t % 4}", f"yb{t % 4}"
                    dma("sp", hh[i][:], h2_s[t * 128:(t + 1) * 128, :], r=["h2_s"], w=[f"hh{i}"])
                    dma("pool", None, None, r=["Ys", "dall"], w=[kya], key=kya,
                        indirect=lambda: G.indirect_dma_start(out=ya[:], out_offset=None, in_=Ys[:, :], in_offset=IOA(ap=dall[:, t, 0:1], axis=0)))
                    dma("pool", None, None, r=["Ys", "dall"], w=[kyb], key=kyb,
                        indirect=lambda: G.indirect_dma_start(out=yb[:], out_offset=None, in_=Ys[:, :], in_offset=IOA(ap=dall[:, t, 1:2], axis=0)))
                    op("dve", lambda: V.scalar_tensor_tensor(out=hh[i][:], in0=ya[:], scalar=call[:, t, 0:1], in1=hh[i][:], op0=ALU.mult, op1=ALU.add),
                       r=[kya, "call", f"hh{i}"], w=[f"hh{i}"])
                    op("dve", lambda: V.scalar_tensor_tensor(out=hh[i][:], in0=yb[:], scalar=call[:, t, 1:2], in1=hh[i][:], op0=ALU.mult, op1=ALU.add),
                       r=[kyb, "call", f"hh{i}"], w=[f"hh{i}"])
                    op("act", lambda: A.activation(out=junk3[:], in_=hh[i][:], func=AF.Square, accum_out=ssf[:]), r=[f"hh{i}"], w=["junk3", "ssf"])
                    op("act", lambda: A.activation(out=ssf[:], in_=ssf[:], func=AF.Sqrt, scale=1.0 / D, bias=epst[:]), r=["ssf", "epst"], w=["ssf"])
                    op("dve", lambda: V.reciprocal(out=ssf[:], in_=ssf[:]), r=["ssf"], w=["ssf"])
                    op("dve", lambda: V.scalar_tensor_tensor(out=oo[i][:], in0=hh[i][:], scalar=ssf[:, 0:1], in1=gfb[:], op0=ALU.mult, op1=ALU.mult),
                       r=[f"hh{i}", "ssf", "gfb"], w=[f"oo{i}"])
                    dma("sp", out[t * 128:(t + 1) * 128, :], oo[i][:], r=[f"oo{i}"], key=f"oo{i}")
                S.barrier()
            S.barrier()
        S.finish()
    return nc, dbg


def host_consts():
    c = {}
    c["c_identb"] = np.eye(128, dtype=np.float32).astype(ml_dtypes.bfloat16)
    c["c_identf"] = np.eye(128, dtype=np.float32)
    p = np.arange(L, dtype=np.float64)
    ang = 2.0 * np.pi * ((p[:, None] * p[None, :]) % L) / L
    c["c_cosL"] = np.cos(ang).astype(np.float32).astype(ml_dtypes.bfloat16)
    c["c_nsinL"] = (-np.sin(ang)).astype(np.float32).astype(ml_dtypes.bfloat16)
    q = np.arange(128, dtype=np.float64)
    a2 = 2.0 * np.pi * ((q[:, None] * q[None, :]) % 128) / 128
    sc = 1.0 / np.sqrt(L * 128.0)
    c["c_cs128"] = np.concatenate([np.cos(a2) * sc, np.sin(a2) * sc], axis=1).astype(np.float32).astype(ml_dtypes.bfloat16)
    c["c_masks"] = make_masks()
    c["c_ecap"] = np.tile((np.arange(64, dtype=np.float32) * CAP)[None, :], (128, 1))
    return c


def make_masks():
    i = np.arange(128)
    same = (i[:, None] // 64) == (i[None, :] // 64)
    m = np.zeros((7, 128, 128), np.float32)
    m[0] = (same & (i[:, None] <= i[None, :])).astype(np.float32)
    m[1] = (same & (i[:, None] >= i[None, :])).astype(np.float32)
    BIG = 30000.0
    m[2] = np.where(same & (i[None, :] < i[:, None]), 0.0, -BIG)
    m[3] = np.where(same & (i[None, :] > i[:, None]), 0.0, -BIG)
    m[4] = np.where(same & (i[:, None] <= i[None, :]), 0.0, BIG)
    m[5] = np.where(same & (i[:, None] >= i[None, :]), 0.0, BIG)
    m[6] = (i[:, None] < i[None, :]).astype(np.float32)
    return m


_CACHE = {}
PARAM_NAMES = ["meta_tokens", "norm1_g", "w_in", "conv_w", "a_log_fwd", "dt_bias_fwd", "a_log_bwd", "dt_bias_bwd",
               "out_norm_g", "w_fourier", "w_delta", "w_out", "norm2_g", "w_router_group", "b_router_group",
               "w_router_expert", "b_router_expert", "w_gate_e", "w_up_e", "w_down_e", "final_norm_g"]


def core_inputs(inputs, b):
    m = {"x": np.ascontiguousarray(np.asarray(inputs["x"])[b], dtype=np.float32)}
    for k in PARAM_NAMES:
        m[k] = np.ascontiguousarray(np.asarray(inputs[k]), dtype=np.float32)
    return m


def kernel(**inputs):
    if "nc" not in _CACHE:
        _CACHE["nc"] = build()[0]
        _CACHE["consts"] = host_consts()
    nc = _CACHE["nc"]
    maps = []
    for b in range(8):
        m = core_inputs(inputs, b)
        m.update(_CACHE["consts"])
        maps.append(m)
    res = run_bass_kernel_spmd(nc, maps, core_ids=list(range(8)))
    return np.stack([np.asarray(r["out"], dtype=np.float32) for r in res.results], axis=0)
```

```python
import contextlib
import os
import numpy as np
import ml_dtypes
import concourse.bass as bass
import concourse.mybir as mybir
from concourse.bass_utils import run_bass_kernel_spmd

F32 = mybir.dt.float32
BF16 = mybir.dt.bfloat16
I32 = mybir.dt.int32
AF = mybir.ActivationFunctionType
ALU = mybir.AluOpType
AX = mybir.AxisListType

D = 1024
NM = 16
SEQ = 2048
L = NM + SEQ
NH = 8
INW = 6688
EPS = 1e-6
NEXP = 64
CAP = 128
DE = 256
NT = SEQ // 128


class Sched:
    def __init__(self, nc):
        self.nc = nc
        self.eng = {"pe": nc.tensor, "act": nc.scalar, "dve": nc.vector, "pool": nc.gpsimd, "sp": nc.sync}
        self.sem = {e: nc.alloc_semaphore(f"s_{e}") for e in self.eng}
        self.cnt = {e: 0 for e in self.eng}
        self.seen = {e: {} for e in self.eng}
        self.semobj = {}
        self.bufs = {}
        self.nsem = 0
        self.excl = set()
        self.free_sems = []

    def _wait(self, e, ev):
        sem, val = ev
        if self.seen[e].get(sem.name, 0) >= val:
            return
        self.eng[e].wait_ge(sem, val)
        self.seen[e][sem.name] = val

    def deps(self, e, reads, writes):
        evs = []
        own = self.sem[e]
        for b in reads:
            st = self.bufs.get(b)
            if st and st["w"]:
                evs.append(st["w"])
            if st and b in self.excl:
                for r in st["r"]:
                    if r[0] is not own:
                        evs.append(r)
        for b in writes:
            st = self.bufs.get(b)
            if st:
                if st["w"]:
                    evs.append(st["w"])
                for r in st["r"]:
                    if r[0] is own:
                        continue
                    evs.append(r)
        best = {}
        for sem, val in evs:
            if e == "pe" and sem is own:
                continue
            if sem.name not in best or best[sem.name][1] < val:
                best[sem.name] = (sem, val)
        for ev in best.values():
            self._wait(e, ev)

    def record(self, ev, reads, writes):
        for b in reads:
            st = self.bufs.setdefault(b, {"w": None, "r": []})
            st["r"] = [r for r in st["r"] if r[0] is not ev[0]] + [ev]
        for b in writes:
            self.bufs[b] = {"w": ev, "r": []}

    def op(self, e, fn, r=(), w=()):
        self.deps(e, r, w)
        ins = fn()
        self.cnt[e] += 1
        ins.then_inc(self.sem[e], 1)
        ev = (self.sem[e], self.cnt[e])
        self.record(ev, r, w)
        return ev

    def dma(self, e, out, in_, r=(), w=(), key=None, indirect=None, **kw):
        self.deps(e, r, w)
        key = key or (w[0] if w else r[0])
        if key not in self.semobj:
            if self.free_sems:
                self.semobj[key] = self.free_sems.pop()
            else:
                self.semobj[key] = [self.nc.alloc_semaphore(f"d{self.nsem}"), 0]
                self.nsem += 1
        so = self.semobj[key]
        if indirect is None:
            ins = self.eng[e].dma_start(out=out, in_=in_, **kw)
        else:
            ins = indirect()
        so[1] += 16
        ins.then_inc(so[0], 16)
        ev = (so[0], so[1])
        self.record(ev, r, w)
        return ev

    def barrier(self):
        allev = {}
        for st in self.bufs.values():
            for ev in ([st["w"]] if st["w"] else []) + st["r"]:
                if ev[0].name not in allev or allev[ev[0].name][1] < ev[1]:
                    allev[ev[0].name] = ev
        for e in self.eng:
            for ev in allev.values():
                self._wait(e, ev)
        self.free_sems.extend(self.semobj.values())
        self.semobj = {}

    def finish(self):
        allev = {}
        for st in self.bufs.values():
            for ev in ([st["w"]] if st["w"] else []) + st["r"]:
                if ev[0].name not in allev or allev[ev[0].name][1] < ev[1]:
                    allev[ev[0].name] = ev
        for ev in allev.values():
            self._wait("sp", ev)


def col_tiles():
    tl = []
    for g in range(4):
        tl.append(("f", g, g * 128))
    for h in range(NH):
        tl.append(("q", h, 512 + h * 128))
    for h in range(NH):
        tl.append(("k", h, 1536 + h * 128))
    for h in range(NH):
        tl.append(("v", h, 2560 + h * 128))
    for h in range(NH):
        tl.append(("z", h, 3584 + h * 128))
    for j in range(8):
        tl.append(("gf", j, 4640 + j * 128))
    for j in range(8):
        tl.append(("gd", j, 5664 + j * 128))
    return tl


PBLK = [(0, 512), (512, 512), (1024, 512), (1536, 512), (2048, 16)]
RBLK = [(16 + 512 * i, 512) for i in range(4)]


def build(stage=99, debug=False):
    nc = bass.Bass("TRN2", target_bir_lowering=False)
    S = Sched(nc)
    global LAST_SCHED
    LAST_SCHED = S
    op, dma = S.op, S.dma
    V, A, PE, G = nc.vector, nc.scalar, nc.tensor, nc.gpsimd

    def din(name, shape, dt=F32):
        return nc.dram_tensor(name, list(shape), dt, kind="ExternalInput").ap()

    x = din("x", [SEQ, D])
    meta = din("meta_tokens", [NM, D])
    norm1_g = din("norm1_g", [1, D])
    w_in = din("w_in", [1, D, INW])
    conv_w = din("conv_w", [1, 5, 3072])
    a_log_f = din("a_log_fwd", [1, 8]); dt_b_f = din("dt_bias_fwd", [1, 8])
    a_log_b = din("a_log_bwd", [1, 8]); dt_b_b = din("dt_bias_bwd", [1, 8])
    out_norm_g = din("out_norm_g", [1, 128])
    w_fourier = din("w_fourier", [1, 512, D])
    w_delta = din("w_delta", [1, D, D])
    w_out = din("w_out", [1, D, D])
    norm2_g = din("norm2_g", [1, D])
    w_rg = din("w_router_group", [1, D, 8]); b_rg = din("b_router_group", [1, 8])
    w_re = din("w_router_expert", [1, D, 64]); b_re = din("b_router_expert", [1, 64])
    w_gate_e = din("w_gate_e", [1, NEXP, D, DE]); w_up_e = din("w_up_e", [1, NEXP, D, DE])
    w_down_e = din("w_down_e", [1, NEXP, DE, D])
    final_g = din("final_norm_g", [D])
    c_identb = din("c_identb", [128, 128], BF16)
    c_identf = din("c_identf", [128, 128], F32)
    c_cosL = din("c_cosL", [L, L], BF16)
    c_nsinL = din("c_nsinL", [L, L], BF16)
    c_cs128 = din("c_cs128", [128, 256], BF16)
    c_masks = din("c_masks", [7, 128, 128], F32)
    c_ecap = din("c_ecap", [128, 64], F32)

    out = nc.dram_tensor("out", [SEQ, D], F32, kind="ExternalOutput").ap()

    dbg = {}

    def scratch(name, shape, dt):
        kind = "ExternalOutput" if debug else "Internal"
        t = nc.dram_tensor(name, list(shape), dt, kind=kind).ap()
        dbg[name] = t
        return t

    qT_s = scratch("qT_s", [NH, 128, L], BF16)
    kT_s = scratch("kT_s", [NH, 128, L], BF16)
    vT_s = scratch("vT_s", [NH, 128, L], BF16)
    z_s = scratch("z_s", [NH, 128, SEQ], BF16)
    gf_s = scratch("gf_s", [8, 128, SEQ], BF16)
    gd_s = scratch("gd_s", [8, 128, SEQ], BF16)
    ab_s = scratch("ab_s", [L, 32], F32)
    fin_s = scratch("fin_s", [4, 128, L], BF16)

    wq_g = nc.dram_tensor("wq_g", [NEXP, 128, 8 * DE], BF16, kind="Internal").ap()
    wq_u = nc.dram_tensor("wq_u", [NEXP, 128, 8 * DE], BF16, kind="Internal").ap()
    wq_d = nc.dram_tensor("wq_d", [NEXP, 128, 2 * D], BF16, kind="Internal").ap()

    def precast_gen():
        for e in range(NEXP):
            kk_ = ("pc", e // 2)
            dma("pool", wq_g[e], w_gate_e[0, e].rearrange("(p kt) j -> p (kt j)", kt=8), w=[("wq", e // 2)], key=kk_)
            yield
            dma("pool", wq_u[e], w_up_e[0, e].rearrange("(p kt) j -> p (kt j)", kt=8), w=[("wq", e // 2)], key=kk_)
            yield
            dma("pool", wq_d[e].rearrange("p (jt d) -> p jt d", jt=2), w_down_e[0, e].rearrange("(jt p) d -> p jt d", p=128), w=[("wq", e // 2)], key=kk_)
            yield

    pcg = precast_gen() if (stage >= 5 and os.environ.get("PRECAST", "1") == "1") else iter(())

    def pc_step(k=1):
        for _ in range(k):
            next(pcg, None)

    with contextlib.ExitStack() as gst:
        def sb(name, shape, dt, st=gst):
            return st.enter_context(nc.sbuf_tensor(name, list(shape), dt))

        def ps(name, shape, dt, st=gst):
            S.excl.add(name)
            return st.enter_context(nc.psum_tensor(name, list(shape), dt))

        identb = sb("identb", [128, 128], BF16)
        identf = sb("identf", [128, 128], F32)
        epst = sb("epst", [128, 1], F32)
        dma("sp", identb[:], c_identb, w=["identb"])
        dma("sp", identf[:], c_identf, w=["identf"])
        op("dve", lambda: V.memset(epst[:], EPS), w=["epst"])
        Xs = nc.dram_tensor("Xs", [NEXP * CAP, D], BF16, kind="Internal").ap()
        Ys = nc.dram_tensor("Ys", [NEXP * CAP, D], F32, kind="Internal").ap()
        zfill = sb("zfill", [128, 2048], BF16)
        op("dve", lambda: V.memset(zfill[:], 0.0), w=["zfill"])
        Xs_v = Xs.rearrange("(p a) c -> p (a c)", p=128)
        for i in range(32):
            dma("pool", Xs_v[:, i * 2048:(i + 1) * 2048], zfill[:], r=["zfill"], w=["Xs"], key="Xs")

        with contextlib.ExitStack() as st:
            hnT = sb("hnT", [128, 8, L], BF16, st)
            stP1 = contextlib.ExitStack()
            g1b = sb("g1b", [128, D], F32, stP1)
            dma("sp", g1b[:], norm1_g[0].partition_broadcast(128), w=["g1b"])
            xt = [sb(f"xt{i}", [128, D], F32, stP1) for i in range(2)]
            junk = sb("junk", [128, D], BF16, stP1)
            hnb = [sb(f"hnb{i}", [128, D], BF16, stP1) for i in range(2)]
            ss = [sb(f"ss{i}", [128, 1], F32, stP1) for i in range(2)]
            with contextlib.ExitStack() as st1:
                tp = [ps(f"tp{i}", [128, 8, 128], BF16, st1) for i in range(2)]

                for t in (range(NT + 1) if 'KT' not in os.environ else [int(v) for v in os.environ['KT'].split(',')]):
                    i = t % 2
                    if t == 0:
                        n, src, p0 = NM, meta, 0
                    else:
                        n, src, p0 = 128, x[(t - 1) * 128:t * 128, :], NM + (t - 1) * 128
                    dma("sp", xt[i][0:n, :], src, w=[f"xt{i}"])
                    op("act", lambda: A.activation(out=junk[0:n, :], in_=xt[i][0:n, :], func=AF.Square, accum_out=ss[i][0:n, :]),
                       r=[f"xt{i}"], w=["junk", f"ss{i}"])
                    op("act", lambda: A.activation(out=ss[i][0:n, :], in_=ss[i][0:n, :], func=AF.Sqrt, scale=1.0 / D, bias=epst[0:n, :]),
                       r=[f"ss{i}", "epst"], w=[f"ss{i}"])
                    op("dve", lambda: V.reciprocal(out=ss[i][0:n, :], in_=ss[i][0:n, :]), r=[f"ss{i}"], w=[f"ss{i}"])
                    op("dve", lambda: V.scalar_tensor_tensor(out=hnb[i][0:n, :], in0=xt[i][0:n, :], scalar=ss[i][0:n, 0:1], in1=g1b[0:n, :],
                                                             op0=ALU.mult, op1=ALU.mult),
                       r=[f"xt{i}", f"ss{i}", "g1b"], w=[f"hnb{i}"])
                    for kt in range(8):
                        op("pe", lambda: PE.transpose(tp[i][:, kt, 0:n], hnb[i][0:n, kt * 128:(kt + 1) * 128], identb[0:n, 0:n]),
                           r=[f"hnb{i}", "identb"], w=[f"tp{i}"])
                    op("act", lambda: A.copy(out=hnT[:, :, p0:p0 + n], in_=tp[i][:, :, 0:n]), r=[f"tp{i}"], w=[("hnT", t)])
                S.barrier()
            stP1.close()
            hnT_all = [("hnT", t) for t in range(NT + 1)]
            if 'KT' in os.environ:
                hnT_all = [("hnT", int(v)) for v in os.environ['KT'].split(',')]

            if debug:
                hnT_d = scratch("hnT_d", [8, 128, L], BF16)
                for kt in range(8):
                    dma("sp", hnT_d[kt], hnT[:, kt, :], r=hnT_all, key="dbg_hnT")

            cwrow = sb("cwrow", [5, 3072], F32, st)
            dma("sp", cwrow[:], conv_w[0], w=["cwrow"])
            cw = sb("cw", [128, 24, 5], F32, st)
            with contextlib.ExitStack() as st1:
                pcw = ps("pcw", [128, 24, 5], F32, st1)
                for tt in range(24):
                    op("pe", lambda: PE.matmul(pcw[:, tt, :], lhsT=cwrow[:, tt * 128:(tt + 1) * 128], rhs=identf[0:5, 0:5], start=True, stop=True),
                       r=["cwrow", "identf"], w=["pcw"])
                op("dve", lambda: V.tensor_copy(out=cw[:], in_=pcw[:]), r=["pcw"], w=["cw"])
                S.barrier()

            NW = 4
            wt = [sb(f"wt{i}", [128, 8, 512], BF16, st) for i in range(NW)]
            NSTG = 3
            stg = [sb(f"stg{i}", [128, L + 4], BF16, st) for i in range(NSTG)]
            dgt = [sb(f"dgt{i}", [128, 5, 128], BF16, st) for i in range(3)]
            for i in range(NSTG):
                op("pool", lambda: G.memset(stg[i][:], 0.0), w=[f"stg{i}"])
            sact2 = [sb(f"sact{i}", [128, L], F32, st) for i in range(3)]
            sq2 = [sb(f"sq{i}", [128, L], BF16, st) for i in range(3)]
            rn2 = [sb(f"rn{i}", [128, L], F32, st) for i in range(3)]
            NOB = 3
            ob = [sb(f"ob{i}", [128, L], BF16, st) for i in range(NOB)]
            onesb = sb("onesb", [128, 128], BF16, st)
            op("dve", lambda: V.memset(onesb[:], 1.0), w=["onesb"])
            w_in_v = w_in[0].rearrange("(kt p) c -> p kt c", p=128)
            tiles_all = col_tiles()
            conv_t = [i for i, tl in enumerate(tiles_all) if tl[0] in ("q", "k", "v")]
            plain_t = [i for i, tl in enumerate(tiles_all) if tl[0] not in ("q", "k", "v")]
            order = []
            for j in range(max(len(conv_t), len(plain_t))):
                if j < len(conv_t):
                    order.append(conv_t[j])
                if j < len(plain_t):
                    order.append(plain_t[j])
            if stage < 1:
                order = order[:int(stage * 100)]
            grp_buf = {}
            cnt = {"na": 0, "nob": 0, "nconv": 0, "npc": 0}
            tstate = {}
            with contextlib.ExitStack() as st1:
                acc = [ps(f"acc{i}", [128, 512], F32, st1) for i in range(4)]
                ssb = [ps(f"ssb{i}", [128, 512], F32, st1) for i in range(2)]
                pcv = [ps(f"pcv{i}", [128, 512], F32, st1) for i in range(2)]

                def next_ob():
                    oi = cnt["nob"] % NOB
                    cnt["nob"] += 1
                    return oi

                def stageA(ti):
                    typ, idx, c0 = tiles_all[ti]
                    g = ti // 4
                    if g not in grp_buf:
                        grp_buf[g] = len(grp_buf) % NW
                        gc0 = tiles_all[g * 4][2]
                        dma("pool", wt[grp_buf[g]][:], w_in_v[:, :, gc0:gc0 + 512], w=[f"wt{grp_buf[g]}"])
                    wi = grp_buf[g]
                    wo_ = (ti % 4) * 128
                    conv = typ in ("q", "k", "v")
                    blks = PBLK if typ in ("f", "q", "k", "v") else RBLK
                    stt = {}
                    if conv:
                        stt["si"] = cnt["nconv"] % NSTG
                        stt["ci"] = cnt["nconv"] % 3
                        cnt["nconv"] += 1
                    else:
                        stt["oi"] = next_ob()
                    tstate[ti] = stt
                    for (b0, bn) in blks:
                        ai = cnt["na"] % 4
                        cnt["na"] += 1
                        for kt in range(8):
                            op("pe", lambda: PE.matmul(acc[ai][:, 0:bn], lhsT=wt[wi][:, kt, wo_:wo_ + 128], rhs=hnT[:, kt, b0:b0 + bn], start=(kt == 0), stop=(kt == 7)),
                               r=[f"wt{wi}"] + hnT_all, w=[f"acc{ai}"])
                        if conv:
                            si = stt["si"]
                            op("act", lambda: A.copy(out=stg[si][:, 2 + b0:2 + b0 + bn], in_=acc[ai][:, 0:bn]), r=[f"acc{ai}"], w=[f"stg{si}"])
                        else:
                            oi = stt["oi"]
                            if typ == "f":
                                op("act", lambda: A.copy(out=ob[oi][:, b0:b0 + bn], in_=acc[ai][:, 0:bn]), r=[f"acc{ai}"], w=[f"ob{oi}"])
                            else:
                                fn_ = AF.Silu if typ == "z" else AF.Sigmoid
                                op("act", lambda: A.activation(out=ob[oi][:, b0:b0 + bn], in_=acc[ai][:, 0:bn], func=fn_), r=[f"acc{ai}"], w=[f"ob{oi}"])
                        yield
                    if not conv:
                        oi = stt["oi"]
                        if typ == "f":
                            dma("sp", fin_s[idx], ob[oi][:], r=[f"ob{oi}"], key=f"ob{oi}")
                        else:
                            dst = {"z": z_s, "gf": gf_s, "gd": gd_s}[typ]
                            dma("sp", dst[idx], ob[oi][:, NM:L], r=[f"ob{oi}"], key=f"ob{oi}")

                def stageB(ti):
                    typ, idx, c0 = tiles_all[ti]
                    if typ not in ("q", "k", "v"):
                        return
                    stt = tstate[ti]
                    si, ci = stt["si"], stt["ci"]
                    sact, sq = sact2[ci], sq2[ci]
                    ks_, kq = f"sact{ci}", f"sq{ci}"
                    ct = {"q": 0, "k": 8, "v": 16}[typ] + idx
                    dg, kdg = dgt[ci], f"dgt{ci}"
                    for kk in range(5):
                        op("dve", lambda: V.tensor_scalar_mul(out=dg[:, kk, :], in0=identb[:], scalar1=cw[:, ct, kk:kk + 1]), r=["identb", "cw"], w=[kdg])
                    if typ == "v":
                        oi = next_ob()
                    for bi, (b0, bn) in enumerate(PBLK):
                        pi = cnt["npc"] % 2
                        cnt["npc"] += 1
                        for kk in range(5):
                            op("pe", lambda: PE.matmul(pcv[pi][:, 0:bn], lhsT=dg[:, kk, :], rhs=stg[si][:, b0 + kk:b0 + kk + bn], start=(kk == 0), stop=(kk == 4)),
                               r=[kdg, f"stg{si}"], w=[f"pcv{pi}"])
                        if typ == "v":
                            op("act", lambda: A.activation(out=ob[oi][:, b0:b0 + bn], in_=pcv[pi][:, 0:bn], func=AF.Silu), r=[f"pcv{pi}"], w=[f"ob{oi}"])
                        else:
                            op("act", lambda: A.activation(out=sact[:, b0:b0 + bn], in_=pcv[pi][:, 0:bn], func=AF.Silu), r=[f"pcv{pi}"], w=[ks_])
                    if typ == "v":
                        dma("sp", vT_s[idx], ob[oi][:], r=[f"ob{oi}"], key=f"ob{oi}")
                    else:
                        op("act", lambda: A.activation(out=sq[:], in_=sact[:], func=AF.Square), r=[ks_], w=[kq])

                def stageC(ti):
                    typ, idx, c0 = tiles_all[ti]
                    if typ not in ("q", "k"):
                        return
                        yield
                    stt = tstate[ti]
                    ci = stt["ci"]
                    sact, sq, rn = sact2[ci], sq2[ci], rn2[ci]
                    ks_, kq, kr = f"sact{ci}", f"sq{ci}", f"rn{ci}"
                    for bi, (b0, bn) in enumerate(PBLK):
                        pi = bi % 2
                        op("pe", lambda: PE.matmul(ssb[pi][:, 0:bn], lhsT=onesb[:], rhs=sq[:, b0:b0 + bn], start=True, stop=True), r=["onesb", kq], w=[f"ssb{pi}"])
                        op("act", lambda: A.activation(out=rn[:, b0:b0 + bn], in_=ssb[pi][:, 0:bn], func=AF.Sqrt, bias=epst[:], scale=1.0), r=[f"ssb{pi}", "epst"], w=[kr])
                        yield
                    op("dve", lambda: V.reciprocal(out=rn[:], in_=rn[:]), r=[kr], w=[kr])
                    oi = next_ob()
                    sc = (128 ** -0.5) if typ == "q" else 1.0
                    op("dve", lambda: V.scalar_tensor_tensor(out=ob[oi][:], in0=sact[:], scalar=sc, in1=rn[:], op0=ALU.mult, op1=ALU.mult), r=[ks_, kr], w=[f"ob{oi}"])
                    dst = qT_s if typ == "q" else kT_s
                    dma("sp", dst[idx], ob[oi][:], r=[f"ob{oi}"], key=f"ob{oi}")

                for s_ in range(len(order) + 4):
                    pc_step(2)
                    gA = stageA(order[s_]) if s_ < len(order) else iter(())
                    gC = stageC(order[s_ - 4]) if 0 <= s_ - 4 < len(order) else iter(())
                    doneA = doneC = False
                    while not (doneA and doneC):
                        if not doneA:
                            doneA = next(gA, "END") == "END"
                        if not doneC:
                            doneC = next(gC, "END") == "END"
                    if 0 <= s_ - 1 < len(order):
                        stageB(order[s_ - 1])
                na = cnt["na"]
                wab = sb("wab", [128, 8, 32], BF16, st)
                dma("pool", wab[:], w_in_v[:, :, 4608:4640], w=["wab"])
                abt = sb("abt", [128, 32], F32, st)
                for t in range(NT + 1 if stage >= 1 else 0):
                    n, p0 = (NM, 0) if t == 0 else (128, NM + (t - 1) * 128)
                    ai = na % 4
                    na += 1
                    for kt in range(8):
                        op("pe", lambda: PE.matmul(acc[ai][0:n, 0:32], lhsT=hnT[:, kt, p0:p0 + n], rhs=wab[:, kt, :], start=(kt == 0), stop=(kt == 7)),
                           r=["wab"] + hnT_all, w=[f"acc{ai}"])
                    op("act", lambda: A.copy(out=abt[0:n, :], in_=acc[ai][0:n, 0:32]), r=[f"acc{ai}"], w=["abt"])
                    dma("sp", ab_s[p0:p0 + n, :], abt[0:n, :], r=["abt"], key="abt")
                S.barrier()
            S.barrier()

        if stage < 2:
            S.finish()
            return nc, dbg
        mrg_s = scratch("mrg_s", [8, 128, SEQ], BF16)
        with contextlib.ExitStack() as st:
            mrgT = sb("mrgT", [128, 8, SEQ], BF16, st)
            finT = sb("finT", [128, 4, L], BF16, st)
            for g in range(4):
                dma("sp", finT[:, g, :], fin_s[g], w=["finT"], key="finT")
            cs128 = sb("cs128", [128, 256], BF16, st)
            dma("sp", cs128[:], c_cs128, w=["cs128"])
            Y = sb("Y", [128, NT + 1, 4, 256], BF16, st)
            fmixT = sb("fmixT", [128, 4, SEQ], BF16, st)
            wf = sb("wf", [128, 4, D], BF16, st)
            dma("pool", wf[:], w_fourier[0].rearrange("(g p) d -> p g d", p=128), w=["wf"])
            CLb = [sb(f"CLb{i}", [128, NT + 1, 512], BF16, st) for i in range(2)]
            SLb = [sb(f"SLb{i}", [128, NT + 1, 512], BF16, st) for i in range(2)]
            sgf = [sb(f"sgf{i}", [128, SEQ], BF16, st) for i in range(2)]
            with contextlib.ExitStack() as st1:
                py = [ps(f"py{i}", [128, 2, 256], F32, st1) for i in range(2)]
                pa = [ps(f"pa{i}", [128, 512], F32, st1) for i in range(4)]
                npy = 0
                for t in range(NT + 1):
                    n, p0 = (NM, 0) if t == 0 else (128, NM + (t - 1) * 128)
                    for g2 in range(2):
                        pi = npy % 2
                        npy += 1
                        for gi in range(2):
                            g = g2 * 2 + gi
                            op("pe", lambda: PE.matmul(py[pi][0:n, gi, :], lhsT=finT[:, g, p0:p0 + n], rhs=cs128[:], start=True, stop=True),
                               r=["finT", "cs128"], w=[f"py{pi}"])
                        op("act", lambda: A.copy(out=Y[0:n, t, g2 * 2:g2 * 2 + 2, :], in_=py[pi][0:n, :, :]), r=[f"py{pi}"], w=["Y"])
                npa = 0
                for bi, (b0, bn) in enumerate(RBLK):
                    ci = bi % 2
                    for (dst, srcm, nm) in ((CLb[ci], c_cosL, f"CLb{ci}"), (SLb[ci], c_nsinL, f"SLb{ci}")):
                        dma("sp", dst[0:NM, 0, :], srcm[0:NM, b0:b0 + bn], w=[nm], key=nm)
                        for hh in range(2):
                            dma("sp", dst[:, 1 + hh * 8:9 + hh * 8, :],
                                srcm[NM + hh * 1024:NM + (hh + 1) * 1024, b0:b0 + bn].rearrange("(t p) c -> p t c", p=128), w=[nm], key=nm)
                    for g in range(4):
                        ai = npa % 4
                        npa += 1
                        for t in range(NT + 1):
                            n = NM if t == 0 else 128
                            op("pe", lambda: PE.matmul(pa[ai][:, :], lhsT=Y[0:n, t, g, 0:128], rhs=CLb[ci][0:n, t, :], start=(t == 0), stop=False),
                               r=["Y", f"CLb{ci}"], w=[f"pa{ai}"])
                            op("pe", lambda: PE.matmul(pa[ai][:, :], lhsT=Y[0:n, t, g, 128:256], rhs=SLb[ci][0:n, t, :], start=False, stop=(t == NT)),
                               r=["Y", f"SLb{ci}"], w=[f"pa{ai}"])
                        op("act", lambda: A.copy(out=fmixT[:, g, b0 - NM:b0 - NM + bn], in_=pa[ai][:, :]), r=[f"pa{ai}"], w=["fmixT"])
                if debug:
                    fmix_d = scratch("fmix_d", [4, 128, SEQ], BF16)
                    for g in range(4):
                        dma("sp", fmix_d[g], fmixT[:, g, :], r=["fmixT"], key="dbg_fmix")
                for dt_ in range(8):
                    gi_ = dt_ % 2
                    dma("sp", sgf[gi_][:], gf_s[dt_], w=[f"sgf{gi_}"])
                    for bi in range(4):
                        ai = npa % 4
                        npa += 1
                        for g in range(4):
                            op("pe", lambda: PE.matmul(pa[ai][:, :], lhsT=wf[:, g, dt_ * 128:(dt_ + 1) * 128], rhs=fmixT[:, g, bi * 512:(bi + 1) * 512],
                                                       start=(g == 0), stop=(g == 3)),
                               r=["wf", "fmixT"], w=[f"pa{ai}"])
                        op("dve", lambda: V.tensor_tensor(out=mrgT[:, dt_, bi * 512:(bi + 1) * 512], in0=pa[ai][:, :], in1=sgf[gi_][:, bi * 512:(bi + 1) * 512], op=ALU.mult),
                           r=[f"pa{ai}", f"sgf{gi_}"], w=[("mrgT", dt_)])
                S.barrier()
            for dt_ in range(8):
                dma("sp", mrg_s[dt_], mrgT[:, dt_, :], r=[("mrgT", dt_)], w=["mrg_s"], key="mrg_s")
            S.barrier()
        if stage < 3:
            S.finish()
            return nc, dbg
        ogT = sb("ogT", [128, 8, SEQ], BF16)
        GE, ge = (G, "pool") if os.environ.get("P4POOL", "1") == "1" else (V, "dve")
        of_s = scratch("of_s", [NT, 128, NH, 128], F32)
        with contextlib.ExitStack() as st:
            masks = sb("masks", [128, 6, 128], F32, st)
            dma("sp", masks[:], c_masks[0:6].rearrange("m p f -> p m f"), w=["masks"])
            onesf = sb("onesf", [128, 128], F32, st)
            op("dve", lambda: V.memset(onesf[:], 1.0), w=["onesf"])
            onec = sb("onec", [128, 1], F32, st)
            op("dve", lambda: V.memset(onec[:], 1.0), w=["onec"])
            gout = sb("gout", [128, 1], F32, st)
            dma("sp", gout[:], out_norm_g.rearrange("o d -> d o"), w=["gout"])
            gball = sb("gball", [128, NT + 1, 2, 2, 8], F32, st)
            dtb = sb("dtb", [128, 2, 8], F32, st)
            nega = sb("nega", [128, 2, 8], F32, st)
            dma("sp", dtb[:, 0, :], dt_b_f[0].partition_broadcast(128), w=["dtb"], key="dtb")
            dma("sp", dtb[:, 1, :], dt_b_b[0].partition_broadcast(128), w=["dtb"], key="dtb")
            dma("sp", nega[:, 0, :], a_log_f[0].partition_broadcast(128), w=["nega"], key="nega")
            dma("sp", nega[:, 1, :], a_log_b[0].partition_broadcast(128), w=["nega"], key="nega")
            op("act", lambda: A.activation(out=nega[:], in_=nega[:], func=AF.Exp), r=["nega"], w=["nega"])
            op("act", lambda: A.mul(out=nega[:], in_=nega[:], mul=-1.0), r=["nega"], w=["nega"])
            abl = sb("abl", [128, NT + 1, 2, 2, 8], F32, st)
            xa = sb("xa", [128, NT + 1, 2, 8], F32, st)
            op("dve", lambda: V.memset(abl[:], 0.0), w=["abl"])
            dma("sp", abl[0:NM, 0], ab_s[0:NM, :].rearrange("p (d t h) -> p d t h", d=2, t=2), w=["abl"], key="abl")
            for hh in range(2):
                dma("sp", abl[:, 1 + hh * 8:9 + hh * 8], ab_s[NM + hh * 1024:NM + (hh + 1) * 1024, :].rearrange("(t p) (d u h) -> p t d u h", p=128, d=2, u=2),
                    w=["abl"], key="abl")
            NT1 = NT + 1
            op("dve", lambda: V.tensor_tensor(out=xa[:], in0=abl[:, :, :, 0, :], in1=dtb[:].unsqueeze(1).to_broadcast([128, NT1, 2, 8]), op=ALU.add), r=["abl", "dtb"], w=["xa"])
            op("act", lambda: A.activation(out=xa[:], in_=xa[:], func=AF.Exp), r=["xa"], w=["xa"])
            op("act", lambda: A.activation(out=xa[:], in_=xa[:], func=AF.Ln, bias=onec[:, :], scale=1.0), r=["xa", "onec"], w=["xa"])
            op("dve", lambda: V.tensor_tensor(out=gball[:, :, :, 0, :], in0=xa[:], in1=nega[:].unsqueeze(1).to_broadcast([128, NT1, 2, 8]), op=ALU.mult), r=["xa", "nega"], w=["gball"])
            op("act", lambda: A.activation(out=gball[:, :, :, 1, :], in_=abl[:, :, :, 1, :], func=AF.Sigmoid), r=["abl"], w=["gball"])
            if debug:
                gb_d = scratch("gb_d", [L, 32], F32)
                for t in range(NT + 1):
                    n, p0 = (NM, 0) if t == 0 else (128, NM + (t - 1) * 128)
                    dma("sp", gb_d[p0:p0 + n, :].rearrange("p (d t h) -> p d t h", d=2, t=2), gball[0:n, t], r=["gball"], key="dbg_gb")

            ob_s = scratch("ob_s", [NT, 128, NH, 128], F32)
            with contextlib.ExitStack() as st2:
                F4 = lambda nm, dt=F32: sb(nm, [128, 4, 128], dt, st2)
                SB = {}
                NSTR = 4
                for sid in range(NSTR):
                    for i in range(2):
                        for nm in ("qTt", "kTt", "vTt"):
                            SB[f"{nm}{i}_{sid}"] = F4(f"{nm}{i}_{sid}", BF16)
                    for nm in ("ktm", "vtm", "Sb_"):
                        SB[f"{nm}_{sid}"] = F4(f"{nm}_{sid}", BF16)
                    for nm in ("Gm", "oTt", "Sf"):
                        SB[f"{nm}_{sid}"] = F4(f"{nm}_{sid}", F32)
                    for nm in ("gcc", "egc", "bgc", "gend", "kds"):
                        SB[f"{nm}_{sid}"] = sb(f"{nm}_{sid}", [128, 4], F32, st2)
                    for nm in ("diff", "DL", "DU", "u_sb", "egb"):
                        SB[f"{nm}_{sid}"] = F4(f"{nm}_{sid}")
                    for nm in ("Ab", "ATb", "QKT", "TTb", "vbt", "kbg", "kdec", "nwT", "qdT", "vnew", "Pb0", "Pb1", "PTb0", "PTb1"):
                        SB[f"{nm}_{sid}"] = F4(f"{nm}_{sid}", BF16)

                def p4_stream(sid, pX, pY):
                    d_, hg = sid // 2, sid % 2
                    H0 = hg * 4
                    K_ = lambda nm: f"{nm}_{sid}"
                    B_ = lambda nm: SB[f"{nm}_{sid}"]
                    Gm, gcc, egc, bgc, gend, kds = B_("Gm"), B_("gcc"), B_("egc"), B_("bgc"), B_("gend"), B_("kds")
                    diff, DL, DU, u_sb, egb = [B_(x) for x in ("diff", "DL", "DU", "u_sb", "egb")]
                    Ab, ATb, QKT, TTb, vbt, kbg, kdec, nwT, qdT, vnew = [B_(x) for x in ("Ab", "ATb", "QKT", "TTb", "vbt", "kbg", "kdec", "nwT", "qdT", "vnew")]
                    ktm, vtm, Sb_, oTt, Sf = B_("ktm"), B_("vtm"), B_("Sb_"), B_("oTt"), B_("Sf")
                    kX, kY = K_("pX"), K_("pY")
                    S.excl.update([kX, kY])
                    pYb = pY[:].bitcast(BF16)
                    op("dve", lambda: V.memset(Sf[:], 0.0), r=[], w=[K_("Sf")])
                    op("dve", lambda: V.memset(Sb_[:], 0.0), r=[], w=[K_("Sb_")])
                    order = list(range(0, NT + 1)) if d_ == 0 else list(range(NT, 0, -1))
                    mC, mA, mQ = masks[:, 0 + d_, :], masks[:, 2 + d_, :], masks[:, 4 + d_, :]
                    o_dst = of_s if d_ == 0 else ob_s
                    for it, t in enumerate(order):
                        n, p0 = (NM, 0) if t == 0 else (128, NM + (t - 1) * 128)
                        li = it % 2
                        qTl, kTl, vTl = B_(f"qTt{li}"), B_(f"kTt{li}"), B_(f"vTt{li}")
                        qn, kn, vn_ = K_(f"qTt{li}"), K_(f"kTt{li}"), K_(f"vTt{li}")
                        for (dst, src, nm) in ((qTl, qT_s, qn), (kTl, kT_s, kn), (vTl, vT_s, vn_)):
                            dma("sp", dst[:, :, 0:n], src[H0:H0 + 4, :, p0:p0 + n].rearrange("h d p -> d h p"), w=[nm])
                        yield
                        for (srcT, dstm, sn, dn) in ((kTl, ktm, kn, K_("ktm")), (vTl, vtm, vn_, K_("vtm"))):
                            for hi in range(4):
                                op("pe", lambda: PE.transpose(pYb[0:n, hi, 0:128], srcT[:, hi, 0:n], identb[:, :]), r=[sn, "identb"], w=[kY])
                            op("act", lambda: A.copy(out=dstm[0:n], in_=pYb[0:n, :, 0:128]), r=[kY], w=[dn])
                            yield
                        gcol = gball[0:n, t, d_, 0, H0:H0 + 4]
                        bcol = gball[0:n, t, d_, 1, H0:H0 + 4]
                        op("dve", lambda: V.tensor_tensor(out=Gm[0:n, :, 0:n], in0=mC[0:n, 0:n].unsqueeze(1).to_broadcast([n, 4, n]),
                                                          in1=gcol.unsqueeze(2).to_broadcast([n, 4, n]), op=ALU.mult), r=["masks", "gball"], w=[K_("Gm")])
                        op("pe", lambda: PE.matmul(pX[0:n, 0, 0:4], lhsT=mC[0:n, 0:n], rhs=gcol, start=True, stop=True), r=["masks", "gball"], w=[kX])
                        op("dve", lambda: V.tensor_copy(out=gcc[0:n], in_=pX[0:n, 0, 0:4]), r=[kX], w=[K_("gcc")])
                        op("act", lambda: A.activation(out=egc[0:n], in_=gcc[0:n], func=AF.Exp), r=[K_("gcc")], w=[K_("egc")])
                        op("dve", lambda: V.tensor_tensor(out=bgc[0:n], in0=egc[0:n], in1=bcol, op=ALU.mult), r=[K_("egc"), "gball"], w=[K_("bgc")])
                        yield
                        if t == 0:
                            chunks, ends = [(0, NM)], [NM - 1]
                        elif d_ == 0:
                            chunks, ends = [(0, 64), (64, 64)], [63, 127]
                        else:
                            chunks, ends = [(64, 64), (0, 64)], [64, 0]
                        if n == 128:
                            op("pe", lambda: PE.matmul(pX[:, :, 0:n], lhsT=onesf[0:n, :], rhs=Gm[0:n, :, 0:n], start=True, stop=True), r=["onesf", K_("Gm")], w=[kX])
                        else:
                            for hi in range(4):
                                op("pe", lambda: PE.matmul(pX[:, hi, 0:n], lhsT=onesf[0:n, :], rhs=Gm[0:n, hi, 0:n], start=True, stop=True), r=["onesf", K_("Gm")], w=[kX])
                        for hi in range(4):
                            op("pe", lambda: PE.matmul(pY[0:n, hi, 0:n], lhsT=kTl[:, hi, 0:n], rhs=kTl[:, hi, 0:n], start=True, stop=True), r=[kn], w=[kY])
                        op("dve", lambda: V.tensor_tensor(out=diff[0:n, :, 0:n], in0=gcc[0:n, :].unsqueeze(2).to_broadcast([n, 4, n]),
                                                          in1=pX[0:n, :, 0:n], op=ALU.subtract), r=[K_("gcc"), kX], w=[K_("diff")])
                        op("act", lambda: A.activation(out=egb[:, :, 0:n], in_=pX[:, :, 0:n], func=AF.Exp), r=[kX], w=[K_("egb")])
                        for ci, (r0, cn) in enumerate(chunks):
                            op("dve", lambda: V.tensor_copy(out=gend[r0:r0 + cn, :], in_=pX[r0:r0 + cn, :, ends[ci]]), r=[kX], w=[K_("gend")])
                        yield
                        op("dve", lambda: V.scalar_tensor_tensor(out=DL[0:n, :, 0:n], in0=diff[0:n, :, 0:n], scalar=0.0,
                                                                 in1=mA[0:n, 0:n].unsqueeze(1).to_broadcast([n, 4, n]), op0=ALU.min, op1=ALU.add),
                           r=[K_("diff"), "masks"], w=[K_("DL")])
                        op("dve", lambda: V.scalar_tensor_tensor(out=DU[0:n, :, 0:n], in0=diff[0:n, :, 0:n], scalar=0.0,
                                                                 in1=mQ[0:n, 0:n].unsqueeze(1).to_broadcast([n, 4, n]), op0=ALU.max, op1=ALU.add),
                           r=[K_("diff"), "masks"], w=[K_("DU")])
                        op("act", lambda: A.activation(out=DL[0:n, :, 0:n], in_=DL[0:n, :, 0:n], func=AF.Exp), r=[K_("DL")], w=[K_("DL")])
                        op("act", lambda: A.activation(out=DU[0:n, :, 0:n], in_=DU[0:n, :, 0:n], func=AF.Exp, scale=-1.0), r=[K_("DU")], w=[K_("DU")])
                        for hi in range(4):
                            op("pe", lambda: PE.matmul(pX[0:n, hi, 0:n], lhsT=kTl[:, hi, 0:n], rhs=qTl[:, hi, 0:n], start=True, stop=True), r=[kn, qn], w=[kX])
                        op("dve", lambda: V.tensor_tensor(out=kds[0:n, :], in0=gend[0:n, :], in1=gcc[0:n, :], op=ALU.subtract), r=[K_("gend"), K_("gcc")], w=[K_("kds")])
                        op("act", lambda: A.activation(out=kds[0:n, :], in_=kds[0:n, :], func=AF.Exp), r=[K_("kds")], w=[K_("kds")])
                        yield
                        op("dve", lambda: V.tensor_tensor(out=diff[0:n, :, 0:n], in0=pY[0:n, :, 0:n], in1=DL[0:n, :, 0:n], op=ALU.mult), r=[kY, K_("DL")], w=[K_("diff")])
                        op(ge, lambda: GE.tensor_tensor(out=Ab[0:n, :, 0:n], in0=diff[0:n, :, 0:n], in1=bcol.unsqueeze(2).to_broadcast([n, 4, n]), op=ALU.mult),
                           r=[K_("diff"), "gball"], w=[K_("Ab")])
                        op("dve", lambda: V.tensor_tensor(out=QKT[0:n, :, 0:n], in0=pX[0:n, :, 0:n], in1=DU[0:n, :, 0:n], op=ALU.mult), r=[kX, K_("DU")], w=[K_("QKT")])
                        yield
                        for hi in range(4):
                            op("pe", lambda: PE.transpose(pYb[0:n, hi, 0:n], Ab[0:n, hi, 0:n], identb[0:n, 0:n]), r=[K_("Ab"), "identb"], w=[kY])
                        op("act", lambda: A.copy(out=ATb[0:n, :, 0:n], in_=pYb[0:n, :, 0:n]), r=[kY], w=[K_("ATb")])
                        yield
                        op("dve", lambda: V.tensor_tensor(out=TTb[0:n, :, 0:n], in0=identf[0:n, 0:n].unsqueeze(1).to_broadcast([n, 4, n]), in1=ATb[0:n, :, 0:n], op=ALU.subtract),
                           r=["identf", K_("ATb")], w=[K_("TTb")])
                        yield
                        Pc, PTc, Pn_, PTn_ = Ab, ATb, K_("Ab"), K_("ATb")
                        for lvl in range(1, 6):
                            Pd, PTd = B_(f"Pb{lvl % 2}"), B_(f"PTb{lvl % 2}")
                            Pdn, PTdn = K_(f"Pb{lvl % 2}"), K_(f"PTb{lvl % 2}")
                            for hi in range(4):
                                op("pe", lambda: PE.matmul(pX[0:n, hi, 0:n], lhsT=PTc[0:n, hi, 0:n], rhs=Pc[0:n, hi, 0:n], start=True, stop=True), r=[Pn_, PTn_], w=[kX])
                            if lvl < 5:
                                for hi in range(4):
                                    op("pe", lambda: PE.matmul(pY[0:n, hi, 0:n], lhsT=Pc[0:n, hi, 0:n], rhs=PTc[0:n, hi, 0:n], start=True, stop=True), r=[Pn_, PTn_], w=[kY])
                            op("act", lambda: A.copy(out=Pd[0:n, :, 0:n], in_=pX[0:n, :, 0:n]), r=[kX], w=[Pdn])
                            if lvl < 5:
                                op("act", lambda: A.copy(out=PTd[0:n, :, 0:n], in_=pY[0:n, :, 0:n]), r=[kY], w=[PTdn])
                            yield
                            for hi in range(4):
                                op("pe", lambda: PE.matmul(pX[0:n, hi, 0:n], lhsT=Pd[0:n, hi, 0:n], rhs=TTb[0:n, hi, 0:n], start=True, stop=True), r=[Pdn, K_("TTb")], w=[kX])
                            op("dve", lambda: V.tensor_tensor(out=TTb[0:n, :, 0:n], in0=TTb[0:n, :, 0:n], in1=pX[0:n, :, 0:n], op=ALU.add), r=[K_("TTb"), kX], w=[K_("TTb")])
                            yield
                            Pc, PTc, Pn_, PTn_ = Pd, PTd, Pdn, PTdn
                        op(ge, lambda: GE.tensor_tensor(out=vbt[0:n], in0=vtm[0:n], in1=bcol.unsqueeze(2).to_broadcast([n, 4, 128]), op=ALU.mult), r=[K_("vtm"), "gball"], w=[K_("vbt")])
                        op(ge, lambda: GE.tensor_tensor(out=kbg[0:n], in0=ktm[0:n], in1=bgc[0:n, :].unsqueeze(2).to_broadcast([n, 4, 128]), op=ALU.mult), r=[K_("ktm"), K_("bgc")], w=[K_("kbg")])
                        op(ge, lambda: GE.tensor_tensor(out=kdec[0:n], in0=ktm[0:n], in1=kds[0:n, :].unsqueeze(2).to_broadcast([n, 4, 128]), op=ALU.mult), r=[K_("ktm"), K_("kds")], w=[K_("kdec")])
                        op(ge, lambda: GE.tensor_tensor(out=qdT[:, :, 0:n], in0=qTl[:, :, 0:n], in1=egb[:, :, 0:n], op=ALU.mult), r=[qn, K_("egb")], w=[K_("qdT")])
                        yield
                        for hi in range(4):
                            op("pe", lambda: PE.matmul(pX[0:n, hi, :], lhsT=TTb[0:n, hi, 0:n], rhs=vbt[0:n, hi, :], start=True, stop=True), r=[K_("TTb"), K_("vbt")], w=[kX])
                        for hi in range(4):
                            op("pe", lambda: PE.matmul(pY[:, hi, 0:n], lhsT=kbg[0:n, hi, :], rhs=TTb[0:n, hi, 0:n], start=True, stop=True), r=[K_("TTb"), K_("kbg")], w=[kY])
                        op("act", lambda: A.copy(out=u_sb[0:n], in_=pX[0:n]), r=[kX], w=[K_("u_sb")])
                        op("act", lambda: A.mul(out=nwT[:, :, 0:n], in_=pY[:, :, 0:n], mul=-1.0), r=[kY], w=[K_("nwT")])
                        yield
                        for ci, (r0, cn) in enumerate(chunks):
                            rs_ = slice(r0, r0 + cn)
                            for hi in range(4):
                                op("pe", lambda: PE.matmul(pX[rs_, hi, :], lhsT=nwT[:, hi, rs_], rhs=Sb_[:, hi, :], start=True, stop=True), r=[K_("nwT"), K_("Sb_")], w=[kX])
                            op("dve", lambda: V.tensor_tensor(out=vnew[rs_], in0=u_sb[rs_], in1=pX[rs_], op=ALU.add), r=[K_("u_sb"), kX], w=[K_("vnew")])
                            yield
                            if t > 0:
                                for hi in range(4):
                                    op("pe", lambda: PE.matmul(pY[:, hi, 0:cn], lhsT=Sb_[:, hi, :], rhs=qdT[:, hi, rs_], start=True, stop=False), r=[K_("Sb_"), K_("qdT")], w=[kY])
                                    op("pe", lambda: PE.matmul(pY[:, hi, 0:cn], lhsT=vnew[rs_, hi, :], rhs=QKT[rs_, hi, rs_], start=False, stop=True), r=[K_("vnew"), K_("QKT")], w=[kY])
                                op("act", lambda: A.copy(out=oTt[:, :, rs_], in_=pY[:, :, 0:cn]), r=[kY], w=[K_("oTt")])
                            for hi in range(4):
                                op("pe", lambda: PE.matmul(pX[:, hi, :], lhsT=kdec[rs_, hi, :], rhs=vnew[rs_, hi, :], start=True, stop=True), r=[K_("kdec"), K_("vnew")], w=[kX])
                            op("dve", lambda: V.tensor_tensor(out=Sf[:], in0=Sf[:], in1=egb[:, :, ends[ci]].unsqueeze(2).to_broadcast([128, 4, 128]), op=ALU.mult),
                               r=[K_("Sf"), K_("egb")], w=[K_("Sf")])
                            op("dve", lambda: V.tensor_tensor(out=Sf[:], in0=Sf[:], in1=pX[:], op=ALU.add), r=[K_("Sf"), kX], w=[K_("Sf")])
                            op("act", lambda: A.copy(out=Sb_[:], in_=Sf[:]), r=[K_("Sf")], w=[K_("Sb_")])
                            yield
                        if t == 0:
                            continue
                        c0 = (t - 1) * 128
                        dma("sp", o_dst[t - 1][:, H0:H0 + 4, :], oTt[:], r=[K_("oTt")], w=[("osc", d_, hg, t)], key=K_("oTt"))
                        yield

                with contextlib.ExitStack() as st1:
                    pXs = [ps(f"p4X{i}", [128, 4, 128], F32, st1) for i in range(NSTR)]
                    pYs = [ps(f"p4Y{i}", [128, 4, 128], F32, st1) for i in range(NSTR)]
                    gens = [p4_stream(i, pXs[i], pYs[i]) for i in range(NSTR)]
                    alive = [True] * NSTR
                    nstep = 0
                    while any(alive):
                        for i in range(NSTR):
                            if alive[i]:
                                try:
                                    next(gens[i])
                                except StopIteration:
                                    alive[i] = False
                                nstep += 1
                                if nstep % 18 == 0:
                                    pc_step(1)
                    S.barrier()
            F8 = lambda nm, dt=F32: sb(nm, [128, 8, 128], dt, st)
            ofl = [F8(f"ofl{i}") for i in range(2)]
            obl = [F8(f"obl{i}") for i in range(2)]
            osq2 = [F8(f"osq{i}") for i in range(2)]
            ors2 = [F8(f"ors{i}") for i in range(2)]
            zall = sb("zall", [128, 8, SEQ], BF16, st)
            for h in range(8):
                dma("sp", zall[:, h, :], z_s[h], w=["zall"], key="zall")
            with contextlib.ExitStack() as st1:
                pss = [ps(f"pss{i}", [128, 4, 128], F32, st1) for i in range(4)]
                for t in (range(1, NT + 1) if os.environ.get('P4COMB', '1') == '1' else []):
                    c0 = (t - 1) * 128
                    i = t % 2
                    osq, ors, kosq, kors = osq2[i], ors2[i], f"osq{i}", f"ors{i}"
                    dma("sp", ofl[i][:], of_s[t - 1], r=[("osc", 0, 0, t), ("osc", 0, 1, t)], w=[f"ofl{i}"])
                    dma("sp", obl[i][:], ob_s[t - 1], r=[("osc", 1, 0, t), ("osc", 1, 1, t)], w=[f"obl{i}"])
                    op("dve", lambda: V.tensor_tensor(out=ofl[i][:], in0=ofl[i][:], in1=obl[i][:], op=ALU.add), r=[f"ofl{i}", f"obl{i}"], w=[f"ofl{i}"])
                    op("act", lambda: A.activation(out=osq[:], in_=ofl[i][:], func=AF.Square), r=[f"ofl{i}"], w=[kosq])
                    for hg in range(2):
                        pi = (2 * t + hg) % 4
                        op("pe", lambda: PE.matmul(pss[pi][:], lhsT=onesf[:], rhs=osq[:, hg * 4:hg * 4 + 4, :], start=True, stop=True), r=["onesf", kosq], w=[f"pss{pi}"])
                        op("act", lambda: A.activation(out=ors[:, hg * 4:hg * 4 + 4, :], in_=pss[pi][:], func=AF.Sqrt, scale=1.0 / 128, bias=epst[:]), r=[f"pss{pi}", "epst"], w=[kors])
                    op("dve", lambda: V.reciprocal(out=ors[:], in_=ors[:]), r=[kors], w=[kors])
                    op("dve", lambda: V.scalar_tensor_tensor(out=ofl[i][:], in0=ofl[i][:], scalar=gout[:, 0:1], in1=ors[:], op0=ALU.mult, op1=ALU.mult),
                       r=[f"ofl{i}", "gout", kors], w=[f"ofl{i}"])
                    op("pool", lambda: G.tensor_tensor(out=ogT[:, :, c0:c0 + 128], in0=ofl[i][:], in1=zall[:, :, c0:c0 + 128], op=ALU.mult), r=[f"ofl{i}", "zall"], w=[("ogT", t)])
                S.barrier()
            if debug:
                og_d = scratch("og_d", [NH, 128, SEQ], BF16)
                for h in range(8):
                    dma("sp", og_d[h], ogT[:, h, :], r=[("ogT", t) for t in range(1, NT + 1)], key="dbg_og")
            S.barrier()
        if stage < 4:
            S.finish()
            return nc, dbg
        h2_s = scratch("h2_s", [SEQ, D], F32)
        hn2_s = scratch("hn2_s", [SEQ, D], BF16)
        IOA = bass.IndirectOffsetOnAxis
        call = sb("call", [128, NT, 2], F32)
        dall = sb("dall", [128, NT, 2], I32)
        NB = 3
        wgu = [sb(f"wgu{i}", [128, 8, 512], BF16) for i in range(NB)]
        wde = [sb(f"wde{i}", [128, 2, D], BF16) for i in range(NB)]

        PRECAST = os.environ.get("PRECAST", "1") == "1"

        def load_expert(e, part="both"):
            wi = e % NB
            if PRECAST:
                rk_ = [("wq", e // 2)]
                if part in ("both", "gu"):
                    dma("pool", wgu[wi][:, :, 0:256], wq_g[e].rearrange("p (kt j) -> p kt j", kt=8), r=rk_, w=[f"wgu{wi}"], key=f"wgu{wi}")
                    dma("pool", wgu[wi][:, :, 256:512], wq_u[e].rearrange("p (kt j) -> p kt j", kt=8), r=rk_, w=[f"wgu{wi}"], key=f"wgu{wi}")
                if part in ("both", "d"):
                    dma("pool", wde[wi][:], wq_d[e].rearrange("p (jt d) -> p jt d", jt=2), r=rk_, w=[f"wde{wi}"])
                return
            dma("pool", wgu[wi][:, :, 0:256], w_gate_e[0, e].rearrange("(p kt) j -> p kt j", kt=8), w=[f"wgu{wi}"], key=f"wgu{wi}")
            dma("pool", wgu[wi][:, :, 256:512], w_up_e[0, e].rearrange("(p kt) j -> p kt j", kt=8), w=[f"wgu{wi}"], key=f"wgu{wi}")
            dma("pool", wde[wi][:], w_down_e[0, e].rearrange("(jt p) d -> p jt d", p=128), w=[f"wde{wi}"])

        pc_step(1000)
        if stage >= 5:
            for e in range(NB):
                load_expert(e)
        with contextlib.ExitStack() as st:
            mrgT = sb("mrgT2", [128, 8, SEQ], BF16, st)
            for dt_ in range(8):
                dma("sp", mrgT[:, dt_, :], mrg_s[dt_], r=["mrg_s"], w=[("mrgT", dt_)], key=f"mrgT2_{dt_}")
            wd = sb("wd", [128, 8, D], BF16, st)
            wo = sb("wo", [128, 8, D], BF16, st)
            dma("pool", wd[:], w_delta[0].rearrange("(h p) d -> p h d", p=128), w=["wd"])
            dma("pool", wo[:], w_out[0].rearrange("(h p) d -> p h d", p=128), w=["wo"])
            sgd = [sb(f"sgd{i}", [128, SEQ], BF16, st) for i in range(2)]
            tmpm = sb("tmpm", [128, 512], F32, st)
            g2b = sb("g2b", [128, D], F32, st)
            dma("sp", g2b[:], norm2_g[0].partition_broadcast(128), w=["g2b"])
            wr = sb("wr", [128, 8, 72], F32, st)
            with nc.allow_non_contiguous_dma(reason="small router weights"):
                dma("sp", wr[:, :, 0:8], w_rg[0].rearrange("(kt p) g -> p kt g", p=128), w=["wr"], key="wr")
                dma("sp", wr[:, :, 8:72], w_re[0].rearrange("(kt p) g -> p kt g", p=128), w=["wr"], key="wr")
            rbias = sb("rbias", [128, 72], F32, st)
            dma("sp", rbias[:, 0:8], b_rg[0].partition_broadcast(128), w=["rbias"], key="rbias")
            dma("sp", rbias[:, 8:72], b_re[0].partition_broadcast(128), w=["rbias"], key="rbias")
            ustf = sb("ustf", [128, 128], F32, st)
            dma("sp", ustf[:], c_masks[6], w=["ustf"])
            ustb = sb("ustb", [128, 128], BF16, st)
            op("act", lambda: A.copy(out=ustb[:], in_=ustf[:]), r=["ustf"], w=["ustb"])
            onesb2 = sb("onesb2", [128, 128], BF16, st)
            op("dve", lambda: V.memset(onesb2[:], 1.0), w=["onesb2"])
            ecap = sb("ecap", [128, 64], F32, st)
            dma("sp", ecap[:], c_ecap, w=["ecap"])
            Mall = sb("Mall", [128, NT, 64], BF16, st)
            lgall = sb("lgall", [128, NT, 72], F32, st)
            sm = {"ss": sb("sm_ss", [128, 1], F32, st)}
            stA = contextlib.ExitStack()
            xr = [sb(f"xr{i}", [128, D], F32, stA) for i in range(2)]
            h2t = [sb(f"h2t{i}", [128, D], F32, stA) for i in range(2)]
            hn2f2 = [sb(f"hn2f{i}", [128, D], F32, stA) for i in range(2)]
            hn2b = [sb(f"hn2b{i}", [128, D], BF16, stA) for i in range(2)]
            junk2 = sb("junk2", [128, D], BF16, stA)
            hn2T = sb("hn2T", [128, 8, 128], F32, stA)
            with contextlib.ExitStack() as st1:
                pa = [ps(f"pb{i}", [128, 512], F32, st1) for i in range(3)]
                ptf = [ps(f"ptf{i}", [128, 4, 128], F32, st1) for i in range(2)]
                plg = ps("plg", [128, 72], F32, st1)
                prk2 = [ps(f"prk{i}", [128, 8, 64], F32, st1) for i in range(2)]
                npa = 0
                for dt_ in range(8):
                    gi_ = dt_ % 2
                    dma("sp", sgd[gi_][:], gd_s[dt_], w=[f"sgd{gi_}"])
                    for bi in range(4):
                        ai = npa % 3
                        npa += 1
                        for h in range(8):
                            op("pe", lambda: PE.matmul(pa[ai][:, :], lhsT=wd[:, h, dt_ * 128:(dt_ + 1) * 128], rhs=ogT[:, h, bi * 512:(bi + 1) * 512],
                                                       start=(h == 0), stop=(h == 7)),
                               r=["wd"] + [("ogT", t) for t in range(1, NT + 1)], w=[f"pb{ai}"])
                        op("dve", lambda: V.tensor_tensor(out=tmpm[:], in0=pa[ai][:, :], in1=sgd[gi_][:, bi * 512:(bi + 1) * 512], op=ALU.mult),
                           r=[f"pb{ai}", f"sgd{gi_}"], w=["tmpm"])
                        op("dve", lambda: V.tensor_tensor(out=mrgT[:, dt_, bi * 512:(bi + 1) * 512], in0=tmpm[:], in1=mrgT[:, dt_, bi * 512:(bi + 1) * 512], op=ALU.add),
                           r=["tmpm", ("mrgT", dt_)], w=[("mrgT", dt_)])
                mrg_all = [("mrgT", d2) for d2 in range(8)]
                def LA(t):
                    i = t % 2
                    hn2f = hn2f2[i]
                    nonlocal_npa = None
                    dma("sp", xr[i][:], x[t * 128:(t + 1) * 128, :], w=[f"xr{i}"])
                    for half in range(2):
                        ai = cntA[0] % 3
                        cntA[0] += 1
                        for dt_ in range(8):
                            op("pe", lambda: PE.matmul(pa[ai][:, :], lhsT=mrgT[:, dt_, t * 128:(t + 1) * 128], rhs=wo[:, dt_, half * 512:(half + 1) * 512],
                                                       start=(dt_ == 0), stop=(dt_ == 7)), r=["wo"] + mrg_all, w=[f"pb{ai}"])
                        op("dve", lambda: V.tensor_tensor(out=h2t[i][:, half * 512:(half + 1) * 512], in0=pa[ai][:, :], in1=xr[i][:, half * 512:(half + 1) * 512], op=ALU.add),
                           r=[f"pb{ai}", f"xr{i}"], w=[f"h2t{i}"])
                    dma("sp", h2_s[t * 128:(t + 1) * 128, :], h2t[i][:], r=[f"h2t{i}"], w=["h2_s"], key="h2_s")
                    op("act", lambda: A.activation(out=junk2[:], in_=h2t[i][:], func=AF.Square, accum_out=sm["ss"][:]), r=[f"h2t{i}"], w=["junk2", "sm_ss"])
                    op("act", lambda: A.activation(out=sm["ss"][:], in_=sm["ss"][:], func=AF.Sqrt, scale=1.0 / D, bias=epst[:]), r=["sm_ss", "epst"], w=["sm_ss"])
                    op("dve", lambda: V.reciprocal(out=sm["ss"][:], in_=sm["ss"][:]), r=["sm_ss"], w=["sm_ss"])
                    op("dve", lambda: V.scalar_tensor_tensor(out=hn2f[:], in0=h2t[i][:], scalar=sm["ss"][:, 0:1], in1=g2b[:], op0=ALU.mult, op1=ALU.mult),
                       r=[f"h2t{i}", "sm_ss", "g2b"], w=[f"hn2f{i}"])
                    op("act", lambda: A.copy(out=hn2b[i][:], in_=hn2f[:]), r=[f"hn2f{i}"], w=[f"hn2b{i}"])
                    dma("sp", hn2_s[t * 128:(t + 1) * 128, :], hn2b[i][:], r=[f"hn2b{i}"], w=["hn2_s"], key=f"hn2b{i}")

                def LB(t):
                    i = t % 2
                    hn2f = hn2f2[i]
                    for kt in range(8):
                        op("pe", lambda: PE.transpose(ptf[kt // 4][:, kt % 4, :], hn2f[:, kt * 128:(kt + 1) * 128], identf[:]), r=[f"hn2f{i}", "identf"], w=[f"ptf{kt // 4}"])
                    for q_ in range(2):
                        op("act", lambda: A.copy(out=hn2T[:, q_ * 4:q_ * 4 + 4, :], in_=ptf[q_][:]), r=[f"ptf{q_}"], w=["hn2T"])
                    for kt in range(8):
                        op("pe", lambda: PE.matmul(plg[:, :], lhsT=hn2T[:, kt, :], rhs=wr[:, kt, :], start=(kt == 0), stop=(kt == 7)), r=["hn2T", "wr"], w=["plg"])
                    op("dve", lambda: V.tensor_tensor(out=lgall[:, t, :], in0=plg[:, :], in1=rbias[:], op=ALU.add), r=["plg", "rbias"], w=["lgall"])

                cntA = [npa]
                for t in range(NT + 1):
                    if t < NT:
                        LA(t)
                    if t >= 1:
                        LB(t - 1)
                S.barrier()
                stA.close()
                hn2b = [sb(f"hn2c{i}", [128, D], BF16, st) for i in range(2)]
                TT_ = lambda o, a, b, o_: V.tensor_tensor(out=o, in0=a, in1=b, op=o_)
                R = {nm: sb("rt_" + nm, [128, NT, w_], F32, st) for nm, w_ in
                     (("gmax", 1), ("ge", 8), ("gsum", 1), ("pg", 1), ("ohg", 8), ("tmp", 64), ("elg", 8), ("m1", 1), ("oh1", 8), ("el2", 8),
                      ("m2", 1), ("oh2", 8), ("d12", 1), ("w1", 1), ("M1", 64), ("M2", 64), ("rk", 64), ("t3", 64), ("d1f", 1), ("d2f", 1))}
                k = lambda nm: "rt_" + nm
                lgg = lgall[:, :, 0:8]
                op("dve", lambda: V.tensor_reduce(out=R["gmax"][:, :, 0], in_=lgg, axis=AX.X, op=ALU.max), r=["lgall"], w=[k("gmax")])
                op("dve", lambda: TT_(R["ge"][:], lgg, R["gmax"][:].to_broadcast([128, NT, 8]), ALU.subtract), r=["lgall", k("gmax")], w=[k("ge")])
                op("act", lambda: A.activation(out=R["ge"][:], in_=R["ge"][:], func=AF.Exp), r=[k("ge")], w=[k("ge")])
                op("dve", lambda: V.tensor_reduce(out=R["gsum"][:, :, 0], in_=R["ge"][:], axis=AX.X, op=ALU.add), r=[k("ge")], w=[k("gsum")])
                op("dve", lambda: V.reciprocal(out=R["pg"][:], in_=R["gsum"][:]), r=[k("gsum")], w=[k("pg")])
                op("dve", lambda: TT_(R["ohg"][:], lgg, R["gmax"][:].to_broadcast([128, NT, 8]), ALU.is_equal), r=["lgall", k("gmax")], w=[k("ohg")])
                el4 = lgall[:, :, 8:72].rearrange("p t (g e) -> p t g e", g=8)
                tmp4 = R["tmp"][:].rearrange("p t (g e) -> p t g e", g=8)
                op("dve", lambda: TT_(tmp4, el4, R["ohg"][:].unsqueeze(3).to_broadcast([128, NT, 8, 8]), ALU.mult), r=["lgall", k("ohg")], w=[k("tmp")])
                op("dve", lambda: V.tensor_reduce(out=R["elg"][:], in_=tmp4.rearrange("p t g e -> p t e g"), axis=AX.X, op=ALU.add), r=[k("tmp")], w=[k("elg")])
                op("dve", lambda: V.tensor_reduce(out=R["m1"][:, :, 0], in_=R["elg"][:], axis=AX.X, op=ALU.max), r=[k("elg")], w=[k("m1")])
                op("dve", lambda: TT_(R["oh1"][:], R["elg"][:], R["m1"][:].to_broadcast([128, NT, 8]), ALU.is_equal), r=[k("elg"), k("m1")], w=[k("oh1")])
                op("dve", lambda: V.scalar_tensor_tensor(out=R["el2"][:], in0=R["oh1"][:], scalar=-1.0e30, in1=R["elg"][:], op0=ALU.mult, op1=ALU.add),
                   r=[k("oh1"), k("elg")], w=[k("el2")])
                op("dve", lambda: V.tensor_reduce(out=R["m2"][:, :, 0], in_=R["el2"][:], axis=AX.X, op=ALU.max), r=[k("el2")], w=[k("m2")])
                op("dve", lambda: TT_(R["oh2"][:], R["el2"][:], R["m2"][:].to_broadcast([128, NT, 8]), ALU.is_equal), r=[k("el2"), k("m2")], w=[k("oh2")])
                op("dve", lambda: TT_(R["d12"][:], R["m1"][:], R["m2"][:], ALU.subtract), r=[k("m1"), k("m2")], w=[k("d12")])
                op("act", lambda: A.activation(out=R["w1"][:], in_=R["d12"][:], func=AF.Sigmoid), r=[k("d12")], w=[k("w1")])
                op("dve", lambda: TT_(call[:, :, 0:1], R["pg"][:], R["w1"][:], ALU.mult), r=[k("pg"), k("w1")], w=["call"])
                op("dve", lambda: TT_(call[:, :, 1:2], R["pg"][:], call[:, :, 0:1], ALU.subtract), r=[k("pg"), "call"], w=["call"])
                for (Mn, ohn) in (("M1", "oh1"), ("M2", "oh2")):
                    op("dve", lambda: TT_(R[Mn][:].rearrange("p t (g e) -> p t g e", g=8), R["ohg"][:].unsqueeze(3).to_broadcast([128, NT, 8, 8]),
                                          R[ohn][:].unsqueeze(2).to_broadcast([128, NT, 8, 8]), ALU.mult), r=[k("ohg"), k(ohn)], w=[k(Mn)])
                op("dve", lambda: TT_(Mall[:], R["M1"][:], R["M2"][:], ALU.add), r=[k("M1"), k("M2")], w=["Mall"])
                for t in range(NT):
                    pr = prk2[t // 8]
                    prn = f"prk{t // 8}"
                    op("pe", lambda: PE.matmul(pr[:, t % 8, :], lhsT=ustb[:], rhs=Mall[:, t, :], start=True, stop=(t == 0)), r=["ustb", "Mall"], w=[prn])
                    for j in range(t):
                        op("pe", lambda: PE.matmul(pr[:, t % 8, :], lhsT=onesb2[:], rhs=Mall[:, j, :], start=False, stop=(j == t - 1)), r=["onesb2", "Mall"], w=[prn])
                for q_ in range(2):
                    op("dve", lambda: TT_(R["rk"][:, q_ * 8:q_ * 8 + 8, :], prk2[q_][:], ecap[:].unsqueeze(1).to_broadcast([128, 8, 64]), ALU.add), r=[f"prk{q_}", "ecap"], w=[k("rk")])
                for (Mn, dn, ci_) in (("M1", "d1f", 0), ("M2", "d2f", 1)):
                    op("dve", lambda: TT_(R["t3"][:], R["rk"][:], R[Mn][:], ALU.mult), r=[k("rk"), k(Mn)], w=[k("t3")])
                    op("dve", lambda: V.tensor_reduce(out=R[dn][:, :, 0], in_=R["t3"][:], axis=AX.X, op=ALU.add), r=[k("t3")], w=[k(dn)])
                    op("dve", lambda: V.tensor_copy(out=dall[:, :, ci_:ci_ + 1], in_=R[dn][:]), r=[k(dn)], w=["dall"])
                for t in range(NT):
                    i = t % 2
                    dma("sp", hn2b[i][:], hn2_s[t * 128:(t + 1) * 128, :], r=["hn2_s"], w=[f"hn2c{i}"])
                    for ci_ in range(2):
                        dma("pool", None, None, r=[f"hn2c{i}", "dall"], w=["Xs"], key="Xs",
                            indirect=lambda: G.indirect_dma_start(out=Xs[:, :], out_offset=IOA(ap=dall[:, t, ci_:ci_ + 1], axis=0), in_=hn2b[i][:], in_offset=None))
                S.barrier()
            S.barrier()
        if stage < 5:
            S.finish()
            return nc, dbg
        with contextlib.ExitStack() as st:
            Xe = [sb(f"Xe{i}", [128, D], BF16, st) for i in range(3)]
            XeT2 = [sb(f"XeT{i}", [128, 8, 128], BF16, st) for i in range(3)]
            sg2 = [sb(f"sg{i}", [128, 256], F32, st) for i in range(2)]
            actb2 = [sb(f"actb{i}", [128, 256], BF16, st) for i in range(2)]
            actT2 = [sb(f"actT{i}", [128, 2, 128], BF16, st) for i in range(2)]
            Ye = [sb(f"Ye{i}", [128, D], F32, st) for i in range(2)]
            gfb = sb("gfb", [128, D], F32, st)
            dma("sp", gfb[:], final_g.partition_broadcast(128), w=["gfb"])
            ya2 = [sb(f"ya{i}", [128, D], F32, st) for i in range(4)]
            yb2 = [sb(f"yb{i}", [128, D], F32, st) for i in range(4)]
            hh = [sb(f"hh{i}", [128, D], F32, st) for i in range(2)]
            oo = [sb(f"oo{i}", [128, D], F32, st) for i in range(2)]
            junk3 = sb("junk3", [128, D], BF16, st)
            ssf = sb("ssf", [128, 1], F32, st)
            with contextlib.ExitStack() as st1:
                ptx = ps("ptx", [128, 8, 128], BF16, st1)
                pta = ps("pta", [128, 8, 128], BF16, st1)
                ph = [ps(f"ph{i}", [128, 512], F32, st1) for i in range(2)]
                pyy = [ps(f"pyy{i}", [128, 512], F32, st1) for i in range(2)]
                NX = 3
                def st1_(e):
                    xi = e % NX
                    for kt in range(8):
                        op("pe", lambda: PE.transpose(ptx[:, kt, :], Xe[xi][:].rearrange("s (p k) -> s k p", k=8)[:, kt, :], identb[:]), r=[f"Xe{xi}", "identb"], w=["ptx"])
                    op("act", lambda: A.copy(out=XeT2[xi][:], in_=ptx[:]), r=["ptx"], w=[f"XeT{xi}"])

                def st2_(e):
                    wi, xi, pi = e % NB, e % NX, e % 2
                    for kt in range(8):
                        op("pe", lambda: PE.matmul(ph[pi][:, :], lhsT=XeT2[xi][:, kt, :], rhs=wgu[wi][:, kt, :], start=(kt == 0), stop=(kt == 7)), r=[f"XeT{xi}", f"wgu{wi}"], w=[f"ph{pi}"])
                    op("act", lambda: A.activation(out=sg2[pi][:], in_=ph[pi][:, 0:256], func=AF.Silu), r=[f"ph{pi}"], w=[f"sg{pi}"])
                    op("dve", lambda: V.tensor_tensor(out=actb2[pi][:], in0=sg2[pi][:], in1=ph[pi][:, 256:512], op=ALU.mult), r=[f"sg{pi}", f"ph{pi}"], w=[f"actb{pi}"])
                    if PRECAST and e + NB < NEXP:
                        load_expert(e + NB, "gu")

                def st3_(e):
                    pi = e % 2
                    for jt in range(2):
                        op("pe", lambda: PE.transpose(pta[:, jt, :], actb2[pi][:, jt * 128:(jt + 1) * 128], identb[:]), r=[f"actb{pi}", "identb"], w=["pta"])
                    op("act", lambda: A.copy(out=actT2[pi][:], in_=pta[:, 0:2, :]), r=["pta"], w=[f"actT{pi}"])

                def st4_(e):
                    wi, pi = e % NB, e % 2
                    for half in range(2):
                        for jt in range(2):
                            op("pe", lambda: PE.matmul(pyy[half][:, :], lhsT=actT2[pi][:, jt, :], rhs=wde[wi][:, jt, half * 512:(half + 1) * 512], start=(jt == 0), stop=(jt == 1)),
                               r=[f"actT{pi}", f"wde{wi}"], w=[f"pyy{half}"])
                    op("act", lambda: A.copy(out=Ye[pi][:, 0:512], in_=pyy[0][:, :]), r=["pyy0"], w=[f"Ye{pi}"])
                    op("dve", lambda: V.tensor_copy(out=Ye[pi][:, 512:1024], in_=pyy[1][:, :]), r=["pyy1"], w=[f"Ye{pi}"])
                    dma("sp", Ys[e * CAP:(e + 1) * CAP, :], Ye[pi][:], r=[f"Ye{pi}"], w=["Ys"], key="Ys")
                    if e + NB < NEXP:
                        load_expert(e + NB, "d" if PRECAST else "both")

                def xload(e):
                    if 0 <= e < NEXP:
                        dma("act", Xe[e % NX][:], Xs[e * CAP:(e + 1) * CAP, :], r=["Xs"], w=[f"Xe{e % NX}"])

                xload(0)
                xload(1)
                for s_ in range(NEXP + 3):
                    xload(s_ + 2)
                    if s_ < NEXP:
                        st1_(s_)
                    if 0 <= s_ - 1 < NEXP:
                        st2_(s_ - 1)
                    if 0 <= s_ - 2 < NEXP:
                        st3_(s_ - 2)
                    if 0 <= s_ - 3 < NEXP:
                        st4_(s_ - 3)
                for t in range(NT):
                    i = t % 2
                    ya, yb, kya, kyb = ya2[t % 4], yb2[t % 4], f"ya{t % 4}", f"yb{t % 4}"
                    dma("sp", hh[i][:], h2_s[t * 128:(t + 1) * 128, :], r=["h2_s"], w=[f"hh{i}"])
                    dma("pool", None, None, r=["Ys", "dall"], w=[kya], key=kya,
                        indirect=lambda: G.indirect_dma_start(out=ya[:], out_offset=None, in_=Ys[:, :], in_offset=IOA(ap=dall[:, t, 0:1], axis=0)))
                    dma("pool", None, None, r=["Ys", "dall"], w=[kyb], key=kyb,
                        indirect=lambda: G.indirect_dma_start(out=yb[:], out_offset=None, in_=Ys[:, :], in_offset=IOA(ap=dall[:, t, 1:2], axis=0)))
                    op("dve", lambda: V.scalar_tensor_tensor(out=hh[i][:], in0=ya[:], scalar=call[:, t, 0:1], in1=hh[i][:], op0=ALU.mult, op1=ALU.add),
                       r=[kya, "call", f"hh{i}"], w=[f"hh{i}"])
                    op("dve", lambda: V.scalar_tensor_tensor(out=hh[i][:], in0=yb[:], scalar=call[:, t, 1:2], in1=hh[i][:], op0=ALU.mult, op1=ALU.add),
                       r=[kyb, "call", f"hh{i}"], w=[f"hh{i}"])
                    op("act", lambda: A.activation(out=junk3[:], in_=hh[i][:], func=AF.Square, accum_out=ssf[:]), r=[f"hh{i}"], w=["junk3", "ssf"])
                    op("act", lambda: A.activation(out=ssf[:], in_=ssf[:], func=AF.Sqrt, scale=1.0 / D, bias=epst[:]), r=["ssf", "epst"], w=["ssf"])
                    op("dve", lambda: V.reciprocal(out=ssf[:], in_=ssf[:]), r=["ssf"], w=["ssf"])
                    op("dve", lambda: V.scalar_tensor_tensor(out=oo[i][:], in0=hh[i][:], scalar=ssf[:, 0:1], in1=gfb[:], op0=ALU.mult, op1=ALU.mult),
                       r=[f"hh{i}", "ssf", "gfb"], w=[f"oo{i}"])
                    dma("sp", out[t * 128:(t + 1) * 128, :], oo[i][:], r=[f"oo{i}"], key=f"oo{i}")
                S.barrier()
            S.barrier()
        S.finish()
    return nc, dbg


def host_consts():
    c = {}
    c["c_identb"] = np.eye(128, dtype=np.float32).astype(ml_dtypes.bfloat16)
    c["c_identf"] = np.eye(128, dtype=np.float32)
    p = np.arange(L, dtype=np.float64)
    ang = 2.0 * np.pi * ((p[:, None] * p[None, :]) % L) / L
    c["c_cosL"] = np.cos(ang).astype(np.float32).astype(ml_dtypes.bfloat16)
    c["c_nsinL"] = (-np.sin(ang)).astype(np.float32).astype(ml_dtypes.bfloat16)
    q = np.arange(128, dtype=np.float64)
    a2 = 2.0 * np.pi * ((q[:, None] * q[None, :]) % 128) / 128
    sc = 1.0 / np.sqrt(L * 128.0)
    c["c_cs128"] = np.concatenate([np.cos(a2) * sc, np.sin(a2) * sc], axis=1).astype(np.float32).astype(ml_dtypes.bfloat16)
    c["c_masks"] = make_masks()
    c["c_ecap"] = np.tile((np.arange(64, dtype=np.float32) * CAP)[None, :], (128, 1))
    return c


def make_masks():
    i = np.arange(128)
    same = (i[:, None] // 64) == (i[None, :] // 64)
    m = np.zeros((7, 128, 128), np.float32)
    m[0] = (same & (i[:, None] <= i[None, :])).astype(np.float32)
    m[1] = (same & (i[:, None] >= i[None, :])).astype(np.float32)
    BIG = 30000.0
    m[2] = np.where(same & (i[None, :] < i[:, None]), 0.0, -BIG)
    m[3] = np.where(same & (i[None, :] > i[:, None]), 0.0, -BIG)
    m[4] = np.where(same & (i[:, None] <= i[None, :]), 0.0, BIG)
    m[5] = np.where(same & (i[:, None] >= i[None, :]), 0.0, BIG)
    m[6] = (i[:, None] < i[None, :]).astype(np.float32)
    return m


_CACHE = {}
PARAM_NAMES = ["meta_tokens", "norm1_g", "w_in", "conv_w", "a_log_fwd", "dt_bias_fwd", "a_log_bwd", "dt_bias_bwd",
               "out_norm_g", "w_fourier", "w_delta", "w_out", "norm2_g", "w_router_group", "b_router_group",
               "w_router_expert", "b_router_expert", "w_gate_e", "w_up_e", "w_down_e", "final_norm_g"]


def core_inputs(inputs, b):
    m = {"x": np.ascontiguousarray(np.asarray(inputs["x"])[b], dtype=np.float32)}
    for k in PARAM_NAMES:
        m[k] = np.ascontiguousarray(np.asarray(inputs[k]), dtype=np.float32)
    return m


def kernel(**inputs):
    if "nc" not in _CACHE:
        _CACHE["nc"] = build()[0]
        _CACHE["consts"] = host_consts()
    nc = _CACHE["nc"]
    maps = []
    for b in range(8):
        m = core_inputs(inputs, b)
        m.update(_CACHE["consts"])
        maps.append(m)
    res = run_bass_kernel_spmd(nc, maps, core_ids=list(range(8)))
    return np.stack([np.asarray(r["out"], dtype=np.float32) for r in res.results], axis=0)
```

```python
import contextlib
import os
import numpy as np
import ml_dtypes
import concourse.bass as bass
import concourse.mybir as mybir
from concourse.bass_utils import run_bass_kernel_spmd

F32 = mybir.dt.float32
BF16 = mybir.dt.bfloat16
I32 = mybir.dt.int32
AF = mybir.ActivationFunctionType
ALU = mybir.AluOpType
AX = mybir.AxisListType

D = 1024
NM = 16
SEQ = 2048
L = NM + SEQ
NH = 8
INW = 6688
EPS = 1e-6
NEXP = 64
CAP = 128
DE = 256
NT = SEQ // 128


class Sched:
    def __init__(self, nc):
        self.nc = nc
        self.eng = {"pe": nc.tensor, "act": nc.scalar, "dve": nc.vector, "pool": nc.gpsimd, "sp": nc.sync}
        self.sem = {e: nc.alloc_semaphore(f"s_{e}") for e in self.eng}
        self.cnt = {e: 0 for e in self.eng}
        self.seen = {e: {} for e in self.eng}
        self.semobj = {}
        self.bufs = {}
        self.nsem = 0
        self.excl = set()
        self.free_sems = []

    def _wait(self, e, ev):
        sem, val = ev
        if self.seen[e].get(sem.name, 0) >= val:
            return
        self.eng[e].wait_ge(sem, val)
        self.seen[e][sem.name] = val

    def deps(self, e, reads, writes):
        evs = []
        own = self.sem[e]
        for b in reads:
            st = self.bufs.get(b)
            if st and st["w"]:
                evs.append(st["w"])
            if st and b in self.excl:
                for r in st["r"]:
                    if r[0] is not own:
                        evs.append(r)
        for b in writes:
            st = self.bufs.get(b)
            if st:
                if st["w"]:
                    evs.append(st["w"])
                for r in st["r"]:
                    if r[0] is own:
                        continue
                    evs.append(r)
        best = {}
        for sem, val in evs:
            if e == "pe" and sem is own:
                continue
            if sem.name not in best or best[sem.name][1] < val:
                best[sem.name] = (sem, val)
        for ev in best.values():
            self._wait(e, ev)

    def record(self, ev, reads, writes):
        for b in reads:
            st = self.bufs.setdefault(b, {"w": None, "r": []})
            st["r"] = [r for r in st["r"] if r[0] is not ev[0]] + [ev]
        for b in writes:
            self.bufs[b] = {"w": ev, "r": []}

    def op(self, e, fn, r=(), w=()):
        self.deps(e, r, w)
        ins = fn()
        self.cnt[e] += 1
        ins.then_inc(self.sem[e], 1)
        ev = (self.sem[e], self.cnt[e])
        self.record(ev, r, w)
        return ev

    def dma(self, e, out, in_, r=(), w=(), key=None, indirect=None, **kw):
        self.deps(e, r, w)
        key = key or (w[0] if w else r[0])
        if key not in self.semobj:
            if self.free_sems:
                self.semobj[key] = self.free_sems.pop()
            else:
                self.semobj[key] = [self.nc.alloc_semaphore(f"d{self.nsem}"), 0]
                self.nsem += 1
        so = self.semobj[key]
        if indirect is None:
            ins = self.eng[e].dma_start(out=out, in_=in_, **kw)
        else:
            ins = indirect()
        so[1] += 16
        ins.then_inc(so[0], 16)
        ev = (so[0], so[1])
        self.record(ev, r, w)
        return ev

    def barrier(self):
        allev = {}
        for st in self.bufs.values():
            for ev in ([st["w"]] if st["w"] else []) + st["r"]:
                if ev[0].name not in allev or allev[ev[0].name][1] < ev[1]:
                    allev[ev[0].name] = ev
        for e in self.eng:
            for ev in allev.values():
                self._wait(e, ev)
        self.free_sems.extend(self.semobj.values())
        self.semobj = {}

    def finish(self):
        allev = {}
        for st in self.bufs.values():
            for ev in ([st["w"]] if st["w"] else []) + st["r"]:
                if ev[0].name not in allev or allev[ev[0].name][1] < ev[1]:
                    allev[ev[0].name] = ev
        for ev in allev.values():
            self._wait("sp", ev)


def col_tiles():
    tl = []
    for g in range(4):
        tl.append(("f", g, g * 128))
    for h in range(NH):
        tl.append(("q", h, 512 + h * 128))
    for h in range(NH):
        tl.append(("k", h, 1536 + h * 128))
    for h in range(NH):
        tl.append(("v", h, 2560 + h * 128))
    for h in range(NH):
        tl.append(("z", h, 3584 + h * 128))
    for j in range(8):
        tl.append(("gf", j, 4640 + j * 128))
    for j in range(8):
        tl.append(("gd", j, 5664 + j * 128))
    return tl


PBLK = [(0, 512), (512, 512), (1024, 512), (1536, 512), (2048, 16)]
RBLK = [(16 + 512 * i, 512) for i in range(4)]


def build(stage=99, debug=False):
    nc = bass.Bass("TRN2", target_bir_lowering=False)
    S = Sched(nc)
    global LAST_SCHED
    LAST_SCHED = S
    op, dma = S.op, S.dma
    V, A, PE, G = nc.vector, nc.scalar, nc.tensor, nc.gpsimd

    def din(name, shape, dt=F32):
        return nc.dram_tensor(name, list(shape), dt, kind="ExternalInput").ap()

    x = din("x", [SEQ, D])
    meta = din("meta_tokens", [NM, D])
    norm1_g = din("norm1_g", [1, D])
    w_in = din("w_in", [1, D, INW])
    conv_w = din("conv_w", [1, 5, 3072])
    a_log_f = din("a_log_fwd", [1, 8]); dt_b_f = din("dt_bias_fwd", [1, 8])
    a_log_b = din("a_log_bwd", [1, 8]); dt_b_b = din("dt_bias_bwd", [1, 8])
    out_norm_g = din("out_norm_g", [1, 128])
    w_fourier = din("w_fourier", [1, 512, D])
    w_delta = din("w_delta", [1, D, D])
    w_out = din("w_out", [1, D, D])
    norm2_g = din("norm2_g", [1, D])
    w_rg = din("w_router_group", [1, D, 8]); b_rg = din("b_router_group", [1, 8])
    w_re = din("w_router_expert", [1, D, 64]); b_re = din("b_router_expert", [1, 64])
    w_gate_e = din("w_gate_e", [1, NEXP, D, DE]); w_up_e = din("w_up_e", [1, NEXP, D, DE])
    w_down_e = din("w_down_e", [1, NEXP, DE, D])
    final_g = din("final_norm_g", [D])
    c_identb = din("c_identb", [128, 128], BF16)
    c_identf = din("c_identf", [128, 128], F32)
    c_cosL = din("c_cosL", [L, L], BF16)
    c_nsinL = din("c_nsinL", [L, L], BF16)
    c_cs128 = din("c_cs128", [128, 256], BF16)
    c_masks = din("c_masks", [7, 128, 128], F32)
    c_ecap = din("c_ecap", [128, 64], F32)

    out = nc.dram_tensor("out", [SEQ, D], F32, kind="ExternalOutput").ap()

    dbg = {}

    def scratch(name, shape, dt):
        kind = "ExternalOutput" if debug else "Internal"
        t = nc.dram_tensor(name, list(shape), dt, kind=kind).ap()
        dbg[name] = t
        return t

    qT_s = scratch("qT_s", [NH, 128, L], BF16)
    kT_s = scratch("kT_s", [NH, 128, L], BF16)
    vT_s = scratch("vT_s", [NH, 128, L], BF16)
    z_s = scratch("z_s", [NH, 128, SEQ], BF16)
    gf_s = scratch("gf_s", [8, 128, SEQ], BF16)
    gd_s = scratch("gd_s", [8, 128, SEQ], BF16)
    ab_s = scratch("ab_s", [L, 32], F32)
    fin_s = scratch("fin_s", [4, 128, L], BF16)

    wq_g = nc.dram_tensor("wq_g", [NEXP, 128, 8 * DE], BF16, kind="Internal").ap()
    wq_u = nc.dram_tensor("wq_u", [NEXP, 128, 8 * DE], BF16, kind="Internal").ap()
    wq_d = nc.dram_tensor("wq_d", [NEXP, 128, 2 * D], BF16, kind="Internal").ap()

    def precast_gen():
        for e in range(NEXP):
            kk_ = ("pc", e // 2)
            dma("pool", wq_g[e], w_gate_e[0, e].rearrange("(p kt) j -> p (kt j)", kt=8), w=[("wq", e // 2)], key=kk_)
            yield
            dma("pool", wq_u[e], w_up_e[0, e].rearrange("(p kt) j -> p (kt j)", kt=8), w=[("wq", e // 2)], key=kk_)
            yield
            dma("pool", wq_d[e].rearrange("p (jt d) -> p jt d", jt=2), w_down_e[0, e].rearrange("(jt p) d -> p jt d", p=128), w=[("wq", e // 2)], key=kk_)
            yield

    pcg = precast_gen() if (stage >= 5 and os.environ.get("PRECAST", "1") == "1") else iter(())

    def pc_step(k=1):
        for _ in range(k):
            next(pcg, None)

    with contextlib.ExitStack() as gst:
        def sb(name, shape, dt, st=gst):
            return st.enter_context(nc.sbuf_tensor(name, list(shape), dt))

        def ps(name, shape, dt, st=gst):
            S.excl.add(name)
            return st.enter_context(nc.psum_tensor(name, list(shape), dt))

        identb = sb("identb", [128, 128], BF16)
        identf = sb("identf", [128, 128], F32)
        epst = sb("epst", [128, 1], F32)
        dma("sp", identb[:], c_identb, w=["identb"])
        dma("sp", identf[:], c_identf, w=["identf"])
        op("dve", lambda: V.memset(epst[:], EPS), w=["epst"])
        Xs = nc.dram_tensor("Xs", [NEXP * CAP, D], BF16, kind="Internal").ap()
        Ys = nc.dram_tensor("Ys", [NEXP * CAP, D], F32, kind="Internal").ap()
        zfill = sb("zfill", [128, 2048], BF16)
        op("dve", lambda: V.memset(zfill[:], 0.0), w=["zfill"])
        Xs_v = Xs.rearrange("(p a) c -> p (a c)", p=128)
        for i in range(32):
            dma("pool", Xs_v[:, i * 2048:(i + 1) * 2048], zfill[:], r=["zfill"], w=["Xs"], key="Xs")

        with contextlib.ExitStack() as st:
            hnT = sb("hnT", [128, 8, L], BF16, st)
            stP1 = contextlib.ExitStack()
            g1b = sb("g1b", [128, D], F32, stP1)
            dma("sp", g1b[:], norm1_g[0].partition_broadcast(128), w=["g1b"])
            xt = [sb(f"xt{i}", [128, D], F32, stP1) for i in range(2)]
            junk = sb("junk", [128, D], BF16, stP1)
            hnb = [sb(f"hnb{i}", [128, D], BF16, stP1) for i in range(2)]
            ss = [sb(f"ss{i}", [128, 1], F32, stP1) for i in range(2)]
            with contextlib.ExitStack() as st1:
                tp = [ps(f"tp{i}", [128, 8, 128], BF16, st1) for i in range(2)]

                for t in (range(NT + 1) if 'KT' not in os.environ else [int(v) for v in os.environ['KT'].split(',')]):
                    i = t % 2
                    if t == 0:
                        n, src, p0 = NM, meta, 0
                    else:
                        n, src, p0 = 128, x[(t - 1) * 128:t * 128, :], NM + (t - 1) * 128
                    dma("sp", xt[i][0:n, :], src, w=[f"xt{i}"])
                    op("act", lambda: A.activation(out=junk[0:n, :], in_=xt[i][0:n, :], func=AF.Square, accum_out=ss[i][0:n, :]),
                       r=[f"xt{i}"], w=["junk", f"ss{i}"])
                    op("act", lambda: A.activation(out=ss[i][0:n, :], in_=ss[i][0:n, :], func=AF.Sqrt, scale=1.0 / D, bias=epst[0:n, :]),
                       r=[f"ss{i}", "epst"], w=[f"ss{i}"])
                    op("dve", lambda: V.reciprocal(out=ss[i][0:n, :], in_=ss[i][0:n, :]), r=[f"ss{i}"], w=[f"ss{i}"])
                    op("dve", lambda: V.scalar_tensor_tensor(out=hnb[i][0:n, :], in0=xt[i][0:n, :], scalar=ss[i][0:n, 0:1], in1=g1b[0:n, :],
                                                             op0=ALU.mult, op1=ALU.mult),
                       r=[f"xt{i}", f"ss{i}", "g1b"], w=[f"hnb{i}"])
                    for kt in range(8):
                        op("pe", lambda: PE.transpose(tp[i][:, kt, 0:n], hnb[i][0:n, kt * 128:(kt + 1) * 128], identb[0:n, 0:n]),
                           r=[f"hnb{i}", "identb"], w=[f"tp{i}"])
                    op("act", lambda: A.copy(out=hnT[:, :, p0:p0 + n], in_=tp[i][:, :, 0:n]), r=[f"tp{i}"], w=[("hnT", t)])
                S.barrier()
            stP1.close()
            hnT_all = [("hnT", t) for t in range(NT + 1)]
            if 'KT' in os.environ:
                hnT_all = [("hnT", int(v)) for v in os.environ['KT'].split(',')]

            if debug:
                hnT_d = scratch("hnT_d", [8, 128, L], BF16)
                for kt in range(8):
                    dma("sp", hnT_d[kt], hnT[:, kt, :], r=hnT_all, key="dbg_hnT")

            cwrow = sb("cwrow", [5, 3072], F32, st)
            dma("sp", cwrow[:], conv_w[0], w=["cwrow"])
            cw = sb("cw", [128, 24, 5], F32, st)
            with contextlib.ExitStack() as st1:
                pcw = ps("pcw", [128, 24, 5], F32, st1)
                for tt in range(24):
                    op("pe", lambda: PE.matmul(pcw[:, tt, :], lhsT=cwrow[:, tt * 128:(tt + 1) * 128], rhs=identf[0:5, 0:5], start=True, stop=True),
                       r=["cwrow", "identf"], w=["pcw"])
                op("dve", lambda: V.tensor_copy(out=cw[:], in_=pcw[:]), r=["pcw"], w=["cw"])
                S.barrier()

            NW = 4
            wt = [sb(f"wt{i}", [128, 8, 512], BF16, st) for i in range(NW)]
            NSTG = 3
            stg = [sb(f"stg{i}", [128, L + 4], BF16, st) for i in range(NSTG)]
            dgt = [sb(f"dgt{i}", [128, 5, 128], BF16, st) for i in range(3)]
            for i in range(NSTG):
                op("pool", lambda: G.memset(stg[i][:], 0.0), w=[f"stg{i}"])
            sact2 = [sb(f"sact{i}", [128, L], F32, st) for i in range(3)]
            sq2 = [sb(f"sq{i}", [128, L], BF16, st) for i in range(3)]
            rn2 = [sb(f"rn{i}", [128, L], F32, st) for i in range(3)]
            NOB = 3
            ob = [sb(f"ob{i}", [128, L], BF16, st) for i in range(NOB)]
            onesb = sb("onesb", [128, 128], BF16, st)
            op("dve", lambda: V.memset(onesb[:], 1.0), w=["onesb"])
            w_in_v = w_in[0].rearrange("(kt p) c -> p kt c", p=128)
            tiles_all = col_tiles()
            conv_t = [i for i, tl in enumerate(tiles_all) if tl[0] in ("q", "k", "v")]
            plain_t = [i for i, tl in enumerate(tiles_all) if tl[0] not in ("q", "k", "v")]
            order = []
            for j in range(max(len(conv_t), len(plain_t))):
                if j < len(conv_t):
                    order.append(conv_t[j])
                if j < len(plain_t):
                    order.append(plain_t[j])
            if stage < 1:
                order = order[:int(stage * 100)]
            grp_buf = {}
            cnt = {"na": 0, "nob": 0, "nconv": 0, "npc": 0}
            tstate = {}
            with contextlib.ExitStack() as st1:
                acc = [ps(f"acc{i}", [128, 512], F32, st1) for i in range(4)]
                ssb = [ps(f"ssb{i}", [128, 512], F32, st1) for i in range(2)]
                pcv = [ps(f"pcv{i}", [128, 512], F32, st1) for i in range(2)]

                def next_ob():
                    oi = cnt["nob"] % NOB
                    cnt["nob"] += 1
                    return oi

                def stageA(ti):
                    typ, idx, c0 = tiles_all[ti]
                    g = ti // 4
                    if g not in grp_buf:
                        grp_buf[g] = len(grp_buf) % NW
                        gc0 = tiles_all[g * 4][2]
                        dma("pool", wt[grp_buf[g]][:], w_in_v[:, :, gc0:gc0 + 512], w=[f"wt{grp_buf[g]}"])
                    wi = grp_buf[g]
                    wo_ = (ti % 4) * 128
                    conv = typ in ("q", "k", "v")
                    blks = PBLK if typ in ("f", "q", "k", "v") else RBLK
                    stt = {}
                    if conv:
                        stt["si"] = cnt["nconv"] % NSTG
                        stt["ci"] = cnt["nconv"] % 3
                        cnt["nconv"] += 1
                    else:
                        stt["oi"] = next_ob()
                    tstate[ti] = stt
                    for (b0, bn) in blks:
                        ai = cnt["na"] % 4
                        cnt["na"] += 1
                        for kt in range(8):
                            op("pe", lambda: PE.matmul(acc[ai][:, 0:bn], lhsT=wt[wi][:, kt, wo_:wo_ + 128], rhs=hnT[:, kt, b0:b0 + bn], start=(kt == 0), stop=(kt == 7)),
                               r=[f"wt{wi}"] + hnT_all, w=[f"acc{ai}"])
                        if conv:
                            si = stt["si"]
                            op("act", lambda: A.copy(out=stg[si][:, 2 + b0:2 + b0 + bn], in_=acc[ai][:, 0:bn]), r=[f"acc{ai}"], w=[f"stg{si}"])
                        else:
                            oi = stt["oi"]
                            if typ == "f":
                                op("act", lambda: A.copy(out=ob[oi][:, b0:b0 + bn], in_=acc[ai][:, 0:bn]), r=[f"acc{ai}"], w=[f"ob{oi}"])
                            else:
                                fn_ = AF.Silu if typ == "z" else AF.Sigmoid
                                op("act", lambda: A.activation(out=ob[oi][:, b0:b0 + bn], in_=acc[ai][:, 0:bn], func=fn_), r=[f"acc{ai}"], w=[f"ob{oi}"])
                        yield
                    if not conv:
                        oi = stt["oi"]
                        if typ == "f":
                            dma("sp", fin_s[idx], ob[oi][:], r=[f"ob{oi}"], key=f"ob{oi}")
                        else:
                            dst = {"z": z_s, "gf": gf_s, "gd": gd_s}[typ]
                            dma("sp", dst[idx], ob[oi][:, NM:L], r=[f"ob{oi}"], key=f"ob{oi}")

                def stageB(ti):
                    typ, idx, c0 = tiles_all[ti]
                    if typ not in ("q", "k", "v"):
                        return
                    stt = tstate[ti]
                    si, ci = stt["si"], stt["ci"]
                    sact, sq = sact2[ci], sq2[ci]
                    ks_, kq = f"sact{ci}", f"sq{ci}"
                    ct = {"q": 0, "k": 8, "v": 16}[typ] + idx
                    dg, kdg = dgt[ci], f"dgt{ci}"
                    for kk in range(5):
                        op("dve", lambda: V.tensor_scalar_mul(out=dg[:, kk, :], in0=identb[:], scalar1=cw[:, ct, kk:kk + 1]), r=["identb", "cw"], w=[kdg])
                    if typ == "v":
                        oi = next_ob()
                    for bi, (b0, bn) in enumerate(PBLK):
                        pi = cnt["npc"] % 2
                        cnt["npc"] += 1
                        for kk in range(5):
                            op("pe", lambda: PE.matmul(pcv[pi][:, 0:bn], lhsT=dg[:, kk, :], rhs=stg[si][:, b0 + kk:b0 + kk + bn], start=(kk == 0), stop=(kk == 4)),
                               r=[kdg, f"stg{si}"], w=[f"pcv{pi}"])
                        if typ == "v":
                            op("act", lambda: A.activation(out=ob[oi][:, b0:b0 + bn], in_=pcv[pi][:, 0:bn], func=AF.Silu), r=[f"pcv{pi}"], w=[f"ob{oi}"])
                        else:
                            op("act", lambda: A.activation(out=sact[:, b0:b0 + bn], in_=pcv[pi][:, 0:bn], func=AF.Silu), r=[f"pcv{pi}"], w=[ks_])
                    if typ == "v":
                        dma("sp", vT_s[idx], ob[oi][:], r=[f"ob{oi}"], key=f"ob{oi}")
                    else:
                        op("act", lambda: A.activation(out=sq[:], in_=sact[:], func=AF.Square), r=[ks_], w=[kq])

                def stageC(ti):
                    typ, idx, c0 = tiles_all[ti]
                    if typ not in ("q", "k"):
                        return
                        yield
                    stt = tstate[ti]
                    ci = stt["ci"]
                    sact, sq, rn = sact2[ci], sq2[ci], rn2[ci]
                    ks_, kq, kr = f"sact{ci}", f"sq{ci}", f"rn{ci}"
                    for bi, (b0, bn) in enumerate(PBLK):
                        pi = bi % 2
                        op("pe", lambda: PE.matmul(ssb[pi][:, 0:bn], lhsT=onesb[:], rhs=sq[:, b0:b0 + bn], start=True, stop=True), r=["onesb", kq], w=[f"ssb{pi}"])
                        op("act", lambda: A.activation(out=rn[:, b0:b0 + bn], in_=ssb[pi][:, 0:bn], func=AF.Sqrt, bias=epst[:], scale=1.0), r=[f"ssb{pi}", "epst"], w=[kr])
                        yield
                    op("dve", lambda: V.reciprocal(out=rn[:], in_=rn[:]), r=[kr], w=[kr])
                    oi = next_ob()
                    sc = (128 ** -0.5) if typ == "q" else 1.0
                    op("dve", lambda: V.scalar_tensor_tensor(out=ob[oi][:], in0=sact[:], scalar=sc, in1=rn[:], op0=ALU.mult, op1=ALU.mult), r=[ks_, kr], w=[f"ob{oi}"])
                    dst = qT_s if typ == "q" else kT_s
                    dma("sp", dst[idx], ob[oi][:], r=[f"ob{oi}"], key=f"ob{oi}")

                for s_ in range(len(order) + 4):
                    pc_step(2)
                    gA = stageA(order[s_]) if s_ < len(order) else iter(())
                    gC = stageC(order[s_ - 4]) if 0 <= s_ - 4 < len(order) else iter(())
                    doneA = doneC = False
                    while not (doneA and doneC):
                        if not doneA:
                            doneA = next(gA, "END") == "END"
                        if not doneC:
                            doneC = next(gC, "END") == "END"
                    if 0 <= s_ - 1 < len(order):
                        stageB(order[s_ - 1])
                na = cnt["na"]
                wab = sb("wab", [128, 8, 32], BF16, st)
                dma("pool", wab[:], w_in_v[:, :, 4608:4640], w=["wab"])
                abt = sb("abt", [128, 32], F32, st)
                for t in range(NT + 1 if stage >= 1 else 0):
                    n, p0 = (NM, 0) if t == 0 else (128, NM + (t - 1) * 128)
                    ai = na % 4
                    na += 1
                    for kt in range(8):
                        op("pe", lambda: PE.matmul(acc[ai][0:n, 0:32], lhsT=hnT[:, kt, p0:p0 + n], rhs=wab[:, kt, :], start=(kt == 0), stop=(kt == 7)),
                           r=["wab"] + hnT_all, w=[f"acc{ai}"])
                    op("act", lambda: A.copy(out=abt[0:n, :], in_=acc[ai][0:n, 0:32]), r=[f"acc{ai}"], w=["abt"])
                    dma("sp", ab_s[p0:p0 + n, :], abt[0:n, :], r=["abt"], key="abt")
                S.barrier()
            S.barrier()

        if stage < 2:
            S.finish()
            return nc, dbg
        mrg_s = scratch("mrg_s", [8, 128, SEQ], BF16)
        with contextlib.ExitStack() as st:
            mrgT = sb("mrgT", [128, 8, SEQ], BF16, st)
            finT = sb("finT", [128, 4, L], BF16, st)
            for g in range(4):
                dma("sp", finT[:, g, :], fin_s[g], w=["finT"], key="finT")
            cs128 = sb("cs128", [128, 256], BF16, st)
            dma("sp", cs128[:], c_cs128, w=["cs128"])
            Y = sb("Y", [128, NT + 1, 4, 256], BF16, st)
            fmixT = sb("fmixT", [128, 4, SEQ], BF16, st)
            wf = sb("wf", [128, 4, D], BF16, st)
            dma("pool", wf[:], w_fourier[0].rearrange("(g p) d -> p g d", p=128), w=["wf"])
            CLb = [sb(f"CLb{i}", [128, NT + 1, 512], BF16, st) for i in range(2)]
            SLb = [sb(f"SLb{i}", [128, NT + 1, 512], BF16, st) for i in range(2)]
            sgf = [sb(f"sgf{i}", [128, SEQ], BF16, st) for i in range(2)]
            with contextlib.ExitStack() as st1:
                py = [ps(f"py{i}", [128, 2, 256], F32, st1) for i in range(2)]
                pa = [ps(f"pa{i}", [128, 512], F32, st1) for i in range(4)]
                npy = 0
                for t in range(NT + 1):
                    n, p0 = (NM, 0) if t == 0 else (128, NM + (t - 1) * 128)
                    for g2 in range(2):
                        pi = npy % 2
                        npy += 1
                        for gi in range(2):
                            g = g2 * 2 + gi
                            op("pe", lambda: PE.matmul(py[pi][0:n, gi, :], lhsT=finT[:, g, p0:p0 + n], rhs=cs128[:], start=True, stop=True),
                               r=["finT", "cs128"], w=[f"py{pi}"])
                        op("act", lambda: A.copy(out=Y[0:n, t, g2 * 2:g2 * 2 + 2, :], in_=py[pi][0:n, :, :]), r=[f"py{pi}"], w=["Y"])
                npa = 0
                for bi, (b0, bn) in enumerate(RBLK):
                    ci = bi % 2
                    for (dst, srcm, nm) in ((CLb[ci], c_cosL, f"CLb{ci}"), (SLb[ci], c_nsinL, f"SLb{ci}")):
                        dma("sp", dst[0:NM, 0, :], srcm[0:NM, b0:b0 + bn], w=[nm], key=nm)
                        for hh in range(2):
                            dma("sp", dst[:, 1 + hh * 8:9 + hh * 8, :],
                                srcm[NM + hh * 1024:NM + (hh + 1) * 1024, b0:b0 + bn].rearrange("(t p) c -> p t c", p=128), w=[nm], key=nm)
                    for g in range(4):
                        ai = npa % 4
                        npa += 1
                        for t in range(NT + 1):
                            n = NM if t == 0 else 128
                            op("pe", lambda: PE.matmul(pa[ai][:, :], lhsT=Y[0:n, t, g, 0:128], rhs=CLb[ci][0:n, t, :], start=(t == 0), stop=False),
                               r=["Y", f"CLb{ci}"], w=[f"pa{ai}"])
                            op("pe", lambda: PE.matmul(pa[ai][:, :], lhsT=Y[0:n, t, g, 128:256], rhs=SLb[ci][0:n, t, :], start=False, stop=(t == NT)),
                               r=["Y", f"SLb{ci}"], w=[f"pa{ai}"])
                        op("act", lambda: A.copy(out=fmixT[:, g, b0 - NM:b0 - NM + bn], in_=pa[ai][:, :]), r=[f"pa{ai}"], w=["fmixT"])
                if debug:
                    fmix_d = scratch("fmix_d", [4, 128, SEQ], BF16)
                    for g in range(4):
                        dma("sp", fmix_d[g], fmixT[:, g, :], r=["fmixT"], key="dbg_fmix")
                for dt_ in range(8):
                    gi_ = dt_ % 2
                    dma("sp", sgf[gi_][:], gf_s[dt_], w=[f"sgf{gi_}"])
                    for bi in range(4):
                        ai = npa % 4
                        npa += 1
                        for g in range(4):
                            op("pe", lambda: PE.matmul(pa[ai][:, :], lhsT=wf[:, g, dt_ * 128:(dt_ + 1) * 128], rhs=fmixT[:, g, bi * 512:(bi + 1) * 512],
                                                       start=(g == 0), stop=(g == 3)),
                               r=["wf", "fmixT"], w=[f"pa{ai}"])
                        op("dve", lambda: V.tensor_tensor(out=mrgT[:, dt_, bi * 512:(bi + 1) * 512], in0=pa[ai][:, :], in1=sgf[gi_][:, bi * 512:(bi + 1) * 512], op=ALU.mult),
                           r=[f"pa{ai}", f"sgf{gi_}"], w=[("mrgT", dt_)])
                S.barrier()
            for dt_ in range(8):
                dma("sp", mrg_s[dt_], mrgT[:, dt_, :], r=[("mrgT", dt_)], w=["mrg_s"], key="mrg_s")
            S.barrier()
        if stage < 3:
            S.finish()
            return nc, dbg
        ogT = sb("ogT", [128, 8, SEQ], BF16)
        GE, ge = (G, "pool") if os.environ.get("P4POOL", "1") == "1" else (V, "dve")
        of_s = scratch("of_s", [NT, 128, NH, 128], F32)
        with contextlib.ExitStack() as st:
            masks = sb("masks", [128, 6, 128], F32, st)
            dma("sp", masks[:], c_masks[0:6].rearrange("m p f -> p m f"), w=["masks"])
            onesf = sb("onesf", [128, 128], F32, st)
            op("dve", lambda: V.memset(onesf[:], 1.0), w=["onesf"])
            onec = sb("onec", [128, 1], F32, st)
            op("dve", lambda: V.memset(onec[:], 1.0), w=["onec"])
            gout = sb("gout", [128, 1], F32, st)
            dma("sp", gout[:], out_norm_g.rearrange("o d -> d o"), w=["gout"])
            gball = sb("gball", [128, NT + 1, 2, 2, 8], F32, st)
            dtb = sb("dtb", [128, 2, 8], F32, st)
            nega = sb("nega", [128, 2, 8], F32, st)
            dma("sp", dtb[:, 0, :], dt_b_f[0].partition_broadcast(128), w=["dtb"], key="dtb")
            dma("sp", dtb[:, 1, :], dt_b_b[0].partition_broadcast(128), w=["dtb"], key="dtb")
            dma("sp", nega[:, 0, :], a_log_f[0].partition_broadcast(128), w=["nega"], key="nega")
            dma("sp", nega[:, 1, :], a_log_b[0].partition_broadcast(128), w=["nega"], key="nega")
            op("act", lambda: A.activation(out=nega[:], in_=nega[:], func=AF.Exp), r=["nega"], w=["nega"])
            op("act", lambda: A.mul(out=nega[:], in_=nega[:], mul=-1.0), r=["nega"], w=["nega"])
            abl = sb("abl", [128, NT + 1, 2, 2, 8], F32, st)
            xa = sb("xa", [128, NT + 1, 2, 8], F32, st)
            op("dve", lambda: V.memset(abl[:], 0.0), w=["abl"])
            dma("sp", abl[0:NM, 0], ab_s[0:NM, :].rearrange("p (d t h) -> p d t h", d=2, t=2), w=["abl"], key="abl")
            for hh in range(2):
                dma("sp", abl[:, 1 + hh * 8:9 + hh * 8], ab_s[NM + hh * 1024:NM + (hh + 1) * 1024, :].rearrange("(t p) (d u h) -> p t d u h", p=128, d=2, u=2),
                    w=["abl"], key="abl")
            NT1 = NT + 1
            op("dve", lambda: V.tensor_tensor(out=xa[:], in0=abl[:, :, :, 0, :], in1=dtb[:].unsqueeze(1).to_broadcast([128, NT1, 2, 8]), op=ALU.add), r=["abl", "dtb"], w=["xa"])
            op("act", lambda: A.activation(out=xa[:], in_=xa[:], func=AF.Exp), r=["xa"], w=["xa"])
            op("act", lambda: A.activation(out=xa[:], in_=xa[:], func=AF.Ln, bias=onec[:, :], scale=1.0), r=["xa", "onec"], w=["xa"])
            op("dve", lambda: V.tensor_tensor(out=gball[:, :, :, 0, :], in0=xa[:], in1=nega[:].unsqueeze(1).to_broadcast([128, NT1, 2, 8]), op=ALU.mult), r=["xa", "nega"], w=["gball"])
            op("act", lambda: A.activation(out=gball[:, :, :, 1, :], in_=abl[:, :, :, 1, :], func=AF.Sigmoid), r=["abl"], w=["gball"])
            if debug:
                gb_d = scratch("gb_d", [L, 32], F32)
                for t in range(NT + 1):
                    n, p0 = (NM, 0) if t == 0 else (128, NM + (t - 1) * 128)
                    dma("sp", gb_d[p0:p0 + n, :].rearrange("p (d t h) -> p d t h", d=2, t=2), gball[0:n, t], r=["gball"], key="dbg_gb")

            ob_s = scratch("ob_s", [NT, 128, NH, 128], F32)
            with contextlib.ExitStack() as st2:
                F4 = lambda nm, dt=F32: sb(nm, [128, 4, 128], dt, st2)
                SB = {}
                NSTR = 4
                for sid in range(NSTR):
                    for i in range(2):
                        for nm in ("qTt", "kTt", "vTt"):
                            SB[f"{nm}{i}_{sid}"] = F4(f"{nm}{i}_{sid}", BF16)
                    for nm in ("ktm", "vtm", "Sb_"):
                        SB[f"{nm}_{sid}"] = F4(f"{nm}_{sid}", BF16)
                    for nm in ("Gm", "oTt", "Sf"):
                        SB[f"{nm}_{sid}"] = F4(f"{nm}_{sid}", F32)
                    for nm in ("gcc", "egc", "bgc", "gend", "kds"):
                        SB[f"{nm}_{sid}"] = sb(f"{nm}_{sid}", [128, 4], F32, st2)
                    for nm in ("diff", "DL", "DU", "u_sb", "egb"):
                        SB[f"{nm}_{sid}"] = F4(f"{nm}_{sid}")
                    for nm in ("Ab", "ATb", "QKT", "TTb", "vbt", "kbg", "kdec", "nwT", "qdT", "vnew", "Pb0", "Pb1", "PTb0", "PTb1"):
                        SB[f"{nm}_{sid}"] = F4(f"{nm}_{sid}", BF16)

                def p4_stream(sid, pX, pY):
                    d_, hg = sid // 2, sid % 2
                    H0 = hg * 4
                    K_ = lambda nm: f"{nm}_{sid}"
                    B_ = lambda nm: SB[f"{nm}_{sid}"]
                    Gm, gcc, egc, bgc, gend, kds = B_("Gm"), B_("gcc"), B_("egc"), B_("bgc"), B_("gend"), B_("kds")
                    diff, DL, DU, u_sb, egb = [B_(x) for x in ("diff", "DL", "DU", "u_sb", "egb")]
                    Ab, ATb, QKT, TTb, vbt, kbg, kdec, nwT, qdT, vnew = [B_(x) for x in ("Ab", "ATb", "QKT", "TTb", "vbt", "kbg", "kdec", "nwT", "qdT", "vnew")]
                    ktm, vtm, Sb_, oTt, Sf = B_("ktm"), B_("vtm"), B_("Sb_"), B_("oTt"), B_("Sf")
                    kX, kY = K_("pX"), K_("pY")
                    S.excl.update([kX, kY])
                    pYb = pY[:].bitcast(BF16)
                    op("dve", lambda: V.memset(Sf[:], 0.0), r=[], w=[K_("Sf")])
                    op("dve", lambda: V.memset(Sb_[:], 0.0), r=[], w=[K_("Sb_")])
                    order = list(range(0, NT + 1)) if d_ == 0 else list(range(NT, 0, -1))
                    mC, mA, mQ = masks[:, 0 + d_, :], masks[:, 2 + d_, :], masks[:, 4 + d_, :]
                    o_dst = of_s if d_ == 0 else ob_s
                    for it, t in enumerate(order):
                        n, p0 = (NM, 0) if t == 0 else (128, NM + (t - 1) * 128)
                        li = it % 2
                        qTl, kTl, vTl = B_(f"qTt{li}"), B_(f"kTt{li}"), B_(f"vTt{li}")
                        qn, kn, vn_ = K_(f"qTt{li}"), K_(f"kTt{li}"), K_(f"vTt{li}")
                        for (dst, src, nm) in ((qTl, qT_s, qn), (kTl, kT_s, kn), (vTl, vT_s, vn_)):
                            dma("sp", dst[:, :, 0:n], src[H0:H0 + 4, :, p0:p0 + n].rearrange("h d p -> d h p"), w=[nm])
                        yield
                        for (srcT, dstm, sn, dn) in ((kTl, ktm, kn, K_("ktm")), (vTl, vtm, vn_, K_("vtm"))):
                            for hi in range(4):
                                op("pe", lambda: PE.transpose(pYb[0:n, hi, 0:128], srcT[:, hi, 0:n], identb[:, :]), r=[sn, "identb"], w=[kY])
                            op("act", lambda: A.copy(out=dstm[0:n], in_=pYb[0:n, :, 0:128]), r=[kY], w=[dn])
                            yield
                        gcol = gball[0:n, t, d_, 0, H0:H0 + 4]
                        bcol = gball[0:n, t, d_, 1, H0:H0 + 4]
                        op("dve", lambda: V.tensor_tensor(out=Gm[0:n, :, 0:n], in0=mC[0:n, 0:n].unsqueeze(1).to_broadcast([n, 4, n]),
                                                          in1=gcol.unsqueeze(2).to_broadcast([n, 4, n]), op=ALU.mult), r=["masks", "gball"], w=[K_("Gm")])
                        op("pe", lambda: PE.matmul(pX[0:n, 0, 0:4], lhsT=mC[0:n, 0:n], rhs=gcol, start=True, stop=True), r=["masks", "gball"], w=[kX])
                        op("dve", lambda: V.tensor_copy(out=gcc[0:n], in_=pX[0:n, 0, 0:4]), r=[kX], w=[K_("gcc")])
                        op("act", lambda: A.activation(out=egc[0:n], in_=gcc[0:n], func=AF.Exp), r=[K_("gcc")], w=[K_("egc")])
                        op("dve", lambda: V.tensor_tensor(out=bgc[0:n], in0=egc[0:n], in1=bcol, op=ALU.mult), r=[K_("egc"), "gball"], w=[K_("bgc")])
                        yield
                        if t == 0:
                            chunks, ends = [(0, NM)], [NM - 1]
                        elif d_ == 0:
                            chunks, ends = [(0, 64), (64, 64)], [63, 127]
                        else:
                            chunks, ends = [(64, 64), (0, 64)], [64, 0]
                        if n == 128:
                            op("pe", lambda: PE.matmul(pX[:, :, 0:n], lhsT=onesf[0:n, :], rhs=Gm[0:n, :, 0:n], start=True, stop=True), r=["onesf", K_("Gm")], w=[kX])
                        else:
                            for hi in range(4):
                                op("pe", lambda: PE.matmul(pX[:, hi, 0:n], lhsT=onesf[0:n, :], rhs=Gm[0:n, hi, 0:n], start=True, stop=True), r=["onesf", K_("Gm")], w=[kX])
                        for hi in range(4):
                            op("pe", lambda: PE.matmul(pY[0:n, hi, 0:n], lhsT=kTl[:, hi, 0:n], rhs=kTl[:, hi, 0:n], start=True, stop=True), r=[kn], w=[kY])
                        op("dve", lambda: V.tensor_tensor(out=diff[0:n, :, 0:n], in0=gcc[0:n, :].unsqueeze(2).to_broadcast([n, 4, n]),
                                                          in1=pX[0:n, :, 0:n], op=ALU.subtract), r=[K_("gcc"), kX], w=[K_("diff")])
                        op("act", lambda: A.activation(out=egb[:, :, 0:n], in_=pX[:, :, 0:n], func=AF.Exp), r=[kX], w=[K_("egb")])
                        for ci, (r0, cn) in enumerate(chunks):
                            op("dve", lambda: V.tensor_copy(out=gend[r0:r0 + cn, :], in_=pX[r0:r0 + cn, :, ends[ci]]), r=[kX], w=[K_("gend")])
                        yield
                        op("dve", lambda: V.scalar_tensor_tensor(out=DL[0:n, :, 0:n], in0=diff[0:n, :, 0:n], scalar=0.0,
                                                                 in1=mA[0:n, 0:n].unsqueeze(1).to_broadcast([n, 4, n]), op0=ALU.min, op1=ALU.add),
                           r=[K_("diff"), "masks"], w=[K_("DL")])
                        op("dve", lambda: V.scalar_tensor_tensor(out=DU[0:n, :, 0:n], in0=diff[0:n, :, 0:n], scalar=0.0,
                                                                 in1=mQ[0:n, 0:n].unsqueeze(1).to_broadcast([n, 4, n]), op0=ALU.max, op1=ALU.add),
                           r=[K_("diff"), "masks"], w=[K_("DU")])
                        op("act", lambda: A.activation(out=DL[0:n, :, 0:n], in_=DL[0:n, :, 0:n], func=AF.Exp), r=[K_("DL")], w=[K_("DL")])
                        op("act", lambda: A.activation(out=DU[0:n, :, 0:n], in_=DU[0:n, :, 0:n], func=AF.Exp, scale=-1.0), r=[K_("DU")], w=[K_("DU")])
                        for hi in range(4):
                            op("pe", lambda: PE.matmul(pX[0:n, hi, 0:n], lhsT=kTl[:, hi, 0:n], rhs=qTl[:, hi, 0:n], start=True, stop=True), r=[kn, qn], w=[kX])
                        op("dve", lambda: V.tensor_tensor(out=kds[0:n, :], in0=gend[0:n, :], in1=gcc[0:n, :], op=ALU.subtract), r=[K_("gend"), K_("gcc")], w=[K_("kds")])
                        op("act", lambda: A.activation(out=kds[0:n, :], in_=kds[0:n, :], func=AF.Exp), r=[K_("kds")], w=[K_("kds")])
                        yield
                        op("dve", lambda: V.tensor_tensor(out=diff[0:n, :, 0:n], in0=pY[0:n, :, 0:n], in1=DL[0:n, :, 0:n], op=ALU.mult), r=[kY, K_("DL")], w=[K_("diff")])
                        op(ge, lambda: GE.tensor_tensor(out=Ab[0:n, :, 0:n], in0=diff[0:n, :, 0:n], in1=bcol.unsqueeze(2).to_broadcast([n, 4, n]), op=ALU.mult),
                           r=[K_("diff"), "gball"], w=[K_("Ab")])
                        op("dve", lambda: V.tensor_tensor(out=QKT[0:n, :, 0:n], in0=pX[0:n, :, 0:n], in1=DU[0:n, :, 0:n], op=ALU.mult), r=[kX, K_("DU")], w=[K_("QKT")])
                        yield
                        for hi in range(4):
                            op("pe", lambda: PE.transpose(pYb[0:n, hi, 0:n], Ab[0:n, hi, 0:n], identb[0:n, 0:n]), r=[K_("Ab"), "identb"], w=[kY])
                        op("act", lambda: A.copy(out=ATb[0:n, :, 0:n], in_=pYb[0:n, :, 0:n]), r=[kY], w=[K_("ATb")])
                        yield
                        op("dve", lambda: V.tensor_tensor(out=TTb[0:n, :, 0:n], in0=identf[0:n, 0:n].unsqueeze(1).to_broadcast([n, 4, n]), in1=ATb[0:n, :, 0:n], op=ALU.subtract),
                           r=["identf", K_("ATb")], w=[K_("TTb")])
                        yield
                        Pc, PTc, Pn_, PTn_ = Ab, ATb, K_("Ab"), K_("ATb")
                        for lvl in range(1, 6):
                            Pd, PTd = B_(f"Pb{lvl % 2}"), B_(f"PTb{lvl % 2}")
                            Pdn, PTdn = K_(f"Pb{lvl % 2}"), K_(f"PTb{lvl % 2}")
                            for hi in range(4):
                                op("pe", lambda: PE.matmul(pX[0:n, hi, 0:n], lhsT=PTc[0:n, hi, 0:n], rhs=Pc[0:n, hi, 0:n], start=True, stop=True), r=[Pn_, PTn_], w=[kX])
                            if lvl < 5:
                                for hi in range(4):
                                    op("pe", lambda: PE.matmul(pY[0:n, hi, 0:n], lhsT=Pc[0:n, hi, 0:n], rhs=PTc[0:n, hi, 0:n], start=True, stop=True), r=[Pn_, PTn_], w=[kY])
                            op("act", lambda: A.copy(out=Pd[0:n, :, 0:n], in_=pX[0:n, :, 0:n]), r=[kX], w=[Pdn])
                            if lvl < 5:
                                op("act", lambda: A.copy(out=PTd[0:n, :, 0:n], in_=pY[0:n, :, 0:n]), r=[kY], w=[PTdn])
                            yield
                            for hi in range(4):
                                op("pe", lambda: PE.matmul(pX[0:n, hi, 0:n], lhsT=Pd[0:n, hi, 0:n], rhs=TTb[0:n, hi, 0:n], start=True, stop=True), r=[Pdn, K_("TTb")], w=[kX])
                            op("dve", lambda: V.tensor_tensor(out=TTb[0:n, :, 0:n], in0=TTb[0:n, :, 0:n], in1=pX[0:n, :, 0:n], op=ALU.add), r=[K_("TTb"), kX], w=[K_("TTb")])
                            yield
                            Pc, PTc, Pn_, PTn_ = Pd, PTd, Pdn, PTdn
                        op(ge, lambda: GE.tensor_tensor(out=vbt[0:n], in0=vtm[0:n], in1=bcol.unsqueeze(2).to_broadcast([n, 4, 128]), op=ALU.mult), r=[K_("vtm"), "gball"], w=[K_("vbt")])
                        op(ge, lambda: GE.tensor_tensor(out=kbg[0:n], in0=ktm[0:n], in1=bgc[0:n, :].unsqueeze(2).to_broadcast([n, 4, 128]), op=ALU.mult), r=[K_("ktm"), K_("bgc")], w=[K_("kbg")])
                        op(ge, lambda: GE.tensor_tensor(out=kdec[0:n], in0=ktm[0:n], in1=kds[0:n, :].unsqueeze(2).to_broadcast([n, 4, 128]), op=ALU.mult), r=[K_("ktm"), K_("kds")], w=[K_("kdec")])
                        op(ge, lambda: GE.tensor_tensor(out=qdT[:, :, 0:n], in0=qTl[:, :, 0:n], in1=egb[:, :, 0:n], op=ALU.mult), r=[qn, K_("egb")], w=[K_("qdT")])
                        yield
                        for hi in range(4):
                            op("pe", lambda: PE.matmul(pX[0:n, hi, :], lhsT=TTb[0:n, hi, 0:n], rhs=vbt[0:n, hi, :], start=True, stop=True), r=[K_("TTb"), K_("vbt")], w=[kX])
                        for hi in range(4):
                            op("pe", lambda: PE.matmul(pY[:, hi, 0:n], lhsT=kbg[0:n, hi, :], rhs=TTb[0:n, hi, 0:n], start=True, stop=True), r=[K_("TTb"), K_("kbg")], w=[kY])
                        op("act", lambda: A.copy(out=u_sb[0:n], in_=pX[0:n]), r=[kX], w=[K_("u_sb")])
                        op("act", lambda: A.mul(out=nwT[:, :, 0:n], in_=pY[:, :, 0:n], mul=-1.0), r=[kY], w=[K_("nwT")])
                        yield
                        for ci, (r0, cn) in enumerate(chunks):
                            rs_ = slice(r0, r0 + cn)
                            for hi in range(4):
                                op("pe", lambda: PE.matmul(pX[rs_, hi, :], lhsT=nwT[:, hi, rs_], rhs=Sb_[:, hi, :], start=True, stop=True), r=[K_("nwT"), K_("Sb_")], w=[kX])
                            op("dve", lambda: V.tensor_tensor(out=vnew[rs_], in0=u_sb[rs_], in1=pX[rs_], op=ALU.add), r=[K_("u_sb"), kX], w=[K_("vnew")])
                            yield
                            if t > 0:
                                for hi in range(4):
                                    op("pe", lambda: PE.matmul(pY[:, hi, 0:cn], lhsT=Sb_[:, hi, :], rhs=qdT[:, hi, rs_], start=True, stop=False), r=[K_("Sb_"), K_("qdT")], w=[kY])
                                    op("pe", lambda: PE.matmul(pY[:, hi, 0:cn], lhsT=vnew[rs_, hi, :], rhs=QKT[rs_, hi, rs_], start=False, stop=True), r=[K_("vnew"), K_("QKT")], w=[kY])
                                op("act", lambda: A.copy(out=oTt[:, :, rs_], in_=pY[:, :, 0:cn]), r=[kY], w=[K_("oTt")])
                            for hi in range(4):
                                op("pe", lambda: PE.matmul(pX[:, hi, :], lhsT=kdec[rs_, hi, :], rhs=vnew[rs_, hi, :], start=True, stop=True), r=[K_("kdec"), K_("vnew")], w=[kX])
                            op("dve", lambda: V.tensor_tensor(out=Sf[:], in0=Sf[:], in1=egb[:, :, ends[ci]].unsqueeze(2).to_broadcast([128, 4, 128]), op=ALU.mult),
                               r=[K_("Sf"), K_("egb")], w=[K_("Sf")])
                            op("dve", lambda: V.tensor_tensor(out=Sf[:], in0=Sf[:], in1=pX[:], op=ALU.add), r=[K_("Sf"), kX], w=[K_("Sf")])
                            op("act", lambda: A.copy(out=Sb_[:], in_=Sf[:]), r=[K_("Sf")], w=[K_("Sb_")])
                            yield
                        if t == 0:
                            continue
                        c0 = (t - 1) * 128
                        dma("sp", o_dst[t - 1][:, H0:H0 + 4, :], oTt[:], r=[K_("oTt")], w=[("osc", d_, hg, t)], key=K_("oTt"))
                        yield

                with contextlib.ExitStack() as st1:
                    pXs = [ps(f"p4X{i}", [128, 4, 128], F32, st1) for i in range(NSTR)]
                    pYs = [ps(f"p4Y{i}", [128, 4, 128], F32, st1) for i in range(NSTR)]
                    gens = [p4_stream(i, pXs[i], pYs[i]) for i in range(NSTR)]
                    alive = [True] * NSTR
                    nstep = 0
                    while any(alive):
                        for i in range(NSTR):
                            if alive[i]:
                                try:
                                    next(gens[i])
                                except StopIteration:
                                    alive[i] = False
                                nstep += 1
                                if nstep % 18 == 0:
                                    pc_step(1)
                    S.barrier()
            F8 = lambda nm, dt=F32: sb(nm, [128, 8, 128], dt, st)
            ofl = [F8(f"ofl{i}") for i in range(2)]
            obl = [F8(f"obl{i}") for i in range(2)]
            osq2 = [F8(f"osq{i}") for i in range(2)]
            ors2 = [F8(f"ors{i}") for i in range(2)]
            zall = sb("zall", [128, 8, SEQ], BF16, st)
            for h in range(8):
                dma("sp", zall[:, h, :], z_s[h], w=["zall"], key="zall")
            with contextlib.ExitStack() as st1:
                pss = [ps(f"pss{i}", [128, 4, 128], F32, st1) for i in range(4)]
                for t in (range(1, NT + 1) if os.environ.get('P4COMB', '1') == '1' else []):
                    c0 = (t - 1) * 128
                    i = t % 2
                    osq, ors, kosq, kors = osq2[i], ors2[i], f"osq{i}", f"ors{i}"
                    dma("sp", ofl[i][:], of_s[t - 1], r=[("osc", 0, 0, t), ("osc", 0, 1, t)], w=[f"ofl{i}"])
                    dma("sp", obl[i][:], ob_s[t - 1], r=[("osc", 1, 0, t), ("osc", 1, 1, t)], w=[f"obl{i}"])
                    op("dve", lambda: V.tensor_tensor(out=ofl[i][:], in0=ofl[i][:], in1=obl[i][:], op=ALU.add), r=[f"ofl{i}", f"obl{i}"], w=[f"ofl{i}"])
                    op("act", lambda: A.activation(out=osq[:], in_=ofl[i][:], func=AF.Square), r=[f"ofl{i}"], w=[kosq])
                    for hg in range(2):
                        pi = (2 * t + hg) % 4
                        op("pe", lambda: PE.matmul(pss[pi][:], lhsT=onesf[:], rhs=osq[:, hg * 4:hg * 4 + 4, :], start=True, stop=True), r=["onesf", kosq], w=[f"pss{pi}"])
                        op("act", lambda: A.activation(out=ors[:, hg * 4:hg * 4 + 4, :], in_=pss[pi][:], func=AF.Sqrt, scale=1.0 / 128, bias=epst[:]), r=[f"pss{pi}", "epst"], w=[kors])
                    op("dve", lambda: V.reciprocal(out=ors[:], in_=ors[:]), r=[kors], w=[kors])
                    op("dve", lambda: V.scalar_tensor_tensor(out=ofl[i][:], in0=ofl[i][:], scalar=gout[:, 0:1], in1=ors[:], op0=ALU.mult, op1=ALU.mult),
                       r=[f"ofl{i}", "gout", kors], w=[f"ofl{i}"])
                    op("pool", lambda: G.tensor_tensor(out=ogT[:, :, c0:c0 + 128], in0=ofl[i][:], in1=zall[:, :, c0:c0 + 128], op=ALU.mult), r=[f"ofl{i}", "zall"], w=[("ogT", t)])
                S.barrier()
            if debug:
                og_d = scratch("og_d", [NH, 128, SEQ], BF16)
                for h in range(8):
                    dma("sp", og_d[h], ogT[:, h, :], r=[("ogT", t) for t in range(1, NT + 1)], key="dbg_og")
            S.barrier()
        if stage < 4:
            S.finish()
            return nc, dbg
        h2_s = scratch("h2_s", [SEQ, D], F32)
        hn2_s = scratch("hn2_s", [SEQ, D], BF16)
        IOA = bass.IndirectOffsetOnAxis
        call = sb("call", [128, NT, 2], F32)
        dall = sb("dall", [128, NT, 2], I32)
        NB = 3
        wgu = [sb(f"wgu{i}", [128, 2, 8, DE], BF16) for i in range(NB)]
        wde = [sb(f"wde{i}", [128, 2, D], BF16) for i in range(NB)]

        PRECAST = os.environ.get("PRECAST", "1") == "1"

        def load_expert(e, part="both"):
            wi = e % NB
            if PRECAST:
                rk_ = [("wq", e // 2)]
                if part in ("both", "gu"):
                    dma("pool", wgu[wi][:, 0], wq_g[e].rearrange("p (kt j) -> p kt j", kt=8), r=rk_, w=[f"wgu{wi}"], key=f"wgu{wi}")
                    dma("pool", wgu[wi][:, 1], wq_u[e].rearrange("p (kt j) -> p kt j", kt=8), r=rk_, w=[f"wgu{wi}"], key=f"wgu{wi}")
                if part in ("both", "d"):
                    dma("pool", wde[wi][:], wq_d[e].rearrange("p (jt d) -> p jt d", jt=2), r=rk_, w=[f"wde{wi}"])
                return
            dma("pool", wgu[wi][:, 0], w_gate_e[0, e].rearrange("(p kt) j -> p kt j", kt=8), w=[f"wgu{wi}"], key=f"wgu{wi}")
            dma("pool", wgu[wi][:, 1], w_up_e[0, e].rearrange("(p kt) j -> p kt j", kt=8), w=[f"wgu{wi}"], key=f"wgu{wi}")
            dma("pool", wde[wi][:], w_down_e[0, e].rearrange("(jt p) d -> p jt d", p=128), w=[f"wde{wi}"])

        pc_step(1000)
        if stage >= 5:
            for e in range(NB):
                load_expert(e)
        with contextlib.ExitStack() as st:
            mrgT = sb("mrgT2", [128, 8, SEQ], BF16, st)
            for dt_ in range(8):
                dma("sp", mrgT[:, dt_, :], mrg_s[dt_], r=["mrg_s"], w=[("mrgT", dt_)], key=f"mrgT2_{dt_}")
            wd = sb("wd", [128, 8, D], BF16, st)
            wo = sb("wo", [128, 8, D], BF16, st)
            dma("pool", wd[:], w_delta[0].rearrange("(h p) d -> p h d", p=128), w=["wd"])
            dma("pool", wo[:], w_out[0].rearrange("(h p) d -> p h d", p=128), w=["wo"])
            sgd = [sb(f"sgd{i}", [128, SEQ], BF16, st) for i in range(2)]
            tmpm = sb("tmpm", [128, 512], F32, st)
            g2b = sb("g2b", [128, D], F32, st)
            dma("sp", g2b[:], norm2_g[0].partition_broadcast(128), w=["g2b"])
            wr = sb("wr", [128, 8, 72], F32, st)
            with nc.allow_non_contiguous_dma(reason="small router weights"):
                dma("sp", wr[:, :, 0:8], w_rg[0].rearrange("(kt p) g -> p kt g", p=128), w=["wr"], key="wr")
                dma("sp", wr[:, :, 8:72], w_re[0].rearrange("(kt p) g -> p kt g", p=128), w=["wr"], key="wr")
            rbias = sb("rbias", [128, 72], F32, st)
            dma("sp", rbias[:, 0:8], b_rg[0].partition_broadcast(128), w=["rbias"], key="rbias")
            dma("sp", rbias[:, 8:72], b_re[0].partition_broadcast(128), w=["rbias"], key="rbias")
            ustf = sb("ustf", [128, 128], F32, st)
            dma("sp", ustf[:], c_masks[6], w=["ustf"])
            ustb = sb("ustb", [128, 128], BF16, st)
            op("act", lambda: A.copy(out=ustb[:], in_=ustf[:]), r=["ustf"], w=["ustb"])
            onesb2 = sb("onesb2", [128, 128], BF16, st)
            op("dve", lambda: V.memset(onesb2[:], 1.0), w=["onesb2"])
            ecap = sb("ecap", [128, 64], F32, st)
            dma("sp", ecap[:], c_ecap, w=["ecap"])
            Mall = sb("Mall", [128, NT, 64], BF16, st)
            lgall = sb("lgall", [128, NT, 72], F32, st)
            sm = {"ss": sb("sm_ss", [128, 1], F32, st)}
            stA = contextlib.ExitStack()
            xr = [sb(f"xr{i}", [128, D], F32, stA) for i in range(2)]
            h2t = [sb(f"h2t{i}", [128, D], F32, stA) for i in range(2)]
            hn2f2 = [sb(f"hn2f{i}", [128, D], F32, stA) for i in range(2)]
            hn2b = [sb(f"hn2b{i}", [128, D], BF16, stA) for i in range(2)]
            junk2 = sb("junk2", [128, D], BF16, stA)
            hn2T = sb("hn2T", [128, 8, 128], F32, stA)
            with contextlib.ExitStack() as st1:
                pa = [ps(f"pb{i}", [128, 512], F32, st1) for i in range(3)]
                ptf = [ps(f"ptf{i}", [128, 4, 128], F32, st1) for i in range(2)]
                plg = ps("plg", [128, 72], F32, st1)
                prk2 = [ps(f"prk{i}", [128, 8, 64], F32, st1) for i in range(2)]
                npa = 0
                for dt_ in range(8):
                    gi_ = dt_ % 2
                    dma("sp", sgd[gi_][:], gd_s[dt_], w=[f"sgd{gi_}"])
                    for bi in range(4):
                        ai = npa % 3
                        npa += 1
                        for h in range(8):
                            op("pe", lambda: PE.matmul(pa[ai][:, :], lhsT=wd[:, h, dt_ * 128:(dt_ + 1) * 128], rhs=ogT[:, h, bi * 512:(bi + 1) * 512],
                                                       start=(h == 0), stop=(h == 7)),
                               r=["wd"] + [("ogT", t) for t in range(1, NT + 1)], w=[f"pb{ai}"])
                        op("dve", lambda: V.tensor_tensor(out=tmpm[:], in0=pa[ai][:, :], in1=sgd[gi_][:, bi * 512:(bi + 1) * 512], op=ALU.mult),
                           r=[f"pb{ai}", f"sgd{gi_}"], w=["tmpm"])
                        op("dve", lambda: V.tensor_tensor(out=mrgT[:, dt_, bi * 512:(bi + 1) * 512], in0=tmpm[:], in1=mrgT[:, dt_, bi * 512:(bi + 1) * 512], op=ALU.add),
                           r=["tmpm", ("mrgT", dt_)], w=[("mrgT", dt_)])
                mrg_all = [("mrgT", d2) for d2 in range(8)]
                def LA(t):
                    i = t % 2
                    hn2f = hn2f2[i]
                    nonlocal_npa = None
                    dma("sp", xr[i][:], x[t * 128:(t + 1) * 128, :], w=[f"xr{i}"])
                    for half in range(2):
                        ai = cntA[0] % 3
                        cntA[0] += 1
                        for dt_ in range(8):
                            op("pe", lambda: PE.matmul(pa[ai][:, :], lhsT=mrgT[:, dt_, t * 128:(t + 1) * 128], rhs=wo[:, dt_, half * 512:(half + 1) * 512],
                                                       start=(dt_ == 0), stop=(dt_ == 7)), r=["wo"] + mrg_all, w=[f"pb{ai}"])
                        op("dve", lambda: V.tensor_tensor(out=h2t[i][:, half * 512:(half + 1) * 512], in0=pa[ai][:, :], in1=xr[i][:, half * 512:(half + 1) * 512], op=ALU.add),
                           r=[f"pb{ai}", f"xr{i}"], w=[f"h2t{i}"])
                    dma("sp", h2_s[t * 128:(t + 1) * 128, :], h2t[i][:], r=[f"h2t{i}"], w=["h2_s"], key="h2_s")
                    op("act", lambda: A.activation(out=junk2[:], in_=h2t[i][:], func=AF.Square, accum_out=sm["ss"][:]), r=[f"h2t{i}"], w=["junk2", "sm_ss"])
                    op("act", lambda: A.activation(out=sm["ss"][:], in_=sm["ss"][:], func=AF.Sqrt, scale=1.0 / D, bias=epst[:]), r=["sm_ss", "epst"], w=["sm_ss"])
                    op("dve", lambda: V.reciprocal(out=sm["ss"][:], in_=sm["ss"][:]), r=["sm_ss"], w=["sm_ss"])
                    op("dve", lambda: V.scalar_tensor_tensor(out=hn2f[:], in0=h2t[i][:], scalar=sm["ss"][:, 0:1], in1=g2b[:], op0=ALU.mult, op1=ALU.mult),
                       r=[f"h2t{i}", "sm_ss", "g2b"], w=[f"hn2f{i}"])
                    op("act", lambda: A.copy(out=hn2b[i][:], in_=hn2f[:]), r=[f"hn2f{i}"], w=[f"hn2b{i}"])
                    dma("sp", hn2_s[t * 128:(t + 1) * 128, :], hn2b[i][:], r=[f"hn2b{i}"], w=["hn2_s"], key=f"hn2b{i}")

                def LB(t):
                    i = t % 2
                    hn2f = hn2f2[i]
                    for kt in range(8):
                        op("pe", lambda: PE.transpose(ptf[kt // 4][:, kt % 4, :], hn2f[:, kt * 128:(kt + 1) * 128], identf[:]), r=[f"hn2f{i}", "identf"], w=[f"ptf{kt // 4}"])
                    for q_ in range(2):
                        op("act", lambda: A.copy(out=hn2T[:, q_ * 4:q_ * 4 + 4, :], in_=ptf[q_][:]), r=[f"ptf{q_}"], w=["hn2T"])
                    for kt in range(8):
                        op("pe", lambda: PE.matmul(plg[:, :], lhsT=hn2T[:, kt, :], rhs=wr[:, kt, :], start=(kt == 0), stop=(kt == 7)), r=["hn2T", "wr"], w=["plg"])
                    op("dve", lambda: V.tensor_tensor(out=lgall[:, t, :], in0=plg[:, :], in1=rbias[:], op=ALU.add), r=["plg", "rbias"], w=["lgall"])

                cntA = [npa]
                for t in range(NT + 1):
                    if t < NT:
                        LA(t)
                    if t >= 1:
                        LB(t - 1)
                S.barrier()
                stA.close()
                hn2b = [sb(f"hn2c{i}", [128, D], BF16, st) for i in range(2)]
                TT_ = lambda o, a, b, o_: V.tensor_tensor(out=o, in0=a, in1=b, op=o_)
                R = {nm: sb("rt_" + nm, [128, NT, w_], F32, st) for nm, w_ in
                     (("gmax", 1), ("ge", 8), ("gsum", 1), ("pg", 1), ("ohg", 8), ("tmp", 64), ("elg", 8), ("m1", 1), ("oh1", 8), ("el2", 8),
                      ("m2", 1), ("oh2", 8), ("d12", 1), ("w1", 1), ("M1", 64), ("M2", 64), ("rk", 64), ("t3", 64), ("d1f", 1), ("d2f", 1))}
                k = lambda nm: "rt_" + nm
                lgg = lgall[:, :, 0:8]
                op("dve", lambda: V.tensor_reduce(out=R["gmax"][:, :, 0], in_=lgg, axis=AX.X, op=ALU.max), r=["lgall"], w=[k("gmax")])
                op("dve", lambda: TT_(R["ge"][:], lgg, R["gmax"][:].to_broadcast([128, NT, 8]), ALU.subtract), r=["lgall", k("gmax")], w=[k("ge")])
                op("act", lambda: A.activation(out=R["ge"][:], in_=R["ge"][:], func=AF.Exp), r=[k("ge")], w=[k("ge")])
                op("dve", lambda: V.tensor_reduce(out=R["gsum"][:, :, 0], in_=R["ge"][:], axis=AX.X, op=ALU.add), r=[k("ge")], w=[k("gsum")])
                op("dve", lambda: V.reciprocal(out=R["pg"][:], in_=R["gsum"][:]), r=[k("gsum")], w=[k("pg")])
                op("dve", lambda: TT_(R["ohg"][:], lgg, R["gmax"][:].to_broadcast([128, NT, 8]), ALU.is_equal), r=["lgall", k("gmax")], w=[k("ohg")])
                el4 = lgall[:, :, 8:72].rearrange("p t (g e) -> p t g e", g=8)
                tmp4 = R["tmp"][:].rearrange("p t (g e) -> p t g e", g=8)
                op("dve", lambda: TT_(tmp4, el4, R["ohg"][:].unsqueeze(3).to_broadcast([128, NT, 8, 8]), ALU.mult), r=["lgall", k("ohg")], w=[k("tmp")])
                op("dve", lambda: V.tensor_reduce(out=R["elg"][:], in_=tmp4.rearrange("p t g e -> p t e g"), axis=AX.X, op=ALU.add), r=[k("tmp")], w=[k("elg")])
                op("dve", lambda: V.tensor_reduce(out=R["m1"][:, :, 0], in_=R["elg"][:], axis=AX.X, op=ALU.max), r=[k("elg")], w=[k("m1")])
                op("dve", lambda: TT_(R["oh1"][:], R["elg"][:], R["m1"][:].to_broadcast([128, NT, 8]), ALU.is_equal), r=[k("elg"), k("m1")], w=[k("oh1")])
                op("dve", lambda: V.scalar_tensor_tensor(out=R["el2"][:], in0=R["oh1"][:], scalar=-1.0e30, in1=R["elg"][:], op0=ALU.mult, op1=ALU.add),
                   r=[k("oh1"), k("elg")], w=[k("el2")])
                op("dve", lambda: V.tensor_reduce(out=R["m2"][:, :, 0], in_=R["el2"][:], axis=AX.X, op=ALU.max), r=[k("el2")], w=[k("m2")])
                op("dve", lambda: TT_(R["oh2"][:], R["el2"][:], R["m2"][:].to_broadcast([128, NT, 8]), ALU.is_equal), r=[k("el2"), k("m2")], w=[k("oh2")])
                op("dve", lambda: TT_(R["d12"][:], R["m1"][:], R["m2"][:], ALU.subtract), r=[k("m1"), k("m2")], w=[k("d12")])
                op("act", lambda: A.activation(out=R["w1"][:], in_=R["d12"][:], func=AF.Sigmoid), r=[k("d12")], w=[k("w1")])
                op("dve", lambda: TT_(call[:, :, 0:1], R["pg"][:], R["w1"][:], ALU.mult), r=[k("pg"), k("w1")], w=["call"])
                op("dve", lambda: TT_(call[:, :, 1:2], R["pg"][:], call[:, :, 0:1], ALU.subtract), r=[k("pg"), "call"], w=["call"])
                for (Mn, ohn) in (("M1", "oh1"), ("M2", "oh2")):
                    op("dve", lambda: TT_(R[Mn][:].rearrange("p t (g e) -> p t g e", g=8), R["ohg"][:].unsqueeze(3).to_broadcast([128, NT, 8, 8]),
                                          R[ohn][:].unsqueeze(2).to_broadcast([128, NT, 8, 8]), ALU.mult), r=[k("ohg"), k(ohn)], w=[k(Mn)])
                op("dve", lambda: TT_(Mall[:], R["M1"][:], R["M2"][:], ALU.add), r=[k("M1"), k("M2")], w=["Mall"])
                for t in range(NT):
                    pr = prk2[t // 8]
                    prn = f"prk{t // 8}"
                    op("pe", lambda: PE.matmul(pr[:, t % 8, :], lhsT=ustb[:], rhs=Mall[:, t, :], start=True, stop=(t == 0)), r=["ustb", "Mall"], w=[prn])
                    for j in range(t):
                        op("pe", lambda: PE.matmul(pr[:, t % 8, :], lhsT=onesb2[:], rhs=Mall[:, j, :], start=False, stop=(j == t - 1)), r=["onesb2", "Mall"], w=[prn])
                for q_ in range(2):
                    op("dve", lambda: TT_(R["rk"][:, q_ * 8:q_ * 8 + 8, :], prk2[q_][:], ecap[:].unsqueeze(1).to_broadcast([128, 8, 64]), ALU.add), r=[f"prk{q_}", "ecap"], w=[k("rk")])
                for (Mn, dn, ci_) in (("M1", "d1f", 0), ("M2", "d2f", 1)):
                    op("dve", lambda: TT_(R["t3"][:], R["rk"][:], R[Mn][:], ALU.mult), r=[k("rk"), k(Mn)], w=[k("t3")])
                    op("dve", lambda: V.tensor_reduce(out=R[dn][:, :, 0], in_=R["t3"][:], axis=AX.X, op=ALU.add), r=[k("t3")], w=[k(dn)])
                    op("dve", lambda: V.tensor_copy(out=dall[:, :, ci_:ci_ + 1], in_=R[dn][:]), r=[k(dn)], w=["dall"])
                for t in range(NT):
                    i = t % 2
                    dma("sp", hn2b[i][:], hn2_s[t * 128:(t + 1) * 128, :], r=["hn2_s"], w=[f"hn2c{i}"])
                    for ci_ in range(2):
                        dma("pool", None, None, r=[f"hn2c{i}", "dall"], w=["Xs"], key="Xs",
                            indirect=lambda: G.indirect_dma_start(out=Xs[:, :], out_offset=IOA(ap=dall[:, t, ci_:ci_ + 1], axis=0), in_=hn2b[i][:], in_offset=None))
                S.barrier()
            S.barrier()
        if stage < 5:
            S.finish()
            return nc, dbg
        with contextlib.ExitStack() as st:
            Xe = [sb(f"Xe{i}", [128, D], BF16, st) for i in range(3)]
            XeT2 = [sb(f"XeT{i}", [128, 8, 128], BF16, st) for i in range(3)]
            sg2 = [sb(f"sg{i}", [128, 256], F32, st) for i in range(2)]
            actb2 = [sb(f"actb{i}", [128, 256], BF16, st) for i in range(2)]
            actT2 = [sb(f"actT{i}", [128, 2, 128], BF16, st) for i in range(2)]
            Ye = [sb(f"Ye{i}", [128, D], F32, st) for i in range(2)]
            gfb = sb("gfb", [128, D], F32, st)
            dma("sp", gfb[:], final_g.partition_broadcast(128), w=["gfb"])
            ya2 = [sb(f"ya{i}", [128, D], F32, st) for i in range(4)]
            yb2 = [sb(f"yb{i}", [128, D], F32, st) for i in range(4)]
            hh = [sb(f"hh{i}", [128, D], F32, st) for i in range(2)]
            oo = [sb(f"oo{i}", [128, D], F32, st) for i in range(2)]
            junk3 = sb("junk3", [128, D], BF16, st)
            ssf = sb("ssf", [128, 1], F32, st)
            with contextlib.ExitStack() as st1:
                ptx = ps("ptx", [128, 8, 128], BF16, st1)
                pta = ps("pta", [128, 8, 128], BF16, st1)
                ph = [ps(f"ph{i}", [128, 512], F32, st1) for i in range(2)]
                pyy = [ps(f"pyy{i}", [128, 512], F32, st1) for i in range(2)]
                NX = 3
                def st1_(e):
                    xi = e % NX
                    for kt in range(8):
                        op("pe", lambda: PE.transpose(ptx[:, kt, :], Xe[xi][:].rearrange("s (p k) -> s k p", k=8)[:, kt, :], identb[:]), r=[f"Xe{xi}", "identb"], w=["ptx"])
                    op("act", lambda: A.copy(out=XeT2[xi][:], in_=ptx[:]), r=["ptx"], w=[f"XeT{xi}"])

                def st2_(e):
                    wi, xi, pi = e % NB, e % NX, e % 2
                    for gu in range(2):
                        for kt in range(8):
                            op("pe", lambda: PE.matmul(ph[pi][:, gu * DE:(gu + 1) * DE], lhsT=XeT2[xi][:, kt, :], rhs=wgu[wi][:, gu, kt, :], start=(kt == 0), stop=(kt == 7)),
                               r=[f"XeT{xi}", f"wgu{wi}"], w=[f"ph{pi}"])
                    op("act", lambda: A.activation(out=sg2[pi][:], in_=ph[pi][:, 0:256], func=AF.Silu), r=[f"ph{pi}"], w=[f"sg{pi}"])
                    op("dve", lambda: V.tensor_tensor(out=actb2[pi][:], in0=sg2[pi][:], in1=ph[pi][:, 256:512], op=ALU.mult), r=[f"sg{pi}", f"ph{pi}"], w=[f"actb{pi}"])
                    if PRECAST and e + NB < NEXP:
                        load_expert(e + NB, "gu")

                def st3_(e):
                    pi = e % 2
                    for jt in range(2):
                        op("pe", lambda: PE.transpose(pta[:, jt, :], actb2[pi][:, jt * 128:(jt + 1) * 128], identb[:]), r=[f"actb{pi}", "identb"], w=["pta"])
                    op("act", lambda: A.copy(out=actT2[pi][:], in_=pta[:, 0:2, :]), r=["pta"], w=[f"actT{pi}"])

                def st4_(e):
                    wi, pi = e % NB, e % 2
                    for half in range(2):
                        for jt in range(2):
                            op("pe", lambda: PE.matmul(pyy[half][:, :], lhsT=actT2[pi][:, jt, :], rhs=wde[wi][:, jt, half * 512:(half + 1) * 512], start=(jt == 0), stop=(jt == 1)),
                               r=[f"actT{pi}", f"wde{wi}"], w=[f"pyy{half}"])
                    op("act", lambda: A.copy(out=Ye[pi][:, 0:512], in_=pyy[0][:, :]), r=["pyy0"], w=[f"Ye{pi}"])
                    op("dve", lambda: V.tensor_copy(out=Ye[pi][:, 512:1024], in_=pyy[1][:, :]), r=["pyy1"], w=[f"Ye{pi}"])
                    dma("sp", Ys[e * CAP:(e + 1) * CAP, :], Ye[pi][:], r=[f"Ye{pi}"], w=["Ys"], key="Ys")
                    if e + NB < NEXP:
                        load_expert(e + NB, "d" if PRECAST else "both")

                def xload(e):
                    if 0 <= e < NEXP:
                        dma("act", Xe[e % NX][:], Xs[e * CAP:(e + 1) * CAP, :], r=["Xs"], w=[f"Xe{e % NX}"])

                xload(0)
                xload(1)
                for s_ in range(NEXP + 3):
                    xload(s_ + 2)
                    if s_ < NEXP:
                        st1_(s_)
                    if 0 <= s_ - 1 < NEXP:
                        st2_(s_ - 1)
                    if 0 <= s_ - 2 < NEXP:
                        st3_(s_ - 2)
                    if 0 <= s_ - 3 < NEXP:
                        st4_(s_ - 3)
                for t in range(NT):
                    i = t % 2
                    ya, yb, kya, kyb = ya2[t % 4], yb2[t % 4], f"ya{t % 4}", f"yb{t % 4}"
                    dma("sp", hh[i][:], h2_s[t * 128:(t + 1) * 128, :], r=["h2_s"], w=[f"hh{i}"])
                    dma("pool", None, None, r=["Ys", "dall"], w=[kya], key=kya,
                        indirect=lambda: G.indirect_dma_start(out=ya[:], out_offset=None, in_=Ys[:, :], in_offset=IOA(ap=dall[:, t, 0:1], axis=0)))
                    dma("pool", None, None, r=["Ys", "dall"], w=[kyb], key=kyb,
                        indirect=lambda: G.indirect_dma_start(out=yb[:], out_offset=None, in_=Ys[:, :], in_offset=IOA(ap=dall[:, t, 1:2], axis=0)))
                    op("dve", lambda: V.scalar_tensor_tensor(out=hh[i][:], in0=ya[:], scalar=call[:, t, 0:1], in1=hh[i][:], op0=ALU.mult, op1=ALU.add),
                       r=[kya, "call", f"hh{i}"], w=[f"hh{i}"])
                    op("dve", lambda: V.scalar_tensor_tensor(out=hh[i][:], in0=yb[:], scalar=call[:, t, 1:2], in1=hh[i][:], op0=ALU.mult, op1=ALU.add),
                       r=[kyb, "call", f"hh{i}"], w=[f"hh{i}"])
                    op("act", lambda: A.activation(out=junk3[:], in_=hh[i][:], func=AF.Square, accum_out=ssf[:]), r=[f"hh{i}"], w=["junk3", "ssf"])
                    op("act", lambda: A.activation(out=ssf[:], in_=ssf[:], func=AF.Sqrt, scale=1.0 / D, bias=epst[:]), r=["ssf", "epst"], w=["ssf"])
                    op("dve", lambda: V.reciprocal(out=ssf[:], in_=ssf[:]), r=["ssf"], w=["ssf"])
                    op("dve", lambda: V.scalar_tensor_tensor(out=oo[i][:], in0=hh[i][:], scalar=ssf[:, 0:1], in1=gfb[:], op0=ALU.mult, op1=ALU.mult),
                       r=[f"hh{i}", "ssf", "gfb"], w=[f"oo{i}"])
                    dma("sp", out[t * 128:(t + 1) * 128, :], oo[i][:], r=[f"oo{i}"], key=f"oo{i}")
                S.barrier()
            S.barrier()
        S.finish()
    return nc, dbg


def host_consts():
    c = {}
    c["c_identb"] = np.eye(128, dtype=np.float32).astype(ml_dtypes.bfloat16)
    c["c_identf"] = np.eye(128, dtype=np.float32)
    p = np.arange(L, dtype=np.float64)
    ang = 2.0 * np.pi * ((p[:, None] * p[None, :]) % L) / L
    c["c_cosL"] = np.cos(ang).astype(np.float32).astype(ml_dtypes.bfloat16)
    c["c_nsinL"] = (-np.sin(ang)).astype(np.float32).astype(ml_dtypes.bfloat16)
    q = np.arange(128, dtype=np.float64)
    a2 = 2.0 * np.pi * ((q[:, None] * q[None, :]) % 128) / 128
    sc = 1.0 / np.sqrt(L * 128.0)
    c["c_cs128"] = np.concatenate([np.cos(a2) * sc, np.sin(a2) * sc], axis=1).astype(np.float32).astype(ml_dtypes.bfloat16)
    c["c_masks"] = make_masks()
    c["c_ecap"] = np.tile((np.arange(64, dtype=np.float32) * CAP)[None, :], (128, 1))
    return c


def make_masks():
    i = np.arange(128)
    same = (i[:, None] // 64) == (i[None, :] // 64)
    m = np.zeros((7, 128, 128), np.float32)
    m[0] = (same & (i[:, None] <= i[None, :])).astype(np.float32)
    m[1] = (same & (i[:, None] >= i[None, :])).astype(np.float32)
    BIG = 30000.0
    m[2] = np.where(same & (i[None, :] < i[:, None]), 0.0, -BIG)
    m[3] = np.where(same & (i[None, :] > i[:, None]), 0.0, -BIG)
    m[4] = np.where(same & (i[:, None] <= i[None, :]), 0.0, BIG)
    m[5] = np.where(same & (i[:, None] >= i[None, :]), 0.0, BIG)
    m[6] = (i[:, None] < i[None, :]).astype(np.float32)
    return m


_CACHE = {}
PARAM_NAMES = ["meta_tokens", "norm1_g", "w_in", "conv_w", "a_log_fwd", "dt_bias_fwd", "a_log_bwd", "dt_bias_bwd",
               "out_norm_g", "w_fourier", "w_delta", "w_out", "norm2_g", "w_router_group", "b_router_group",
               "w_router_expert", "b_router_expert", "w_gate_e", "w_up_e", "w_down_e", "final_norm_g"]


def core_inputs(inputs, b):
    m = {"x": np.ascontiguousarray(np.asarray(inputs["x"])[b], dtype=np.float32)}
    for k in PARAM_NAMES:
        m[k] = np.ascontiguousarray(np.asarray(inputs[k]), dtype=np.float32)
    return m


def kernel(**inputs):
    if "nc" not in _CACHE:
        _CACHE["nc"] = build()[0]
        _CACHE["consts"] = host_consts()
    nc = _CACHE["nc"]
    maps = []
    for b in range(8):
        m = core_inputs(inputs, b)
        m.update(_CACHE["consts"])
        maps.append(m)
    res = run_bass_kernel_spmd(nc, maps, core_ids=list(range(8)))
    return np.stack([np.asarray(r["out"], dtype=np.float32) for r in res.results], axis=0)
```
